# Optimizing a Trainium2 kernel written in Bass

```python
import jax
import jax.numpy as jnp
from jax import lax
import numpy as np

D_MODEL = 1024
BATCH = 4
SEQ = 4096
DEPTH = 1

CTX_LEN = 256
GRID_W = 64
EPS = 1e-6

HG_HEADS = 4
HG_DK = 128
HG_DV = 128
HG_KEY_WIDTH = HG_HEADS * HG_DK
HG_WIDTH = HG_HEADS * HG_DV
HG_CHUNK = 16

CM_CHUNK = 128
CM_GROUPS = 4
CM_WIDTH = 512
CM_GROUP_DIM = CM_WIDTH // CM_GROUPS
ROWS_PER_CHUNK = CM_CHUNK // GRID_W

SPLIT_IDX = (HG_KEY_WIDTH, 2 * HG_KEY_WIDTH, 3 * HG_KEY_WIDTH, 3 * HG_KEY_WIDTH + HG_WIDTH,
             3 * HG_KEY_WIDTH + 2 * HG_WIDTH, 3 * HG_KEY_WIDTH + 2 * HG_WIDTH + 2 * CM_WIDTH)
D_IN = 3 * HG_KEY_WIDTH + 2 * HG_WIDTH + 2 * CM_WIDTH + 2 * D_MODEL

N_EXPERTS = 64
TOP_K = 8
N_EXPERT_GROUPS = 8
TOPK_GROUPS = 4
D_EXPERT = 256
D_SHARED = 256
ROUTED_SCALE = 2.5
MOE_BLOCK = 256

kernel_name = 'hybrid_hgrn2_chunkmlp_moe_prefix_dit'


def rms_norm(x, g):
    x32 = x.astype(jnp.float32)
    y = x32 * lax.rsqrt(jnp.mean(x32 * x32, axis=-1, keepdims=True) + EPS)
    return (y * g.astype(jnp.float32)).astype(x.dtype)


def layer_norm(x, g, b):
    x32 = x.astype(jnp.float32)
    xc = x32 - jnp.mean(x32, axis=-1, keepdims=True)
    y = xc * lax.rsqrt(jnp.mean(xc * xc, axis=-1, keepdims=True) + EPS)
    return (y * g.astype(jnp.float32) + b.astype(jnp.float32)).astype(x.dtype)


def hgrn2_gates(z, lb):
    B, T, _ = z.shape
    z = z.astype(jnp.float32).reshape(B, T, HG_HEADS, HG_DK)
    lb = lb.reshape(HG_HEADS, HG_DK)
    log_f = jnp.log(lb + (1.0 - lb) * jax.nn.sigmoid(z))
    k = (1.0 - lb) * jax.nn.sigmoid(-z)
    return k, log_f


def gla_chunk_scan(q, k, v, log_f, s0, readout):
    B, T, H, DK = k.shape
    DV = v.shape[-1]
    n = T // HG_CHUNK

    def to_chunks(a):
        return a.astype(jnp.float32).reshape(B, n, HG_CHUNK, H, a.shape[-1]).transpose(1, 0, 3, 2, 4)

    kc, vc = to_chunks(k), to_chunks(v)
    bc = jnp.cumsum(to_chunks(log_f), axis=3)
    tri = jnp.tril(jnp.ones((HG_CHUNK, HG_CHUNK), dtype=bool))
    mid = HG_CHUNK // 2 - 1

    def step(S, xs):
        k_, v_, b_ = xs[0], xs[1], xs[2]
        b_last = b_[:, :, -1:, :]
        S_new = jnp.exp(b_last[:, :, 0, :, None]) * S + jnp.einsum('bhsd,bhsv->bhdv', k_ * jnp.exp(b_last - b_), v_)
        if not readout:
            return S_new, None
        q_ = xs[3]
        b_mid = b_[:, :, mid:mid + 1, :]
        A = jnp.einsum('bhtd,bhsd->bhts', q_ * jnp.exp(b_ - b_mid), k_ * jnp.exp(b_mid - b_))
        A = jnp.where(tri, A, 0.0)
        o = jnp.einsum('bhts,bhsv->bhtv', A, v_) + jnp.einsum('bhtd,bhdv->bhtv', q_ * jnp.exp(b_), S)
        return S_new, o

    xs = (kc, vc, bc, to_chunks(q)) if readout else (kc, vc, bc)
    S_fin, o = lax.scan(step, s0, xs)
    if readout:
        o = o.transpose(1, 0, 3, 2, 4).reshape(B, T, H, DV)
    return o, S_fin


def chunk_spatial_gating(p, n_chunks, ln_g, ln_b, w_s, b_s):
    a = jax.nn.gelu(p)
    u, v = jnp.split(a, 2, axis=-1)
    v = layer_norm(v, ln_g, ln_b)
    B, T, _ = v.shape
    v = v.reshape(B, n_chunks, CM_CHUNK, CM_GROUPS, CM_GROUP_DIM)
    z = jnp.einsum('gpq,bnqgc->bnpgc', w_s, v) + b_s[None, None, :, :, None]
    return u * z.reshape(B, T, CM_WIDTH)


def token_mixer(h_lat, h_ctx, w_in, lb, g_out, ln_g, ln_b, w_s, b_s, w_a, w_b, w_o,
                n_lat_chunks, n_ctx_chunks, need_ctx):
    B, T, _ = h_lat.shape
    L = h_ctx.shape[1]
    p_lat = h_lat @ w_in
    p_ctx = h_ctx @ w_in
    ql, fl_fwd, fl_bwd, il, gl, cml, gtl = jnp.split(p_lat, SPLIT_IDX, axis=-1)
    qc, fc_fwd, fc_bwd, ic, gc, cmc, gtc = jnp.split(p_ctx, SPLIT_IDX, axis=-1)

    q_lat = jax.nn.silu(ql).reshape(B, T, HG_HEADS, HG_DK)
    v_lat = il.reshape(B, T, HG_HEADS, HG_DV)
    q_ctx = jax.nn.silu(qc).reshape(B, L, HG_HEADS, HG_DK) if need_ctx else None
    v_ctx = ic.reshape(B, L, HG_HEADS, HG_DV)
    s0 = jnp.zeros((B, HG_HEADS, HG_DK, HG_DV), jnp.float32)

    lat_outs, ctx_outs = [], []
    for d, (f_lat, f_ctx) in enumerate(((fl_fwd, fc_fwd), (fl_bwd, fc_bwd))):
        rev = (lambda a: a[:, ::-1]) if d == 1 else (lambda a: a)
        k_l, lf_l = hgrn2_gates(f_lat, lb[d])
        k_c, lf_c = hgrn2_gates(f_ctx, lb[d])
        q_c = rev(q_ctx) if need_ctx else None
        oc, s_ctx = gla_chunk_scan(q_c, rev(k_c), rev(v_ctx), rev(lf_c), s0, need_ctx)
        ol, _ = gla_chunk_scan(rev(q_lat), rev(k_l), rev(v_lat), rev(lf_l), s_ctx, True)
        lat_outs.append(rev(ol))
        if need_ctx:
            ctx_outs.append(rev(oc))

    def merge(o, g, cm, gt, n_chunks, dtype):
        n_tok = o.shape[1]
        g = g.reshape(B, n_tok, HG_HEADS, HG_DV).astype(jnp.float32)
        a = (rms_norm(o, g_out) * jax.nn.silu(g)).reshape(B, n_tok, HG_WIDTH).astype(dtype)
        bm = chunk_spatial_gating(cm, n_chunks, ln_g, ln_b, w_s, b_s)
        ga, gb = jnp.split(gt, 2, axis=-1)
        y = jax.nn.sigmoid(ga) * (a @ w_a) + jax.nn.sigmoid(gb) * (bm @ w_b)
        return y @ w_o

    y_lat = merge(lat_outs[0] + lat_outs[1], gl, cml, gtl, n_lat_chunks, h_lat.dtype)
    y_ctx = merge(ctx_outs[0] + ctx_outs[1], gc, cmc, gtc, n_ctx_chunks, h_ctx.dtype) if need_ctx else None
    return y_lat, y_ctx


def swiglu(h, w_gu, w_down):
    gate, up = jnp.split(h @ w_gu, 2, axis=-1)
    return (jax.nn.silu(gate) * up) @ w_down


def moe_ffn(h, w_router, b_router, w_gu, w_down, w_sh_gu, w_sh_down):
    N, D = h.shape
    scores = jax.nn.sigmoid((h @ w_router).astype(jnp.float32))
    sel = scores + b_router.astype(jnp.float32)
    grp = sel.reshape(N, N_EXPERT_GROUPS, N_EXPERTS // N_EXPERT_GROUPS)
    grp_score = jnp.sum(lax.top_k(grp, 2)[0], axis=-1)
    _, top_g = lax.top_k(grp_score, TOPK_GROUPS)
    gmask = jnp.any(top_g[:, :, None] == jnp.arange(N_EXPERT_GROUPS)[None, None, :], axis=1)
    sel = jnp.where(jnp.repeat(gmask, N_EXPERTS // N_EXPERT_GROUPS, axis=1), sel, -jnp.inf)
    _, top_e = lax.top_k(sel, TOP_K)
    w = jnp.take_along_axis(scores, top_e, axis=1)
    w = w / jnp.sum(w, axis=-1, keepdims=True) * ROUTED_SCALE

    M = N * TOP_K
    flat_e = top_e.reshape(M).astype(jnp.int32)
    flat_tok = jnp.repeat(jnp.arange(N, dtype=jnp.int32), TOP_K)
    flat_w = w.reshape(M)
    counts = jnp.bincount(flat_e, length=N_EXPERTS).astype(jnp.int32)
    padded = (counts + MOE_BLOCK - 1) // MOE_BLOCK * MOE_BLOCK
    pad_end = jnp.cumsum(padded)
    pad_start = pad_end - padded
    start = jnp.cumsum(counts) - counts
    order = jnp.argsort(flat_e)
    se, stok, sw = flat_e[order], flat_tok[order], flat_w[order]
    dest = pad_start[se] + jnp.arange(M, dtype=jnp.int32) - start[se]
    NB = -(-M // MOE_BLOCK) + N_EXPERTS
    row_tok = jnp.full((NB * MOE_BLOCK,), N, jnp.int32).at[dest].set(stok)
    row_w = jnp.zeros((NB * MOE_BLOCK,), jnp.float32).at[dest].set(sw)
    block_e = jnp.clip(jnp.searchsorted(pad_end, jnp.arange(NB, dtype=jnp.int32) * MOE_BLOCK, side='right'),
                       0, N_EXPERTS - 1)
    h_pad = jnp.concatenate([h, jnp.zeros((1, D), h.dtype)], axis=0)

    def expert_block(acc, blk):
        tok, wts, e = blk
        out = swiglu(h_pad[tok], w_gu[e], w_down[e])
        return acc.at[tok].add(out * wts[:, None].astype(out.dtype)), None

    acc, _ = lax.scan(expert_block, jnp.zeros((N + 1, D), h.dtype),
                      (row_tok.reshape(NB, MOE_BLOCK), row_w.reshape(NB, MOE_BLOCK), block_e))
    return acc[:N] + swiglu(h, w_sh_gu, w_sh_down)


def setup_inputs(seed: int = 0) -> dict:
    key = jax.random.key(seed)
    ks = jax.random.split(key, 26)

    def nrm(k, shape, scale):
        return jax.random.normal(k, shape, jnp.float32) * scale

    def gain(k, shape):
        return 1.0 + nrm(k, shape, 0.05)

    D = D_MODEL
    return {
        'x': nrm(ks[0], (BATCH, SEQ, D), 1.0),
        'c': nrm(ks[1], (BATCH, D), 1.0),
        'ctx': nrm(ks[2], (BATCH, CTX_LEN, D), 1.0),
        'c_ctx': nrm(ks[3], (D,), 1.0),
        'w_ada': nrm(ks[4], (DEPTH, D, 6 * D), 0.5 * D ** -0.5),
        'b_ada': nrm(ks[5], (DEPTH, 6 * D), 0.02),
        'g_pre_mix': gain(ks[6], (DEPTH, D)),
        'g_post_mix': gain(ks[7], (DEPTH, D)),
        'g_pre_ffn': gain(ks[8], (DEPTH, D)),
        'g_post_ffn': gain(ks[9], (DEPTH, D)),
        'w_in': nrm(ks[10], (DEPTH, D, D_IN), D ** -0.5),
        'lb_logits': nrm(ks[11], (2, DEPTH + 1, HG_KEY_WIDTH), 0.1),
        'g_hgrn_out': gain(ks[12], (DEPTH, HG_DV)),
        'cm_ln_g': gain(ks[13], (DEPTH, CM_WIDTH)),
        'cm_ln_b': nrm(ks[14], (DEPTH, CM_WIDTH), 0.02),
        'w_spatial': nrm(ks[15], (DEPTH, CM_GROUPS, CM_CHUNK, CM_CHUNK), CM_CHUNK ** -0.5),
        'b_spatial': 1.0 + nrm(ks[16], (DEPTH, CM_CHUNK, CM_GROUPS), 0.1),
        'w_branch_a': nrm(ks[17], (DEPTH, HG_WIDTH, D), HG_WIDTH ** -0.5),
        'w_branch_b': nrm(ks[18], (DEPTH, CM_WIDTH, D), CM_WIDTH ** -0.5),
        'w_out': nrm(ks[19], (DEPTH, D, D), D ** -0.5),
        'w_router': nrm(ks[20], (DEPTH, D, N_EXPERTS), D ** -0.5),
        'b_router': nrm(ks[21], (DEPTH, N_EXPERTS), 0.01),
        'w_expert_gu': nrm(ks[22], (DEPTH, N_EXPERTS, D, 2 * D_EXPERT), D ** -0.5),
        'w_expert_down': nrm(ks[23], (DEPTH, N_EXPERTS, D_EXPERT, D), D_EXPERT ** -0.5),
        'w_shared_gu': nrm(ks[24], (DEPTH, D, 2 * D_SHARED), D ** -0.5),
        'w_shared_down': nrm(ks[25], (DEPTH, D_SHARED, D), D_SHARED ** -0.5),
    }


def reference(x, c, ctx, c_ctx, w_ada, b_ada, g_pre_mix, g_post_mix, g_pre_ffn, g_post_ffn, w_in,
              lb_logits, g_hgrn_out, cm_ln_g, cm_ln_b, w_spatial, b_spatial, w_branch_a, w_branch_b,
              w_out, w_router, b_router, w_expert_gu, w_expert_down, w_shared_gu, w_shared_down):
    B, T, D = x.shape
    L = ctx.shape[1]
    rows = T // GRID_W
    n_lat_chunks = rows // ROWS_PER_CHUNK
    n_ctx_chunks = L // CM_CHUNK
    lower_bounds = jnp.cumsum(jax.nn.softmax(lb_logits.astype(jnp.float32), axis=1), axis=1)
    silu_c = jax.nn.silu(c)
    silu_cc = jax.nn.silu(c_ctx)
    x_lat, x_ctx = x, ctx
    for l in range(DEPTH):
        need_ctx = l < DEPTH - 1
        mod_lat = (silu_c @ w_ada[l] + b_ada[l])[:, None, :]
        mod_ctx = silu_cc @ w_ada[l] + b_ada[l]
        sh1, sc1, gt1, sh2, sc2, gt2 = jnp.split(mod_lat, 6, axis=-1)
        csh1, csc1, cgt1, csh2, csc2, cgt2 = jnp.split(mod_ctx, 6, axis=-1)

        h_lat = rms_norm(x_lat, g_pre_mix[l]) * (1.0 + sc1) + sh1
        h_ctx = rms_norm(x_ctx, g_pre_mix[l]) * (1.0 + csc1) + csh1
        y_lat, y_ctx = token_mixer(h_lat, h_ctx, w_in[l], lower_bounds[:, l], g_hgrn_out[l], cm_ln_g[l],
                                   cm_ln_b[l], w_spatial[l], b_spatial[l], w_branch_a[l], w_branch_b[l],
                                   w_out[l], n_lat_chunks, n_ctx_chunks, need_ctx)
        x_lat = x_lat + gt1 * rms_norm(y_lat, g_post_mix[l])
        if need_ctx:
            x_ctx = x_ctx + cgt1 * rms_norm(y_ctx, g_post_mix[l])

        h2_lat = (rms_norm(x_lat, g_pre_ffn[l]) * (1.0 + sc2) + sh2).reshape(B * T, D)
        if need_ctx:
            h2_ctx = (rms_norm(x_ctx, g_pre_ffn[l]) * (1.0 + csc2) + csh2).reshape(B * L, D)
            f_all = moe_ffn(jnp.concatenate([h2_lat, h2_ctx], axis=0), w_router[l], b_router[l],
                            w_expert_gu[l], w_expert_down[l], w_shared_gu[l], w_shared_down[l])
            f_lat, f_ctx = f_all[:B * T], f_all[B * T:]
            x_ctx = x_ctx + cgt2 * rms_norm(f_ctx.reshape(B, L, D), g_post_ffn[l])
        else:
            f_lat = moe_ffn(h2_lat, w_router[l], b_router[l], w_expert_gu[l], w_expert_down[l],
                            w_shared_gu[l], w_shared_down[l])
        x_lat = x_lat + gt2 * rms_norm(f_lat.reshape(B, T, D), g_post_ffn[l])
    return x_lat
```

```python
import os
import numpy as np
from contextlib import ExitStack
import concourse.bass as bass
import concourse.mybir as mybir
from concourse.bass_utils import run_bass_kernel_spmd

F32 = mybir.dt.float32
BF16 = mybir.dt.bfloat16
AF = mybir.ActivationFunctionType
ALU = mybir.AluOpType

NT = 2048
D = 1024
EPS = 1e-6
NE = 64


class Tok:
    __slots__ = ("sem", "val", "eng")

    def __init__(self, sem, val, eng):
        self.sem, self.val, self.eng = sem, val, eng


class Buf:
    def __init__(self, name):
        self.name = name
        self.w = None
        self.r = []
        self.dsem = None
        self.dcnt = 0
        self.excl = False


class TT:
    def __init__(self, t, name):
        self.t = t
        self.b = Buf(name)

    def __getitem__(self, k):
        return self.t[k]


class Sched:
    def __init__(self, nc, es):
        self.nc, self.es = nc, es
        self.eng = {"pe": nc.tensor, "act": nc.scalar, "dve": nc.vector, "pool": nc.gpsimd, "sp": nc.sync}
        self.esem, self.ecnt = {}, {}
        for e in ("pe", "act", "dve", "pool"):
            self.esem[e] = es.enter_context(nc.semaphore("sem_" + e))
            self.ecnt[e] = 0
        self.seen = {e: {} for e in self.eng}
        self.dsems = []
        self.nwaits = 0
        self.ninstr = {e: 0 for e in self.eng}

    def _wait(self, e, tok):
        key = id(tok.sem)
        if self.seen[e].get(key, 0) >= tok.val:
            return
        if tok.eng != "dma":
            assert tok.val <= self.ecnt[tok.eng], "wait on future inc (%s on %s)" % (e, tok.eng)
        self.eng[e].wait_ge(tok.sem, tok.val)
        self.seen[e][key] = tok.val
        self.nwaits += 1

    def _deps(self, e, reads, writes):
        same_ok = (e == "pe") or os.environ.get("KD_SAMEENG", "1") == "0"
        for b in reads:
            if b.w is not None:
                self._wait(e, b.w)
            if b.excl:
                for t in b.r:
                    if t.eng != e:
                        self._wait(e, t)
        for b in writes:
            if b.w is not None and not (same_ok and b.w.eng == e):
                self._wait(e, b.w)
            for t in b.r:
                if not (same_ok and t.eng == e):
                    self._wait(e, t)

    @staticmethod
    def _compact(toks):
        best = {}
        for t in toks:
            k = id(t.sem)
            if k not in best or best[k].val < t.val:
                best[k] = t
        return list(best.values())

    def _mark(self, tok, reads, writes):
        for b in reads:
            b.r.append(tok)
            if len(b.r) > 32:
                b.r = self._compact(b.r)
        for b in writes:
            b.w = tok
            b.r = []

    def op(self, e, fn, reads=(), writes=(), inc=True):
        reads = [x.b if isinstance(x, TT) else x for x in reads]
        writes = [x.b if isinstance(x, TT) else x for x in writes]
        self._deps(e, reads, writes)
        ins = fn(self.eng[e])
        self.ninstr[e] += 1
        if inc:
            self.ecnt[e] += 1
            ins.then_inc(self.esem[e], 1)
            tok = Tok(self.esem[e], self.ecnt[e], e)
        else:
            tok = Tok(self.esem[e], self.ecnt[e] + 1, e)
        self._mark(tok, reads, writes)
        return ins

    def dma(self, e, out, in_, reads=(), writes=()):
        reads = [x.b if isinstance(x, TT) else x for x in reads]
        writes = [x.b if isinstance(x, TT) else x for x in writes]
        self._deps(e, reads, writes)
        owner = writes[0] if writes else reads[0]
        if owner.dsem is None:
            owner.dsem = self.es.enter_context(self.nc.semaphore("ds%d_%s" % (len(self.dsems), owner.name)))
            self.dsems.append(owner)
        ins = self.eng[e].dma_start(out=out, in_=in_)
        owner.dcnt += 16
        ins.then_inc(owner.dsem, 16)
        self.ninstr[e] += 1
        self._mark(Tok(owner.dsem, owner.dcnt, "dma"), reads, writes)
        return ins

    def barrier(self):
        toks = [Tok(self.esem[e], self.ecnt[e], e) for e in self.esem if self.ecnt[e] > 0]
        toks += [Tok(b.dsem, b.dcnt, "dma") for b in self.dsems]
        for e in self.eng:
            for t in toks:
                if t.eng != e:
                    self._wait(e, t)


class Ring:
    def __init__(self, items):
        self.items, self.i = items, 0

    def next(self):
        it = self.items[self.i % len(self.items)]
        self.i += 1
        return it


def build_program(stage=99, dbg_w=0):
    nc = bass.Bass("TRN2", target_bir_lowering=False)

    def din(name, shape):
        return nc.dram_tensor(name, list(shape), F32, kind="ExternalInput").ap()

    x_own = din("x_own", [NT, D]); x_oth = din("x_oth", [NT, D]); ctx = din("ctx", [256, D])
    cvec = din("cvec", [128, 16]); w_ada = din("w_ada", [D, 6 * D]); b_ada = din("b_ada", [1, 6 * D])
    gains = din("gains", [1, 4 * D]); w_in = din("w_in", [D, 5632]); lbl = din("lbl", [128, 16])
    gout = din("gout", [128, 1]); lng = din("lng", [1, 512]); lnb = din("lnb", [1, 512])
    wsT = din("wsT", [128, 512]); bsp = din("bsp", [1, 512])
    w_a = din("w_a", [512, D]); w_b = din("w_b", [512, D]); w_o = din("w_o", [D, D])
    w_rt = din("w_rt", [D, NE]); b_rt = din("b_rt", [1, NE])
    w_gu = din("w_gu", [NE + 1, D, 512]); w_dn = din("w_dn", [NE + 1, 256, D])
    ident_d = din("ident", [128, 128]); maskA_d = din("maskA", [128, 128]); maskB_d = din("maskB", [128, 128])
    resetm_d = din("resetm", [128, 512])
    out = nc.dram_tensor("out", [NT, D], F32, kind="ExternalOutput").ap()
    x1s = nc.dram_tensor("x1s", [NT, D], F32).ap()
    h2Td = nc.dram_tensor("h2Td", [128, 8, NT], BF16).ap()
    h2Td_b = Buf("h2Td")
    g2s = nc.dram_tensor("g2s", [128, D], F32).ap()
    g2s_b = Buf("g2s")
    dbg = nc.dram_tensor("dbg", [128, dbg_w], F32, kind="ExternalOutput").ap() if dbg_w else None
    x1s_bs = [Buf("x1s%d" % i) for i in range(4)]; out_bs = [Buf("out%d" % i) for i in range(4)]; dbg_b = Buf("dbg")

    w_in_v = w_in.rearrange("(kc p) n -> p kc n", p=128)
    w_ada_v = w_ada.rearrange("(kc p) n -> p kc n", p=128)

    with ExitStack() as es:
        S = Sched(nc, es)

        uid = [0]

        def sb(scope, name, shape, dt):
            uid[0] += 1
            return TT(scope.enter_context(nc.sbuf_tensor("s%d_%s" % (uid[0], name), list(shape), dt)), name)

        def psb(scope, name, shape, dt):
            uid[0] += 1
            t = TT(scope.enter_context(nc.psum_tensor("p%d_%s" % (uid[0], name), list(shape), dt)), name)
            t.b.excl = True
            return t

        def mm(ps, out_ap, pairs, reads, first=True, last=True, inc=None):
            n = len(pairs)
            for i, (l, r) in enumerate(pairs):
                fin = (i == n - 1)
                S.op("pe", lambda e, l=l, r=r, i=i, fin=fin: e.matmul(out_ap, lhsT=l, rhs=r, start=(first and i == 0),
                                                                     stop=(last and fin)),
                     reads=reads, writes=[ps], inc=(fin if inc is None else (inc and fin)))

        dbg_col = [0]

        def dump(tt, ap, w):
            if dbg is None:
                return
            S.dma("pool", dbg[:, dbg_col[0]:dbg_col[0] + w], ap, reads=[tt], writes=[dbg_b])
            dbg_col[0] += w

        ident_bf = sb(es, "ident_bf", [128, 128], BF16)
        ident_f = sb(es, "ident_f", [128, 128], F32)
        maskA = sb(es, "maskA", [128, 128], BF16)
        maskB = sb(es, "maskB", [128, 128], BF16)
        resetm = sb(es, "resetm", [128, 512], F32)
        ones_bf = sb(es, "ones_bf", [128, 128], BF16)
        ones_f = sb(es, "ones_f", [128, 512], F32)
        mod = sb(es, "mod", [128, 2 * D], F32)
        wt_all = sb(es, "wt_all", [128, 16, NE + 1], F32)
        lbt = sb(es, "lbt", [128, 16], F32)
        lbv = sb(es, "lbv", [128, 8], F32)
        oml = sb(es, "oml", [128, 8], F32)
        noml = sb(es, "noml", [128, 8], F32)
        gout_t = sb(es, "gout_t", [128, 1], F32)
        screp = sb(es, "screp", [128, 16, 128], BF16)

        S.dma("pool", ident_bf[:], ident_d[:, :], writes=[ident_bf])
        S.dma("sp", ident_f[:], ident_d[:, :], writes=[ident_f])
        S.dma("pool", maskA[:], maskA_d[:, :], writes=[maskA])
        S.dma("pool", maskB[:], maskB_d[:, :], writes=[maskB])
        S.dma("sp", resetm[:], resetm_d[:, :], writes=[resetm])
        S.dma("sp", lbt[:], lbl[:, :], writes=[lbt])
        S.dma("sp", gout_t[:], gout[:, :], writes=[gout_t])
        S.op("dve", lambda e: e.memset(ones_bf[:], 1.0), writes=[ones_bf])
        S.op("dve", lambda e: e.memset(ones_f[:], 1.0), writes=[ones_f])
        S.op("dve", lambda e: e.memset(wt_all[:], 1.0), writes=[wt_all])
        lb3 = lbt[:].rearrange("p (d s h) -> p d s h", d=2, s=2)
        S.op("dve", lambda e: e.tensor_tensor(out=oml[:].rearrange("p (d h) -> p d h", d=2), in0=lb3[:, :, 0, :],
                                              in1=lb3[:, :, 1, :], op=ALU.subtract), reads=[lbt], writes=[oml])
        S.op("act", lambda e: e.activation(out=lbv[:], in_=oml[:], func=AF.Sigmoid), reads=[oml], writes=[lbv])
        S.op("dve", lambda e: e.tensor_scalar(out=oml[:], in0=lbv[:], scalar1=-1.0, scalar2=1.0, op0=ALU.mult, op1=ALU.add),
             reads=[lbv], writes=[oml])
        S.op("dve", lambda e: e.tensor_scalar(out=noml[:], in0=lbv[:], scalar1=1.0, scalar2=-1.0, op0=ALU.mult, op1=ALU.add),
             reads=[lbv], writes=[noml])

        def norm_pool(scope):
            np_ = dict(
                xts=Ring([sb(scope, "xt%d" % i, [128, D], F32) for i in range(3)]),
                junk=sb(scope, "junk", [128, D], BF16),
                hns=Ring([sb(scope, "hn%d" % i, [128, D], F32) for i in range(2)]),
                hbs=Ring([sb(scope, "hb%d" % i, [128, D], BF16) for i in range(3)]),
                stats=Ring([sb(scope, "nst%d" % i, [128, 4], F32) for i in range(4)]),
                tps=Ring([psb(scope, "tps%d" % i, [128, 1024], BF16) for i in range(2)]),
            )
            return np_

        def norm_a(np_, src_rows, src_bufs, Aap, Bap, mods):
            xt = np_["xts"].next(); hn = np_["hns"].next(); hb = np_["hbs"].next(); st = np_["stats"].next()
            junk = np_["junk"]
            S.dma("sp", xt[:], src_rows, reads=src_bufs, writes=[xt])
            S.op("act", lambda e: e.activation(out=junk[:], in_=xt[:], func=AF.Square, accum_out=st[:, 0:1]),
                 reads=[xt], writes=[junk, st])
            S.op("act", lambda e: e.activation(out=st[:, 1:2], in_=st[:, 0:1], func=AF.Sqrt, scale=1.0 / D, bias=EPS),
                 reads=[st], writes=[st])
            S.op("dve", lambda e: e.reciprocal(out=st[:, 2:3], in_=st[:, 1:2]), reads=[st], writes=[st])
            S.op("dve", lambda e: e.scalar_tensor_tensor(out=hn[:], in0=xt[:], scalar=st[:, 2:3], in1=Aap,
                                                          op0=ALU.mult, op1=ALU.mult), reads=[xt, st] + mods, writes=[hn])
            S.op("dve", lambda e: e.tensor_tensor(out=hb[:], in0=hn[:], in1=Bap, op=ALU.add),
                 reads=[hn] + mods, writes=[hb])
            return hb

        def norm_b(np_, hb, hT_dst, hT_tt):
            tp = np_["tps"].next()
            for kc in range(8):
                S.op("pe", lambda e, kc=kc: e.transpose(tp[:, kc * 128:(kc + 1) * 128], hb[:, kc * 128:(kc + 1) * 128],
                                                        ident_bf[:]),
                     reads=[hb, ident_bf], writes=[tp], inc=(kc == 7))
            S.op("act", lambda e: e.activation(out=hT_dst, in_=tp[:].rearrange("p (a b) -> p a b", a=8), func=AF.Copy),
                 reads=[tp], writes=[hT_tt])

        def norm_seq(np_, items):
            prev = None
            for it in items:
                hb = norm_a(np_, *it[:5])
                if prev is not None:
                    norm_b(np_, prev[0], prev[1], prev[2])
                prev = (hb, it[5], it[6])
            norm_b(np_, prev[0], prev[1], prev[2])

        with ExitStack() as sm:
            hT_own = sb(sm, "hT_own", [128, 8, NT], BF16)
            aT = sb(sm, "aT", [128, 4, NT], BF16)
            sw = ExitStack()
            wfA = sb(sw, "wfA", [128, 8, 512], BF16)
            wfB = sb(sw, "wfB", [128, 8, 512], BF16)
            wi = sb(sw, "wi", [128, 8, 512], BF16)
            Sst = [sb(sw, "SstA", [128, 4, 128], F32), sb(sw, "SstB", [128, 4, 128], F32)]
            S.dma("pool", wfA[:], w_in_v[:, :, 512:1024], writes=[wfA])
            S.dma("pool", wfB[:], w_in_v[:, :, 1024:1536], writes=[wfB])
            S.dma("pool", wi[:], w_in_v[:, :, 1536:2048], writes=[wi])

            with ExitStack() as s12:
                modc = sb(s12, "modc", [128, 2 * D], F32)
                with ExitStack() as p1:
                    cv = sb(p1, "cv", [128, 16], F32)
                    scv = sb(p1, "scv", [128, 16], F32)
                    gns = sb(p1, "gns", [128, D], F32)
                    wada = [sb(p1, "wada%d" % i, [128, 8, 512], BF16) for i in range(2)]
                    bada = [sb(p1, "bada%d" % i, [128, 512], F32) for i in range(2)]
                    pp = Ring([psb(p1, "p1ps%d" % i, [128, 512], F32) for i in range(4)])
                    S.dma("sp", cv[:], cvec[:, :], writes=[cv])
                    S.dma("sp", gns[:], gains[0:1, 0:D].partition_broadcast(128), writes=[gns])
                    S.op("act", lambda e: e.activation(out=scv[:], in_=cv[:], func=AF.Silu), reads=[cv], writes=[scv])
                    S.op("dve", lambda e: e.tensor_copy(out=screp[:], in_=scv[:].unsqueeze(2).to_broadcast([128, 16, 128])),
                         reads=[scv], writes=[screp])
                    for j in range(4):
                        wb_, bb_ = wada[j % 2], bada[j % 2]
                        S.dma("pool", wb_[:], w_ada_v[:, :, j * 512:(j + 1) * 512], writes=[wb_])
                        S.dma("sp", bb_[:], b_ada[0:1, j * 512:(j + 1) * 512].partition_broadcast(128), writes=[bb_])
                        ps = pp.next()
                        mm(ps, ps[:], [(screp[:, kc, :], wb_[:, kc, :]) for kc in range(8)], [screp, wb_])
                        S.op("dve", lambda e, ps=ps, bb_=bb_, j=j: e.tensor_tensor(out=mod[:, j * 512:(j + 1) * 512], in0=ps[:],
                                                                                    in1=bb_[:], op=ALU.add),
                             reads=[ps, bb_], writes=[mod])
                        if j < 4:
                            ps = pp.next()
                            mm(ps, ps[:], [(screp[:, 8 + kc, :], wb_[:, kc, :]) for kc in range(8)], [screp, wb_])
                            S.op("dve", lambda e, ps=ps, bb_=bb_, j=j: e.tensor_tensor(out=modc[:, j * 512:(j + 1) * 512],
                                                                                        in0=ps[:], in1=bb_[:], op=ALU.add),
                                 reads=[ps, bb_], writes=[modc])

                    def scale1p(dst, lo, g0):
                        S.op("dve", lambda e: e.scalar_tensor_tensor(out=dst[:, lo:lo + D], in0=dst[:, lo:lo + D], scalar=1.0,
                                                                      in1=gns[:, g0 * D:(g0 + 1) * D], op0=ALU.add, op1=ALU.mult),
                             reads=[dst, gns], writes=[dst])

                    def scaleg(dst, lo, g0):
                        S.op("dve", lambda e: e.tensor_tensor(out=dst[:, lo:lo + D], in0=dst[:, lo:lo + D],
                                                              in1=gns[:, g0 * D:(g0 + 1) * D], op=ALU.mult),
                             reads=[dst, gns], writes=[dst])
                    scale1p(mod, 1 * D, 0); scale1p(modc, 1 * D, 0)
                    if stage == 1:
                        dump(mod, mod[:, 0:2 * D], 2 * D); dump(modc, modc[:, :], 2 * D)
                    S.barrier()
                if stage == 1:
                    S.barrier()
                    return nc, S

                with ExitStack() as p2:
                    npl = norm_pool(p2)
                    tps = npl["tps"]
                    hTg = sb(p2, "hTg", [128, 8, 512], BF16)
                    vg = sb(p2, "vg", [128, 4, 512], BF16)
                    ktT = sb(p2, "ktT", [128, 4, 512], BF16)
                    ktoks = Ring([sb(p2, "ktok%d" % i, [128, 512], BF16) for i in range(2)])
                    carry = sb(p2, "carry", [128, 4], F32)
                    tots = sb(p2, "tots", [128, 4], F32)
                    tf = {n: Ring([sb(p2, "p2%s%d" % (n, i), [128, 512], F32) for i in range(4 if n == "sig" else 2)])
                          for n in ("sig", "lf", "k", "pin", "pex")}
                    S0ps = [psb(p2, "S0psA", [128, 512], F32), psb(p2, "S0psB", [128, 512], F32)]
                    pr = Ring([psb(p2, "p2ps%d" % i, [128, 512], F32) for i in range(4)])
                    S.op("dve", lambda e: e.memset(carry[:], 0.0), writes=[carry])
                    nB = [0]
                    totB = 18

                    def gate_chain_state(z, ncol, d, h, mode):
                        lf = tf["lf"].next(); kk = tf["k"].next(); pin = tf["pin"].next()
                        pex = tf["pex"].next()
                        c = d * 4 + h
                        sg = z
                        S.op("act", lambda e: e.activation(out=lf[:, :ncol], in_=sg[:, :ncol], func=AF.Ln, scale=oml[:, c:c + 1],
                                                           bias=lbv[:, c:c + 1]), reads=[sg, oml, lbv], writes=[lf])
                        S.op("dve", lambda e: e.tensor_scalar(out=kk[:, :ncol], in0=sg[:, :ncol], scalar1=noml[:, c:c + 1],
                                                               scalar2=oml[:, c:c + 1], op0=ALU.mult, op1=ALU.add),
                             reads=[sg, noml, oml], writes=[kk])
                        if mode == "B":
                            S.op("dve", lambda e: e.tensor_tensor_scan(out=pin[:, :ncol], data0=ones_f[:, :ncol], data1=lf[:, :ncol],
                                                                        initial=carry[:, h:h + 1], op0=ALU.mult, op1=ALU.add),
                                 reads=[ones_f, lf, carry], writes=[pin])
                            S.op("act", lambda e: e.activation(out=carry[:, h:h + 1], in_=pin[:, ncol - 1:ncol], func=AF.Copy),
                                 reads=[pin], writes=[carry])
                            S.op("dve", lambda e: e.tensor_tensor(out=pex[:, :ncol], in0=pin[:, :ncol], in1=lf[:, :ncol],
                                                                  op=ALU.subtract), reads=[pin, lf], writes=[pex])
                            S.op("act", lambda e: e.activation(out=pex[:, :ncol], in_=pex[:, :ncol], func=AF.Exp),
                                 reads=[pex], writes=[pex])
                        else:
                            S.op("dve", lambda e: e.tensor_tensor_scan(out=pin[:, :ncol], data0=ones_f[:, :ncol], data1=lf[:, :ncol],
                                                                        initial=0.0, op0=ALU.mult, op1=ALU.add),
                                 reads=[ones_f, lf], writes=[pin])
                            S.op("act", lambda e: e.activation(out=tots[:, h:h + 1], in_=pin[:, ncol - 1:ncol], func=AF.Copy),
                                 reads=[pin], writes=[tots])
                            S.op("act", lambda e: e.activation(out=pex[:, :ncol], in_=pin[:, :ncol], func=AF.Exp, scale=-1.0,
                                                               bias=tots[:, h:h + 1]), reads=[pin, tots], writes=[pex])
                        S.op("dve", lambda e: e.tensor_tensor(out=ktT[:, h, :ncol], in0=kk[:, :ncol], in1=pex[:, :ncol], op=ALU.mult),
                             reads=[kk, pex], writes=[ktT])

                    def state_accum(ntile, d):
                        for t in range(ntile):
                            tp = tps.next(); kt = ktoks.next()
                            for h in range(4):
                                S.op("pe", lambda e, h=h, t=t, tp=tp: e.transpose(tp[:, h * 128:(h + 1) * 128],
                                                                                   ktT[:, h, t * 128:(t + 1) * 128], ident_bf[:]),
                                     reads=[ktT, ident_bf], writes=[tp], inc=(h == 3))
                            S.op("act", lambda e, tp=tp, kt=kt: e.activation(out=kt[:], in_=tp[:, 0:512], func=AF.Copy),
                                 reads=[tp], writes=[kt])
                            for h in range(4):
                                if d == 1:
                                    st_ = (nB[0] == 0); nB[0] += 1; sp_ = (nB[0] > totB * 4 - 4)
                                else:
                                    st_ = (t == 0 and h == 0); sp_ = (t == ntile - 1)
                                S.op("pe", lambda e, h=h, t=t, kt=kt, st_=st_, sp_=sp_: e.matmul(
                                    S0ps[d][:, h * 128:(h + 1) * 128], lhsT=kt[:, h * 128:(h + 1) * 128],
                                    rhs=vg[:, t, h * 128:(h + 1) * 128], start=st_, stop=sp_, skip_group_check=True),
                                    reads=[kt, vg], writes=[S0ps[d]], inc=True)

                    for g in range(4):
                        norm_seq(npl, [(x_oth[(g * 4 + t) * 128:(g * 4 + t + 1) * 128, :], [], mod[:, D:2 * D], mod[:, 0:D], [mod],
                                        hTg[:, :, t * 128:(t + 1) * 128], hTg) for t in range(4)])
                        for t in range(4):
                            ps = pr.next()
                            mm(ps, ps[:], [(hTg[:, kc, t * 128:(t + 1) * 128], wi[:, kc, :]) for kc in range(8)], [hTg, wi])
                            S.op("act", lambda e, ps=ps, t=t: e.activation(out=vg[:, t, :], in_=ps[:], func=AF.Copy), reads=[ps], writes=[vg])
                        sgs_ = []
                        for h in range(4):
                            ps = pr.next()
                            mm(ps, ps[:], [(wfB[:, kc, h * 128:(h + 1) * 128], hTg[:, kc, :]) for kc in range(8)], [hTg, wfB])
                            sg = tf["sig"].next()
                            S.op("act", lambda e, ps=ps, sg=sg: e.activation(out=sg[:], in_=ps[:], func=AF.Sigmoid), reads=[ps], writes=[sg])
                            sgs_.append(sg)
                        for h in range(4):
                            gate_chain_state(sgs_[h], 512, 1, h, "B")
                        state_accum(4, 1)
                    norm_seq(npl, [(ctx[t * 128:(t + 1) * 128, :], [], modc[:, D:2 * D], modc[:, 0:D], [modc],
                                    hTg[:, :, t * 128:(t + 1) * 128], hTg) for t in range(2)])
                    for t in range(2):
                        ps = pr.next()
                        mm(ps, ps[:], [(hTg[:, kc, t * 128:(t + 1) * 128], wi[:, kc, :]) for kc in range(8)], [hTg, wi])
                        S.op("act", lambda e, ps=ps, t=t: e.activation(out=vg[:, t, :], in_=ps[:], func=AF.Copy), reads=[ps], writes=[vg])
                    for (wf_, d_, mode_) in ((wfB, 1, "B"), (wfA, 0, "A")):
                        sgs_ = []
                        for h in range(4):
                            ps = pr.next()
                            mm(ps, ps[:, 0:256], [(wf_[:, kc, h * 128:(h + 1) * 128], hTg[:, kc, 0:256]) for kc in range(8)], [hTg, wf_])
                            sg = tf["sig"].next()
                            S.op("act", lambda e, ps=ps, sg=sg: e.activation(out=sg[:, 0:256], in_=ps[:, 0:256], func=AF.Sigmoid), reads=[ps], writes=[sg])
                            sgs_.append(sg)
                        for h in range(4):
                            gate_chain_state(sgs_[h], 256, d_, h, mode_)
                        state_accum(2, d_)
                    assert nB[0] == totB * 4
                    for d in range(2):
                        S.op("act", lambda e, d=d: e.activation(out=Sst[d][:].rearrange("p a b -> p (a b)"), in_=S0ps[d][:], func=AF.Copy),
                             reads=[S0ps[d]], writes=[Sst[d]])
                    if stage == 2:
                        dump(Sst[0], Sst[0][:].rearrange("p a b -> p (a b)"), 512)
                        dump(Sst[1], Sst[1][:].rearrange("p a b -> p (a b)"), 512)
                    S.barrier()
                if stage == 2:
                    S.barrier()
                    return nc, S

            with ExitStack() as s3:
                npl = norm_pool(s3)
                norm_seq(npl, [(x_own[t * 128:(t + 1) * 128, :], [], mod[:, D:2 * D], mod[:, 0:D], [mod],
                                hT_own[:, :, t * 128:(t + 1) * 128], hT_own) for t in range(16)])
                S.barrier()

            with ExitStack() as sh:
                v_own = sb(sh, "v_own", [128, 16, 512], BF16)
                hps = Ring([psb(sh, "hps%d" % i, [128, 512], F32) for i in range(6)])
                tps = Ring([psb(sh, "tpsh%d" % i, [128, 1024], BF16) for i in range(2)])
                for t in range(16):
                    ps = hps.next()
                    mm(ps, ps[:], [(hT_own[:, kc, t * 128:(t + 1) * 128], wi[:, kc, :]) for kc in range(8)], [hT_own, wi])
                    S.op("act", lambda e, ps=ps, t=t: e.activation(out=v_own[:, t, :], in_=ps[:], func=AF.Copy), reads=[ps], writes=[v_own])
                qh = [sb(sh, "qh%d" % d, [128, NT], BF16) for d in range(2)]
                kh = [sb(sh, "kh%d" % d, [128, NT], BF16) for d in range(2)]
                ktok = [sb(sh, "ktokh%d" % d, [128, 16, 128], BF16) for d in range(2)]
                oacc = sb(sh, "oacc", [128, NT], F32)
                gT = sb(sh, "gT", [128, NT], BF16)
                expT = [sb(sh, "expT%d" % d, [128, 33], F32) for d in range(2)]
                for d in range(2):
                    S.op("dve", lambda e, d=d: e.memset(expT[d][:, 32:33], 1.0), writes=[expT[d]])
                wqs = Ring([sb(sh, "wq%d" % i, [128, 8, 128], BF16) for i in range(2)])
                wgs = Ring([sb(sh, "wg%d" % i, [128, 8, 128], BF16) for i in range(2)])
                tf = {n: Ring([sb(sh, "h%s%d" % (n, i), [128, 512], F32) for i in range(2)])
                      for n in ("qf", "sig", "lf", "k", "pin", "e1", "e2")}
                Sfb = [[sb(sh, "Sf%d_%d" % (d, i), [128, 128], F32) for i in range(2)] for d in range(2)]
                sfi = [0, 0]
                Sbf = [Ring([sb(sh, "Sbf%d_%d" % (d, i), [128, 128], BF16) for i in range(3)]) for d in range(2)]
                ATs = Ring([sb(sh, "ATs%d" % i, [128, 128], BF16) for i in range(4)])
                og_sq = sb(sh, "og_sq", [128, 512], BF16)
                og_sd = sb(sh, "og_sd", [128, 512], F32)
                og_t1 = sb(sh, "og_t1", [128, 512], F32)

                for h in range(4):
                    hc = slice(h * 128, (h + 1) * 128)
                    wq_h = wqs.next(); wg_h = wgs.next()
                    S.dma("pool", wq_h[:], w_in_v[:, :, h * 128:(h + 1) * 128], writes=[wq_h])
                    S.dma("pool", wg_h[:], w_in_v[:, :, 2048 + h * 128:2048 + (h + 1) * 128], writes=[wg_h])
                    for tg in range(4):
                        cs = slice(tg * 512, (tg + 1) * 512)
                        ps = hps.next()
                        mm(ps, ps[:], [(wq_h[:, kc, :], hT_own[:, kc, cs]) for kc in range(8)], [wq_h, hT_own])
                        qf = tf["qf"].next()
                        S.op("act", lambda e, ps=ps, qf=qf: e.activation(out=qf[:], in_=ps[:], func=AF.Silu), reads=[ps], writes=[qf])
                        ps = hps.next()
                        mm(ps, ps[:], [(wg_h[:, kc, :], hT_own[:, kc, cs]) for kc in range(8)], [wg_h, hT_own])
                        S.op("act", lambda e, ps=ps, cs=cs: e.activation(out=gT[:, cs], in_=ps[:], func=AF.Silu), reads=[ps], writes=[gT])
                        sgd = []
                        for d in range(2):
                            wf = wfA if d == 0 else wfB
                            ps = hps.next()
                            mm(ps, ps[:], [(wf[:, kc, hc], hT_own[:, kc, cs]) for kc in range(8)], [wf, hT_own])
                            sg = tf["sig"].next()
                            S.op("act", lambda e, ps=ps, sg=sg: e.activation(out=sg[:], in_=ps[:], func=AF.Sigmoid), reads=[ps], writes=[sg])
                            sgd.append(sg)
                        for d in range(2):
                            c = d * 4 + h
                            sg = sgd[d]
                            lf = tf["lf"].next(); kk = tf["k"].next(); pin = tf["pin"].next()
                            e1 = tf["e1"].next(); e2 = tf["e2"].next()
                            S.op("act", lambda e, sg=sg, lf=lf, c=c: e.activation(out=lf[:], in_=sg[:], func=AF.Ln, scale=oml[:, c:c + 1],
                                                                                  bias=lbv[:, c:c + 1]), reads=[sg, oml, lbv], writes=[lf])
                            S.op("dve", lambda e, sg=sg, kk=kk, c=c: e.tensor_scalar(out=kk[:], in0=sg[:], scalar1=noml[:, c:c + 1],
                                                                                      scalar2=oml[:, c:c + 1], op0=ALU.mult, op1=ALU.add),
                                 reads=[sg, noml, oml], writes=[kk])
                            S.op("dve", lambda e, pin=pin, lf=lf: e.tensor_tensor_scan(out=pin[:], data0=resetm[:], data1=lf[:], initial=0.0,
                                                                                        op0=ALU.mult, op1=ALU.add),
                                 reads=[resetm, lf], writes=[pin])
                            S.op("act", lambda e, pin=pin, d=d, tg=tg: e.activation(out=expT[d][:, tg * 8:(tg + 1) * 8], in_=pin[:, 63::64],
                                                                                    func=AF.Exp), reads=[pin], writes=[expT[d]])
                            if d == 0:
                                S.op("act", lambda e, pin=pin, e1=e1: e.activation(out=e1[:], in_=pin[:], func=AF.Exp), reads=[pin], writes=[e1])
                                S.op("act", lambda e, pin=pin, e2=e2: e.activation(out=e2[:], in_=pin[:], func=AF.Exp, scale=-1.0),
                                     reads=[pin], writes=[e2])
                            else:
                                S.op("dve", lambda e, pin=pin, lf=lf: e.tensor_tensor(out=pin[:], in0=pin[:], in1=lf[:], op=ALU.subtract),
                                     reads=[pin, lf], writes=[pin])
                                S.op("act", lambda e, pin=pin, e1=e1: e.activation(out=e1[:], in_=pin[:], func=AF.Exp, scale=-1.0),
                                     reads=[pin], writes=[e1])
                                S.op("act", lambda e, pin=pin, e2=e2: e.activation(out=e2[:], in_=pin[:], func=AF.Exp), reads=[pin], writes=[e2])
                            S.op("dve", lambda e, qf=qf, e1=e1, d=d, cs=cs: e.tensor_tensor(out=qh[d][:, cs], in0=qf[:], in1=e1[:], op=ALU.mult),
                                 reads=[qf, e1], writes=[qh[d]])
                            S.op("dve", lambda e, kk=kk, e2=e2, d=d, cs=cs: e.tensor_tensor(out=kh[d][:, cs], in0=kk[:], in1=e2[:], op=ALU.mult),
                                 reads=[kk, e2], writes=[kh[d]])
                    for d in range(2):
                        for t4 in range(4):
                            tp = tps.next()
                            for j in range(4):
                                t = t4 * 4 + j
                                S.op("pe", lambda e, tp=tp, j=j, t=t, d=d: e.transpose(tp[:, j * 128:(j + 1) * 128],
                                                                                        kh[d][:, t * 128:(t + 1) * 128], ident_bf[:]),
                                     reads=[kh[d], ident_bf], writes=[tp], inc=(j == 3))
                            S.op("act", lambda e, tp=tp, d=d, t4=t4: e.activation(out=ktok[d][:, t4 * 4:(t4 + 1) * 4, :],
                                                                                  in_=tp[:, 0:512].rearrange("p (a b) -> p a b", a=4),
                                                                                  func=AF.Copy), reads=[tp], writes=[ktok[d]])
                    qi = [0, 0]
                    prev_ci = [32, None]
                    for d in range(2):
                        S.op("dve", lambda e, d=d: e.tensor_copy(out=Sfb[d][0][:], in_=Sst[d][:, h, :]), reads=[Sst[d]], writes=[Sfb[d][0]])

                    def o_store(t, oTp, first):
                        tok = slice(t * 128, (t + 1) * 128)
                        if first:
                            S.op("act", lambda e: e.activation(out=oacc[:, tok], in_=oTp[:, 0:128], func=AF.Copy), reads=[oTp], writes=[oacc])
                        else:
                            S.op("dve", lambda e: e.tensor_tensor(out=oacc[:, tok], in0=oTp[:, 0:128], in1=oacc[:, tok], op=ALU.add),
                                 reads=[oTp, oacc], writes=[oacc])

                    def step_pre(d, t):
                        tok = slice(t * 128, (t + 1) * 128)
                        order = (0, 1) if d == 0 else (1, 0)
                        mask = maskA if d == 0 else maskB
                        ATp = hps.next()
                        mm(ATp, ATp[:, 0:128], [(kh[d][:, tok], qh[d][:, tok])], [kh[d], qh[d]])
                        Pbs = {}
                        for c in order:
                            ck = slice(c * 64, (c + 1) * 64)
                            Pb = hps.next()
                            Pbs[c] = Pb
                            S.op("pe", lambda e, c=c, ck=ck, Pb=Pb: e.matmul(Pb[:, 0:128], lhsT=ktok[d][ck, t, :], rhs=v_own[ck, t, hc],
                                                                             start=True, stop=True),
                                 reads=[ktok[d], v_own], writes=[Pb])
                        at = ATs.next()
                        S.op("dve", lambda e: e.tensor_tensor(out=at[:], in0=ATp[:, 0:128], in1=mask[:], op=ALU.mult),
                             reads=[ATp, mask], writes=[at])
                        oTp = ATp
                        S.op("pe", lambda e: e.matmul(oTp[:, 0:128], lhsT=v_own[:, t, hc], rhs=at[:], start=True, stop=False),
                             reads=[v_own, at], writes=[oTp], inc=False)
                        return (d, t, order, Pbs, oTp)

                    def step_chunk(ctx_, k):
                        d, t, order, Pbs, oTp = ctx_
                        c = order[k]
                        ck = slice(c * 64, (c + 1) * 64)
                        toks = slice(t * 128 + c * 64, t * 128 + (c + 1) * 64)
                        ci = t * 2 + c
                        ei = prev_ci[0] if d == 0 else ci
                        qo, qn = Sfb[d][qi[d]], Sfb[d][1 - qi[d]]
                        qi[d] = 1 - qi[d]
                        nb = Sbf[d].next()
                        S.op("act", lambda e: e.activation(out=nb[:], in_=qo[:], func=AF.Identity, scale=expT[d][:, ei:ei + 1]),
                             reads=[qo, expT[d]], writes=[nb])
                        S.op("pe", lambda e: e.matmul(oTp[:, ck], lhsT=nb[:], rhs=qh[d][:, toks], start=False, stop=(k == 1)),
                             reads=[nb, qh[d]], writes=[oTp], inc=(k == 1))
                        S.op("dve", lambda e: e.scalar_tensor_tensor(out=qn[:], in0=qo[:], scalar=expT[d][:, ei:ei + 1], in1=Pbs[c][:, 0:128],
                                                                      op0=ALU.mult, op1=ALU.add), reads=[qo, expT[d], Pbs[c]], writes=[qn])
                        if d == 0:
                            prev_ci[0] = ci

                    for i in range(16):
                        ca = step_pre(0, i)
                        cb = step_pre(1, 15 - i)
                        for k in range(2):
                            step_chunk(ca, k)
                            step_chunk(cb, k)
                        o_store(i, ca[4], first=(i <= 7))
                        o_store(15 - i, cb[4], first=(i <= 7))
                    for g in range(4):
                        cs = slice(g * 512, (g + 1) * 512)
                        S.op("act", lambda e, cs=cs: e.activation(out=og_sq[:], in_=oacc[:, cs], func=AF.Square), reads=[oacc], writes=[og_sq])
                        ssp = hps.next()
                        mm(ssp, ssp[:], [(ones_bf[:], og_sq[:])], [ones_bf, og_sq])
                        S.op("act", lambda e, ssp=ssp: e.activation(out=og_sd[:], in_=ssp[:], func=AF.Sqrt, scale=1.0 / 128, bias=EPS),
                             reads=[ssp], writes=[og_sd])
                        S.op("dve", lambda e: e.reciprocal(out=og_sd[:], in_=og_sd[:]), reads=[og_sd], writes=[og_sd])
                        S.op("dve", lambda e, cs=cs: e.scalar_tensor_tensor(out=og_t1[:], in0=oacc[:, cs], scalar=gout_t[:, 0:1], in1=og_sd[:],
                                                                             op0=ALU.mult, op1=ALU.mult),
                             reads=[oacc, gout_t, og_sd], writes=[og_t1])
                        S.op("dve", lambda e, cs=cs: e.tensor_tensor(out=aT[:, h, cs], in0=og_t1[:], in1=gT[:, cs], op=ALU.mult),
                             reads=[og_t1, gT], writes=[aT])
                if stage == 3:
                    for h in range(4):
                        dump(aT, aT[:, h, 0:1024], 1024)
                S.barrier()
            sw.close()
            if stage == 3:
                S.barrier()
                return nc, S

            with ExitStack() as scm:
                bmT = sb(scm, "bmT", [128, 4, NT], BF16)
                modB = sb(scm, "modB", [128, 4 * D], F32)
                with ExitStack() as sc:
                    wu = sb(sc, "wu", [128, 8, 512], BF16)
                    wcv = sb(sc, "wcv", [128, 8, 512], BF16)
                    lng_bc = sb(sc, "lng_bc", [128, 512], F32)
                    lnb_bc = sb(sc, "lnb_bc", [128, 512], F32)
                    bs_bc = sb(sc, "bs_bc", [128, 512], F32)
                    wsT_bf = sb(sc, "wsT_bf", [128, 512], BF16)
                    S.dma("pool", wu[:], w_in_v[:, :, 2560:3072], writes=[wu])
                    S.dma("pool", wcv[:], w_in_v[:, :, 3072:3584], writes=[wcv])
                    S.dma("pool", wsT_bf[:], wsT[:, :], writes=[wsT_bf])
                    S.dma("sp", lng_bc[:], lng[0:1, :].partition_broadcast(128), writes=[lng_bc])
                    S.dma("sp", lnb_bc[:], lnb[0:1, :].partition_broadcast(128), writes=[lnb_bc])
                    S.dma("sp", bs_bc[:], bsp[0:1, :].partition_broadcast(128), writes=[bs_bc])
                    uTs = Ring([sb(sc, "uT%d" % i, [128, 4, 512], BF16) for i in range(2)])
                    ges = Ring([sb(sc, "ge%d" % i, [128, 512], F32) for i in range(2)])
                    xns = Ring([sb(sc, "xn%d" % i, [128, 512], F32) for i in range(2)])
                    vlns = Ring([sb(sc, "vln%d" % i, [128, 512], BF16) for i in range(2)])
                    t1s = Ring([sb(sc, "ct1%d" % i, [128, 512], F32) for i in range(2)])
                    csts = Ring([sb(sc, "cst%d" % i, [128, 12], F32) for i in range(4)])
                    cps = Ring([psb(sc, "cps%d" % i, [128, 512], F32) for i in range(6)])
                    wada2 = [sb(sc, "wadb%d" % i, [128, 8, 512], BF16) for i in range(2)]
                    bada2 = [sb(sc, "badb%d" % i, [128, 512], F32) for i in range(2)]
                    gn2 = sb(sc, "gn2", [128, D], F32)

                    def p1_late(j):
                        wb_, bb_ = wada2[j % 2], bada2[j % 2]
                        S.dma("pool", wb_[:], w_ada_v[:, :, j * 512:(j + 1) * 512], writes=[wb_])
                        S.dma("sp", bb_[:], b_ada[0:1, j * 512:(j + 1) * 512].partition_broadcast(128), writes=[bb_])
                        ps = cps.next()
                        mm(ps, ps[:], [(screp[:, kc, :], wb_[:, kc, :]) for kc in range(8)], [screp, wb_])
                        S.op("dve", lambda e: e.tensor_tensor(out=modB[:, (j - 4) * 512:(j - 3) * 512], in0=ps[:], in1=bb_[:], op=ALU.add),
                             reads=[ps, bb_], writes=[modB])

                    def late_scale(lo, g0, plus1):
                        S.dma("sp", gn2[:], gains[0:1, g0 * D:(g0 + 1) * D].partition_broadcast(128), writes=[gn2])
                        if plus1:
                            S.op("dve", lambda e: e.scalar_tensor_tensor(out=modB[:, lo:lo + D], in0=modB[:, lo:lo + D], scalar=1.0, in1=gn2[:],
                                                                          op0=ALU.add, op1=ALU.mult), reads=[modB, gn2], writes=[modB])
                        else:
                            S.op("dve", lambda e: e.tensor_tensor(out=modB[:, lo:lo + D], in0=modB[:, lo:lo + D], in1=gn2[:], op=ALU.mult),
                                 reads=[modB, gn2], writes=[modB])

                    for tg in range(4):
                        p1_late(4 + 2 * tg); p1_late(5 + 2 * tg)
                        cs = slice(tg * 512, (tg + 1) * 512)
                        uTg = uTs.next()
                        for g in range(4):
                            ps = cps.next()
                            mm(ps, ps[:], [(wu[:, kc, g * 128:(g + 1) * 128], hT_own[:, kc, cs]) for kc in range(8)], [wu, hT_own])
                            S.op("act", lambda e, ps=ps, g=g, uTg=uTg: e.activation(out=uTg[:, g, :], in_=ps[:], func=AF.Gelu_apprx_tanh),
                                 reads=[ps], writes=[uTg])
                        def cm_a(t):
                            tile_ = tg * 4 + t
                            tok = slice(tile_ * 128, (tile_ + 1) * 128)
                            ps = cps.next()
                            mm(ps, ps[:], [(hT_own[:, kc, tok], wcv[:, kc, :]) for kc in range(8)], [hT_own, wcv])
                            ge = ges.next(); xn = xns.next(); vln = vlns.next(); st = csts.next()
                            S.op("act", lambda e: e.activation(out=ge[:], in_=ps[:], func=AF.Gelu_apprx_tanh), reads=[ps], writes=[ge])
                            S.op("dve", lambda e: e.bn_stats(out=st[:, 0:6], in_=ge[:]), reads=[ge], writes=[st])
                            S.op("dve", lambda e: e.bn_aggr(out=st[:, 6:8], in_=st[:, 0:6]), reads=[st], writes=[st])
                            S.op("act", lambda e: e.activation(out=st[:, 8:9], in_=st[:, 7:8], func=AF.Sqrt, bias=EPS), reads=[st], writes=[st])
                            S.op("dve", lambda e: e.reciprocal(out=st[:, 9:10], in_=st[:, 8:9]), reads=[st], writes=[st])
                            S.op("dve", lambda e: e.tensor_scalar(out=xn[:], in0=ge[:], scalar1=st[:, 6:7], scalar2=st[:, 9:10],
                                                                   op0=ALU.subtract, op1=ALU.mult), reads=[ge, st], writes=[xn])
                            S.op("dve", lambda e: e.tensor_tensor(out=xn[:], in0=xn[:], in1=lng_bc[:], op=ALU.mult), reads=[xn, lng_bc], writes=[xn])
                            S.op("dve", lambda e: e.tensor_tensor(out=vln[:], in0=xn[:], in1=lnb_bc[:], op=ALU.add), reads=[xn, lnb_bc], writes=[vln])
                            return (t, tok, vln)

                        def cm_b(t, tok, vln):
                            zps = cps.next()
                            for g in range(4):
                                S.op("pe", lambda e, g=g: e.matmul(zps[:, g * 128:(g + 1) * 128], lhsT=vln[:, g * 128:(g + 1) * 128],
                                                                   rhs=wsT_bf[:, g * 128:(g + 1) * 128], start=True, stop=True, skip_group_check=True),
                                     reads=[vln, wsT_bf], writes=[zps], inc=(g == 3))
                            t1 = t1s.next()
                            S.op("dve", lambda e: e.tensor_tensor(out=t1[:], in0=zps[:], in1=bs_bc[:], op=ALU.add), reads=[zps, bs_bc], writes=[t1])
                            S.op("dve", lambda e: e.tensor_tensor(out=bmT[:, :, tok], in0=t1[:].rearrange("p (a b) -> p a b", a=4),
                                                                  in1=uTg[:, :, t * 128:(t + 1) * 128], op=ALU.mult), reads=[t1, uTg], writes=[bmT])

                        prev = None
                        for t in range(4):
                            cur_ = cm_a(t)
                            if prev is not None:
                                cm_b(*prev)
                            prev = cur_
                        cm_b(*prev)
                    late_scale(0, 1, False); late_scale(2 * D, 2, True); late_scale(3 * D, 3, False)
                    S.dma("sp", g2s[:, :], modB[:, 3 * D:4 * D], reads=[modB], writes=[g2s_b])
                    if stage == 4:
                        for g in range(4):
                            dump(bmT, bmT[:, g, 0:1024], 1024)
                    S.barrier()
                if stage == 4:
                    S.barrier()
                    return nc, S

                with ExitStack() as sg:
                    yT = sb(sg, "yT", [128, 8, NT], BF16)
                    wo = sb(sg, "wo", [128, 8, D], BF16)
                    S.dma("pool", wo[:], w_o.rearrange("(kc p) n -> p kc n", p=128), writes=[wo])
                    sgc = ExitStack()
                    wgas = Ring([sb(sgc, "wga%d" % i, [128, 8, 128], BF16) for i in range(2)])
                    wgbs = Ring([sb(sgc, "wgb%d" % i, [128, 8, 128], BF16) for i in range(2)])
                    was = Ring([sb(sgc, "wa%d" % i, [128, 4, 128], BF16) for i in range(2)])
                    wbs = Ring([sb(sgc, "wb%d" % i, [128, 4, 128], BF16) for i in range(2)])
                    sgs = Ring([sb(sgc, "sgm%d" % i, [128, 512], F32) for i in range(2)])
                    y1s = Ring([sb(sgc, "y1%d" % i, [128, 512], F32) for i in range(2)])
                    y2s = Ring([sb(sgc, "y2%d" % i, [128, 512], F32) for i in range(2)])
                    gps = Ring([psb(sgc, "gps%d" % i, [128, 512], F32) for i in range(8)])
                    w_a_v = w_a.rearrange("(kc p) n -> p kc n", p=128)
                    w_b_v = w_b.rearrange("(kc p) n -> p kc n", p=128)
                    for c in range(8):
                        wga = wgas.next(); wgb = wgbs.next(); wa_ = was.next(); wb_ = wbs.next()
                        S.dma("pool", wga[:], w_in_v[:, :, 3584 + c * 128:3584 + (c + 1) * 128], writes=[wga])
                        S.dma("pool", wgb[:], w_in_v[:, :, 4608 + c * 128:4608 + (c + 1) * 128], writes=[wgb])
                        S.dma("pool", wa_[:], w_a_v[:, :, c * 128:(c + 1) * 128], writes=[wa_])
                        S.dma("pool", wb_[:], w_b_v[:, :, c * 128:(c + 1) * 128], writes=[wb_])
                        for tg in range(4):
                            cs = slice(tg * 512, (tg + 1) * 512)
                            pa = gps.next()
                            mm(pa, pa[:], [(wa_[:, kc, :], aT[:, kc, cs]) for kc in range(4)], [wa_, aT])
                            pg = gps.next()
                            mm(pg, pg[:], [(wga[:, kc, :], hT_own[:, kc, cs]) for kc in range(8)], [wga, hT_own])
                            sga = sgs.next(); y1 = y1s.next()
                            S.op("act", lambda e, pg=pg, sga=sga: e.activation(out=sga[:], in_=pg[:], func=AF.Sigmoid), reads=[pg], writes=[sga])
                            S.op("dve", lambda e, sga=sga, pa=pa, y1=y1: e.tensor_tensor(out=y1[:], in0=pa[:], in1=sga[:], op=ALU.mult),
                                 reads=[pa, sga], writes=[y1])
                            pb = gps.next()
                            mm(pb, pb[:], [(wb_[:, kc, :], bmT[:, kc, cs]) for kc in range(4)], [wb_, bmT])
                            pg2 = gps.next()
                            mm(pg2, pg2[:], [(wgb[:, kc, :], hT_own[:, kc, cs]) for kc in range(8)], [wgb, hT_own])
                            sgb = sgs.next(); y2 = y2s.next()
                            S.op("act", lambda e, pg2=pg2, sgb=sgb: e.activation(out=sgb[:], in_=pg2[:], func=AF.Sigmoid), reads=[pg2], writes=[sgb])
                            S.op("dve", lambda e, sgb=sgb, pb=pb, y2=y2: e.tensor_tensor(out=y2[:], in0=pb[:], in1=sgb[:], op=ALU.mult),
                                 reads=[pb, sgb], writes=[y2])
                            S.op("dve", lambda e, y1=y1, y2=y2, c=c, cs=cs: e.tensor_tensor(out=yT[:, c, cs], in0=y1[:], in1=y2[:], op=ALU.add),
                                 reads=[y1, y2], writes=[yT])
                    if stage == 5:
                        for c in range(4):
                            dump(yT, yT[:, c, 0:1024], 1024)
                    S.barrier()
                    sgc.close()
                    xts = Ring([sb(sg, "mxt%d" % i, [128, D], F32) for i in range(2)])
                    x1t = Ring([sb(sg, "x1t%d" % i, [128, D], F32) for i in range(2)])
                    hns = Ring([sb(sg, "fhn%d" % i, [128, D], F32) for i in range(1)])
                    h2s = Ring([sb(sg, "fh2%d" % i, [128, D], F32) for i in range(2)])
                    h2Tf = Ring([sb(sg, "h2Tf%d" % i, [128, 8, 128], F32) for i in range(2)])
                    h2Tt = Ring([sb(sg, "h2Tt%d" % i, [128, 8, 128], BF16) for i in range(2)])
                    mjunk = sb(sg, "mjunk", [128, D], BF16)
                    msts = Ring([sb(sg, "mst%d" % i, [128, 8], F32) for i in range(4)])
                    wrt = sb(sg, "wrt", [128, 8, NE], F32)
                    brt_bc = sb(sg, "brt_bc", [128, NE], F32)
                    S.dma("sp", wrt[:], w_rt.rearrange("(kc p) n -> p kc n", p=128), writes=[wrt])
                    S.dma("sp", brt_bc[:], b_rt[0:1, :].partition_broadcast(128), writes=[brt_bc])
                    rt = {n: Ring([sb(sg, "rt_%s%d" % (n, i), [128, NE], F32) for i in range(3)]) for n in ("sc", "sel", "selm", "em", "w")}
                    m8g = Ring([sb(sg, "m8g%d" % i, [128, 8, 8], F32) for i in range(2)])
                    rsm = Ring([sb(sg, "rsm%d" % i, [128, 48], F32) for i in range(2)])
                    gps2 = Ring([psb(sg, "gps2_%d" % i, [128, 2 * 512], F32) for i in range(3)])
                    lpsr = Ring([psb(sg, "lps%d" % i, [128, 512], F32) for i in range(2)])

                    def ffn_a(t):
                        tok = slice(t * 128, (t + 1) * 128)
                        xt = xts.next(); x1 = x1t.next(); st = msts.next(); hn = hns.next(); h2 = h2s.next(); hf = h2Tf.next()
                        S.dma("sp", xt[:], x_own[tok, :], writes=[xt])
                        pz = gps2.next()
                        for n in range(2):
                            mm(pz, pz[:, n * 512:(n + 1) * 512], [(yT[:, kc, tok], wo[:, kc, n * 512:(n + 1) * 512]) for kc in range(8)], [yT, wo])
                        S.op("act", lambda e: e.activation(out=mjunk[:], in_=pz[:], func=AF.Square, accum_out=st[:, 0:1]), reads=[pz], writes=[mjunk, st])
                        S.op("act", lambda e: e.activation(out=st[:, 1:2], in_=st[:, 0:1], func=AF.Sqrt, scale=1.0 / D, bias=EPS), reads=[st], writes=[st])
                        S.op("dve", lambda e: e.reciprocal(out=st[:, 2:3], in_=st[:, 1:2]), reads=[st], writes=[st])
                        S.op("dve", lambda e: e.scalar_tensor_tensor(out=x1[:], in0=pz[:], scalar=st[:, 2:3], in1=modB[:, 0:D],
                                                                      op0=ALU.mult, op1=ALU.mult), reads=[pz, st, modB], writes=[x1])
                        S.op("dve", lambda e: e.tensor_tensor(out=x1[:], in0=x1[:], in1=xt[:], op=ALU.add), reads=[x1, xt], writes=[x1])
                        S.dma("sp", x1s[tok, :], x1[:], reads=[x1], writes=[x1s_bs[t % 4]])
                        if stage == 6 and t < 4:
                            dump(x1, x1[:, :], 1024)
                        S.op("act", lambda e: e.activation(out=mjunk[:], in_=x1[:], func=AF.Square, accum_out=st[:, 3:4]), reads=[x1], writes=[mjunk, st])
                        S.op("act", lambda e: e.activation(out=st[:, 4:5], in_=st[:, 3:4], func=AF.Sqrt, scale=1.0 / D, bias=EPS), reads=[st], writes=[st])
                        S.op("dve", lambda e: e.reciprocal(out=st[:, 5:6], in_=st[:, 4:5]), reads=[st], writes=[st])
                        S.op("dve", lambda e: e.scalar_tensor_tensor(out=hn[:], in0=x1[:], scalar=st[:, 5:6], in1=modB[:, 2 * D:3 * D],
                                                                      op0=ALU.mult, op1=ALU.mult), reads=[x1, st, modB], writes=[hn])
                        S.op("dve", lambda e: e.tensor_tensor(out=h2[:], in0=hn[:], in1=modB[:, D:2 * D], op=ALU.add), reads=[hn, modB], writes=[h2])
                        return (t, tok, h2, hf)

                    def ffn_a2(t, tok, h2, hf):
                        tp = gps2.next()
                        for kc in range(8):
                            S.op("pe", lambda e, kc=kc: e.transpose(tp[:, kc * 128:(kc + 1) * 128], h2[:, kc * 128:(kc + 1) * 128], ident_f[:]),
                                 reads=[h2, ident_f], writes=[tp], inc=(kc == 7))
                        hb16 = h2Tt.next()
                        S.op("act", lambda e: e.activation(out=hb16[:], in_=tp[:].rearrange("p (a b) -> p a b", a=8), func=AF.Copy),
                             reads=[tp], writes=[hb16])
                        S.dma("sp", h2Td[:, :, tok], hb16[:], reads=[hb16], writes=[h2Td_b])
                        S.op("act", lambda e: e.activation(out=hf[:], in_=tp[:].rearrange("p (a b) -> p a b", a=8), func=AF.Copy),
                             reads=[tp], writes=[hf])
                        lps = lpsr.next()
                        mm(lps, lps[:, 0:NE], [(hf[:, kc, :], wrt[:, kc, :]) for kc in range(8)], [hf, wrt])
                        sc_ = rt["sc"].next()
                        S.op("act", lambda e: e.activation(out=sc_[:], in_=lps[:, 0:NE], func=AF.Sigmoid), reads=[lps], writes=[sc_])
                        return sc_

                    def ffn_b(t, sc_):
                        sel = rt["sel"].next(); selm = rt["selm"].next(); em = rt["em"].next(); w_ = rt["w"].next()
                        mg = m8g.next(); sm_ = rsm.next()
                        S.op("dve", lambda e: e.tensor_tensor(out=sel[:], in0=sc_[:], in1=brt_bc[:], op=ALU.add), reads=[sc_, brt_bc], writes=[sel])
                        for g in range(8):
                            S.op("dve", lambda e, g=g: e.max(out=mg[:, g, :], in_=sel[:, g * 8:(g + 1) * 8]), reads=[sel], writes=[mg])
                        S.op("dve", lambda e: e.tensor_tensor(out=sm_[:, 0:8], in0=mg[:, :, 0], in1=mg[:, :, 1], op=ALU.add), reads=[mg], writes=[sm_])
                        S.op("dve", lambda e: e.max(out=sm_[:, 8:16], in_=sm_[:, 0:8]), reads=[sm_], writes=[sm_])
                        S.op("dve", lambda e: e.tensor_scalar(out=sm_[:, 16:24], in0=sm_[:, 0:8], scalar1=sm_[:, 11:12], scalar2=None, op0=ALU.is_ge),
                             reads=[sm_], writes=[sm_])
                        S.op("dve", lambda e: e.tensor_scalar(out=sm_[:, 24:32], in0=sm_[:, 16:24], scalar1=4.0, scalar2=-4.0, op0=ALU.mult, op1=ALU.add),
                             reads=[sm_], writes=[sm_])
                        S.op("dve", lambda e: e.tensor_tensor(out=selm[:].rearrange("p (a b) -> p a b", a=8), in0=sel[:].rearrange("p (a b) -> p a b", a=8),
                                                              in1=sm_[:, 16:24].unsqueeze(2).to_broadcast([128, 8, 8]), op=ALU.mult),
                             reads=[sel, sm_], writes=[selm])
                        S.op("dve", lambda e: e.tensor_tensor(out=selm[:].rearrange("p (a b) -> p a b", a=8), in0=selm[:].rearrange("p (a b) -> p a b", a=8),
                                                              in1=sm_[:, 24:32].unsqueeze(2).to_broadcast([128, 8, 8]), op=ALU.add),
                             reads=[selm, sm_], writes=[selm])
                        S.op("dve", lambda e: e.max(out=sm_[:, 32:40], in_=selm[:]), reads=[selm], writes=[sm_])
                        S.op("dve", lambda e: e.tensor_scalar(out=em[:], in0=selm[:], scalar1=sm_[:, 39:40], scalar2=None, op0=ALU.is_ge),
                             reads=[selm, sm_], writes=[em])
                        S.op("dve", lambda e: e.tensor_tensor(out=w_[:], in0=sc_[:], in1=em[:], op=ALU.mult), reads=[sc_, em], writes=[w_])
                        S.op("dve", lambda e: e.tensor_tensor_scan(out=em[:], data0=ones_f[:, 0:NE], data1=w_[:], initial=0.0, op0=ALU.mult, op1=ALU.add),
                             reads=[ones_f, w_], writes=[em])
                        S.op("dve", lambda e: e.tensor_scalar(out=sm_[:, 40:41], in0=em[:, NE - 1:NE], scalar1=0.4, scalar2=None, op0=ALU.mult),
                             reads=[em], writes=[sm_])
                        S.op("dve", lambda e: e.reciprocal(out=sm_[:, 41:42], in_=sm_[:, 40:41]), reads=[sm_], writes=[sm_])
                        S.op("dve", lambda e: e.tensor_scalar(out=wt_all[:, t, 0:NE], in0=w_[:], scalar1=sm_[:, 41:42], scalar2=None, op0=ALU.mult),
                             reads=[w_, sm_], writes=[wt_all])

                    pa = ffn_a(0)
                    prev_b = None
                    for t in range(16):
                        nxt = ffn_a(t + 1) if t + 1 < 16 else None
                        sc_t = ffn_a2(*pa)
                        if prev_b is not None:
                            ffn_b(*prev_b)
                        prev_b = (t, sc_t)
                        pa = nxt
                    ffn_b(*prev_b)
                    if stage == 7:
                        dump(wt_all, wt_all[:].rearrange("p a b -> p (a b)"), 16 * (NE + 1))
                    S.barrier()
        if stage in (5, 6):
            S.barrier()
            return nc, S

        with ExitStack() as se:
            h2T = sb(se, "h2T", [128, 8, NT], BF16)
            acc = sb(se, "acc", [128, 16, D], F32)
            acc_b = [Buf("acc%d" % t) for t in range(16)]
            S.dma("sp", h2T[:], h2Td[:, :, :], reads=[h2Td_b], writes=[h2T])
            for t in range(16):
                S.op("pool", lambda e, t=t: e.memset(acc[:, t, :], 0.0), writes=[acc_b[t]])
            eps_ = Ring([psb(se, "eps%d" % i, [128, 512], F32) for i in range(8)])
            if stage == 7:
                for kc in range(2):
                    dump(h2T, h2T[:, kc, 0:1024], 1024)
                S.barrier()
                return nc, S

            with ExitStack() as sx:
                wgus = Ring([sb(sx, "wgu%d" % i, [128, 8, 512], BF16) for i in range(3)])
                wdns = Ring([sb(sx, "wdn%d" % i, [128, 2, D], BF16) for i in range(3)])
                actTs = Ring([sb(sx, "actT%d" % i, [128, 2, 512], BF16) for i in range(3)])
                sgts = Ring([sb(sx, "sgt%d" % i, [128, 512], F32) for i in range(2)])
                xts_f = Ring([sb(sx, "oxt%d" % i, [128, D], F32) for i in range(2)])
                ots_f = Ring([sb(sx, "ot%d" % i, [128, D], F32) for i in range(2)])
                ojunk = sb(sx, "ojunk", [128, D], BF16)
                ost = Ring([sb(sx, "ost%d" % i, [128, 4], F32) for i in range(4)])
                g2t = sb(sx, "g2t", [128, D], F32)
                S.dma("sp", g2t[:], g2s[:, :], reads=[g2s_b], writes=[g2t])

                def final_tile(t):
                    tok = slice(t * 128, (t + 1) * 128)
                    xt = xts_f.next(); ot = ots_f.next(); st = ost.next()
                    S.dma("sp", xt[:], x1s[tok, :], reads=[x1s_bs[t % 4]], writes=[xt])
                    S.op("act", lambda e: e.activation(out=ojunk[:], in_=acc[:, t, :], func=AF.Square, accum_out=st[:, 0:1]),
                         reads=[acc_b[t]], writes=[ojunk, st])
                    S.op("act", lambda e: e.activation(out=st[:, 1:2], in_=st[:, 0:1], func=AF.Sqrt, scale=1.0 / D, bias=EPS), reads=[st], writes=[st])
                    S.op("dve", lambda e: e.reciprocal(out=st[:, 2:3], in_=st[:, 1:2]), reads=[st], writes=[st])
                    S.op("dve", lambda e: e.scalar_tensor_tensor(out=ot[:], in0=acc[:, t, :], scalar=st[:, 2:3], in1=g2t[:],
                                                                  op0=ALU.mult, op1=ALU.mult), reads=[acc_b[t], st, g2t], writes=[ot])
                    S.op("dve", lambda e: e.tensor_tensor(out=ot[:], in0=ot[:], in1=xt[:], op=ALU.add), reads=[ot, xt], writes=[ot])
                    S.dma("sp", out[tok, :], ot[:], reads=[ot], writes=[out_bs[t % 4]])

                n_exp = NE + 1 if stage >= 9 else (2 if stage == 8 else NE + 1)
                wbuf = {}

                def moe_load(ex):
                    wg = wgus.next(); wd = wdns.next()
                    S.dma("pool", wg[:], w_gu[ex].rearrange("(kc p) n -> p kc n", p=128), writes=[wg])
                    S.dma("pool", wd[:], w_dn[ex].rearrange("(kc p) n -> p kc n", p=128), writes=[wd])
                    wbuf[ex] = (wg, wd)

                def moe_s1(ex, tg):
                    wg = wbuf[ex][0]
                    cs = slice(tg * 512, (tg + 1) * 512)
                    G = []
                    for c in range(4):
                        ps = eps_.next()
                        mm(ps, ps[:], [(wg[:, kc, c * 128:(c + 1) * 128], h2T[:, kc, cs]) for kc in range(8)], [wg, h2T])
                        G.append(ps)
                    aT_ = actTs.next()
                    for j in range(2):
                        sgt = sgts.next()
                        S.op("act", lambda e, sgt=sgt, g_=G[j]: e.activation(out=sgt[:], in_=g_[:], func=AF.Silu), reads=[G[j]], writes=[sgt])
                        S.op("dve", lambda e, sgt=sgt, j=j, g_=G[2 + j]: e.tensor_tensor(out=aT_[:, j, :], in0=g_[:], in1=sgt[:], op=ALU.mult),
                             reads=[G[2 + j], sgt], writes=[aT_])
                    return aT_

                def moe_s2(ex, tg, aT_):
                    wd = wbuf[ex][1]
                    for t in range(4):
                        tile_ = tg * 4 + t
                        for n in range(2):
                            dps = eps_.next()
                            mm(dps, dps[:], [(aT_[:, j, t * 128:(t + 1) * 128], wd[:, j, n * 512:(n + 1) * 512]) for j in range(2)], [aT_, wd])
                            S.op("dve", lambda e, dps=dps, tile_=tile_, n=n: e.scalar_tensor_tensor(
                                out=acc[:, tile_, n * 512:(n + 1) * 512], in0=dps[:], scalar=wt_all[:, tile_, ex:ex + 1],
                                in1=acc[:, tile_, n * 512:(n + 1) * 512], op0=ALU.mult, op1=ALU.add),
                                reads=[dps, wt_all, acc_b[tile_]], writes=[acc_b[tile_]])

                its = [(ex, tg) for ex in range(n_exp) for tg in range(4)]
                moe_load(0)
                if n_exp > 1:
                    moe_load(1)
                pend = None
                for i, (ex, tg) in enumerate(its):
                    if tg == 0 and ex >= 1 and ex + 1 < n_exp:
                        moe_load(ex + 1)
                    aT_ = moe_s1(ex, tg)
                    if pend is not None:
                        moe_s2(*pend)
                        if pend[0] == n_exp - 1:
                            for t_ in range(4):
                                final_tile(pend[1] * 4 + t_)
                    pend = (ex, tg, aT_)
                moe_s2(*pend)
                for t_ in range(4):
                    final_tile(pend[1] * 4 + t_)
                S.barrier()

        S.barrier()
    return nc, S


def _consts():
    s = np.arange(128)[:, None]; t = np.arange(128)[None, :]
    same = (s // 64) == (t // 64)
    maskA = (same & (s <= t)).astype(np.float32)
    maskB = (same & (s >= t)).astype(np.float32)
    resetm = np.ones((128, 512), np.float32); resetm[:, ::64] = 0.0
    return np.eye(128, dtype=np.float32), maskA, maskB, resetm


def make_in_maps(inp):
    f = lambda a: np.ascontiguousarray(np.asarray(a, dtype=np.float32))
    x = f(inp["x"]); ctx = f(inp["ctx"]); c = f(inp["c"]); cc = f(inp["c_ctx"])
    w_in = f(inp["w_in"][0])
    w_in_sw = w_in.copy(); w_in_sw[:, 512:1024] = w_in[:, 1024:1536]; w_in_sw[:, 1024:1536] = w_in[:, 512:1024]
    lbl = f(inp["lb_logits"])
    ws = f(inp["w_spatial"][0]); bs = f(inp["b_spatial"][0])
    gains = np.concatenate([f(inp[k][0]) for k in ("g_pre_mix", "g_post_mix", "g_pre_ffn", "g_post_ffn")])[None, :]
    w_gu = np.concatenate([f(inp["w_expert_gu"][0]), f(inp["w_shared_gu"])], axis=0)
    w_dn = np.concatenate([f(inp["w_expert_down"][0]), f(inp["w_shared_down"])], axis=0)
    ident, maskA, maskB, resetm = _consts()
    shared = dict(w_ada=f(inp["w_ada"][0]), b_ada=f(inp["b_ada"]), gains=f(gains), gout=f(inp["g_hgrn_out"][0][:, None]),
                  lng=f(inp["cm_ln_g"]), lnb=f(inp["cm_ln_b"]), w_a=f(inp["w_branch_a"][0]), w_b=f(inp["w_branch_b"][0]),
                  w_o=f(inp["w_out"][0]), w_rt=f(inp["w_router"][0]), b_rt=f(inp["b_router"]), w_gu=w_gu, w_dn=w_dn,
                  ident=ident, maskA=maskA, maskB=maskB, resetm=resetm)
    maps = []
    for i in range(8):
        b, half = i // 2, i % 2
        m = dict(shared)
        if half == 0:
            xs = x[b]; m["ctx"] = f(ctx[b]); m["w_in"] = w_in; dirs = [0, 1]; ws_l, bs_l = ws, bs
        else:
            xs = x[b, ::-1]; m["ctx"] = f(ctx[b, ::-1]); m["w_in"] = w_in_sw; dirs = [1, 0]
            ws_l, bs_l = ws[:, ::-1, ::-1], bs[::-1, :]
        m["x_own"] = f(xs[0:NT]); m["x_oth"] = f(xs[NT:2 * NT])
        m["cvec"] = f(np.concatenate([c[b].reshape(8, 128).T, cc.reshape(8, 128).T], axis=1))
        l4 = lbl[dirs].reshape(2, 2, 4, 128)
        m["lbl"] = f(l4.transpose(3, 0, 1, 2).reshape(128, 16))
        m["wsT"] = f(ws_l.transpose(2, 0, 1).reshape(128, 512))
        m["bsp"] = f(bs_l.T.reshape(1, 512))
        maps.append(m)
    return maps


_CACHE = {}


def kernel(**inputs):
    if "nc" not in _CACHE:
        _CACHE["nc"] = build_program()[0]
    nc = _CACHE["nc"]
    maps = make_in_maps(inputs)
    res = run_bass_kernel_spmd(nc, maps, core_ids=list(range(8)))
    B, T = inputs["x"].shape[0], inputs["x"].shape[1]
    out = np.empty((B, T, D), np.float32)
    for i in range(8):
        b, half = i // 2, i % 2
        r = np.asarray(res.results[i]["out"], dtype=np.float32)
        if half == 0:
            out[b, 0:NT] = r
        else:
            out[b, NT:2 * NT] = r[::-1]
    return out
```

```python
import os
import numpy as np
from contextlib import ExitStack
import concourse.bass as bass
import concourse.mybir as mybir
from concourse.bass_utils import run_bass_kernel_spmd

F32 = mybir.dt.float32
BF16 = mybir.dt.bfloat16
AF = mybir.ActivationFunctionType
ALU = mybir.AluOpType

NT = 2048
D = 1024
EPS = 1e-6
NE = 64


class Tok:
    __slots__ = ("sem", "val", "eng")

    def __init__(self, sem, val, eng):
        self.sem, self.val, self.eng = sem, val, eng


class Buf:
    def __init__(self, name):
        self.name = name
        self.w = None
        self.r = []
        self.dsem = None
        self.dcnt = 0
        self.excl = False


class TT:
    def __init__(self, t, name):
        self.t = t
        self.b = Buf(name)

    def __getitem__(self, k):
        return self.t[k]


class Sched:
    def __init__(self, nc, es):
        self.nc, self.es = nc, es
        self.eng = {"pe": nc.tensor, "act": nc.scalar, "dve": nc.vector, "pool": nc.gpsimd, "sp": nc.sync}
        self.esem, self.ecnt = {}, {}
        for e in ("pe", "act", "dve", "pool"):
            self.esem[e] = es.enter_context(nc.semaphore("sem_" + e))
            self.ecnt[e] = 0
        self.seen = {e: {} for e in self.eng}
        self.dsems = []
        self.nwaits = 0
        self.ninstr = {e: 0 for e in self.eng}

    def _wait(self, e, tok):
        key = id(tok.sem)
        if self.seen[e].get(key, 0) >= tok.val:
            return
        if tok.eng != "dma":
            assert tok.val <= self.ecnt[tok.eng], "wait on future inc (%s on %s)" % (e, tok.eng)
        self.eng[e].wait_ge(tok.sem, tok.val)
        self.seen[e][key] = tok.val
        self.nwaits += 1

    def _deps(self, e, reads, writes):
        same_ok = (e == "pe") or os.environ.get("KD_SAMEENG", "1") == "0"
        for b in reads:
            if b.w is not None:
                self._wait(e, b.w)
            if b.excl:
                for t in b.r:
                    if t.eng != e:
                        self._wait(e, t)
        for b in writes:
            if b.w is not None and not (same_ok and b.w.eng == e):
                self._wait(e, b.w)
            for t in b.r:
                if not (same_ok and t.eng == e):
                    self._wait(e, t)

    @staticmethod
    def _compact(toks):
        best = {}
        for t in toks:
            k = id(t.sem)
            if k not in best or best[k].val < t.val:
                best[k] = t
        return list(best.values())

    def _mark(self, tok, reads, writes):
        for b in reads:
            b.r.append(tok)
            if len(b.r) > 32:
                b.r = self._compact(b.r)
        for b in writes:
            b.w = tok
            b.r = []

    def op(self, e, fn, reads=(), writes=(), inc=True):
        reads = [x.b if isinstance(x, TT) else x for x in reads]
        writes = [x.b if isinstance(x, TT) else x for x in writes]
        self._deps(e, reads, writes)
        ins = fn(self.eng[e])
        self.ninstr[e] += 1
        if inc:
            self.ecnt[e] += 1
            ins.then_inc(self.esem[e], 1)
            tok = Tok(self.esem[e], self.ecnt[e], e)
        else:
            tok = Tok(self.esem[e], self.ecnt[e] + 1, e)
        self._mark(tok, reads, writes)
        return ins

    def dma(self, e, out, in_, reads=(), writes=()):
        reads = [x.b if isinstance(x, TT) else x for x in reads]
        writes = [x.b if isinstance(x, TT) else x for x in writes]
        self._deps(e, reads, writes)
        owner = writes[0] if writes else reads[0]
        if owner.dsem is None:
            owner.dsem = self.es.enter_context(self.nc.semaphore("ds%d_%s" % (len(self.dsems), owner.name)))
            self.dsems.append(owner)
        ins = self.eng[e].dma_start(out=out, in_=in_)
        owner.dcnt += 16
        ins.then_inc(owner.dsem, 16)
        self.ninstr[e] += 1
        self._mark(Tok(owner.dsem, owner.dcnt, "dma"), reads, writes)
        return ins

    def barrier(self):
        toks = [Tok(self.esem[e], self.ecnt[e], e) for e in self.esem if self.ecnt[e] > 0]
        toks += [Tok(b.dsem, b.dcnt, "dma") for b in self.dsems]
        for e in self.eng:
            for t in toks:
                if t.eng != e:
                    self._wait(e, t)


class Ring:
    def __init__(self, items):
        self.items, self.i = items, 0

    def next(self):
        it = self.items[self.i % len(self.items)]
        self.i += 1
        return it


def build_program(stage=99, dbg_w=0):
    nc = bass.Bass("TRN2", target_bir_lowering=False)

    def din(name, shape):
        return nc.dram_tensor(name, list(shape), F32, kind="ExternalInput").ap()

    x_own = din("x_own", [NT, D]); x_oth = din("x_oth", [NT, D]); ctx = din("ctx", [256, D])
    cvec = din("cvec", [128, 16]); w_ada = din("w_ada", [D, 6 * D]); b_ada = din("b_ada", [1, 6 * D])
    gains = din("gains", [1, 4 * D]); w_in = din("w_in", [D, 5632]); lbl = din("lbl", [128, 16])
    gout = din("gout", [128, 1]); lng = din("lng", [1, 512]); lnb = din("lnb", [1, 512])
    wsT = din("wsT", [128, 512]); bsp = din("bsp", [1, 512])
    w_a = din("w_a", [512, D]); w_b = din("w_b", [512, D]); w_o = din("w_o", [D, D])
    w_rt = din("w_rt", [D, NE]); b_rt = din("b_rt", [1, NE])
    w_gu = din("w_gu", [NE + 1, D, 512]); w_dn = din("w_dn", [NE + 1, 256, D])
    ident_d = din("ident", [128, 128]); maskA_d = din("maskA", [128, 128]); maskB_d = din("maskB", [128, 128])
    resetm_d = din("resetm", [128, 512])
    out = nc.dram_tensor("out", [NT, D], F32, kind="ExternalOutput").ap()
    x1s = nc.dram_tensor("x1s", [NT, D], F32).ap()
    h2Td = nc.dram_tensor("h2Td", [128, 8, NT], BF16).ap()
    h2Td_b = Buf("h2Td")
    g2s = nc.dram_tensor("g2s", [128, D], F32).ap()
    g2s_b = Buf("g2s")
    dbg = nc.dram_tensor("dbg", [128, dbg_w], F32, kind="ExternalOutput").ap() if dbg_w else None
    x1s_bs = [Buf("x1s%d" % i) for i in range(4)]; out_bs = [Buf("out%d" % i) for i in range(4)]; dbg_b = Buf("dbg")

    w_in_v = w_in.rearrange("(kc p) n -> p kc n", p=128)
    w_ada_v = w_ada.rearrange("(kc p) n -> p kc n", p=128)

    with ExitStack() as es:
        S = Sched(nc, es)

        uid = [0]

        def sb(scope, name, shape, dt):
            uid[0] += 1
            return TT(scope.enter_context(nc.sbuf_tensor("s%d_%s" % (uid[0], name), list(shape), dt)), name)

        def psb(scope, name, shape, dt):
            uid[0] += 1
            t = TT(scope.enter_context(nc.psum_tensor("p%d_%s" % (uid[0], name), list(shape), dt)), name)
            t.b.excl = True
            return t

        def mm(ps, out_ap, pairs, reads, first=True, last=True, inc=None):
            n = len(pairs)
            for i, (l, r) in enumerate(pairs):
                fin = (i == n - 1)
                S.op("pe", lambda e, l=l, r=r, i=i, fin=fin: e.matmul(out_ap, lhsT=l, rhs=r, start=(first and i == 0),
                                                                     stop=(last and fin)),
                     reads=reads, writes=[ps], inc=(fin if inc is None else (inc and fin)))

        dbg_col = [0]

        def dump(tt, ap, w):
            if dbg is None:
                return
            S.dma("pool", dbg[:, dbg_col[0]:dbg_col[0] + w], ap, reads=[tt], writes=[dbg_b])
            dbg_col[0] += w

        ident_bf = sb(es, "ident_bf", [128, 128], BF16)
        ident_f = sb(es, "ident_f", [128, 128], F32)
        maskA = sb(es, "maskA", [128, 128], BF16)
        maskB = sb(es, "maskB", [128, 128], BF16)
        resetm = sb(es, "resetm", [128, 512], F32)
        ones_bf = sb(es, "ones_bf", [128, 128], BF16)
        ones_f = sb(es, "ones_f", [128, 512], F32)
        mod = sb(es, "mod", [128, 2 * D], F32)
        wt_all = sb(es, "wt_all", [128, 16, NE + 1], F32)
        lbt = sb(es, "lbt", [128, 16], F32)
        lbv = sb(es, "lbv", [128, 8], F32)
        oml = sb(es, "oml", [128, 8], F32)
        noml = sb(es, "noml", [128, 8], F32)
        gout_t = sb(es, "gout_t", [128, 1], F32)
        screp = sb(es, "screp", [128, 16, 128], BF16)

        S.dma("pool", ident_bf[:], ident_d[:, :], writes=[ident_bf])
        S.dma("sp", ident_f[:], ident_d[:, :], writes=[ident_f])
        S.dma("pool", maskA[:], maskA_d[:, :], writes=[maskA])
        S.dma("pool", maskB[:], maskB_d[:, :], writes=[maskB])
        S.dma("sp", resetm[:], resetm_d[:, :], writes=[resetm])
        S.dma("sp", lbt[:], lbl[:, :], writes=[lbt])
        S.dma("sp", gout_t[:], gout[:, :], writes=[gout_t])
        S.op("dve", lambda e: e.memset(ones_bf[:], 1.0), writes=[ones_bf])
        S.op("dve", lambda e: e.memset(ones_f[:], 1.0), writes=[ones_f])
        S.op("dve", lambda e: e.memset(wt_all[:], 1.0), writes=[wt_all])
        lb3 = lbt[:].rearrange("p (d s h) -> p d s h", d=2, s=2)
        S.op("dve", lambda e: e.tensor_tensor(out=oml[:].rearrange("p (d h) -> p d h", d=2), in0=lb3[:, :, 0, :],
                                              in1=lb3[:, :, 1, :], op=ALU.subtract), reads=[lbt], writes=[oml])
        S.op("act", lambda e: e.activation(out=lbv[:], in_=oml[:], func=AF.Sigmoid), reads=[oml], writes=[lbv])
        S.op("dve", lambda e: e.tensor_scalar(out=oml[:], in0=lbv[:], scalar1=-1.0, scalar2=1.0, op0=ALU.mult, op1=ALU.add),
             reads=[lbv], writes=[oml])
        S.op("dve", lambda e: e.tensor_scalar(out=noml[:], in0=lbv[:], scalar1=1.0, scalar2=-1.0, op0=ALU.mult, op1=ALU.add),
             reads=[lbv], writes=[noml])

        def norm_pool(scope):
            np_ = dict(
                xts=Ring([sb(scope, "xt%d" % i, [128, D], F32) for i in range(3)]),
                junk=sb(scope, "junk", [128, D], BF16),
                hns=Ring([sb(scope, "hn%d" % i, [128, D], F32) for i in range(2)]),
                hbs=Ring([sb(scope, "hb%d" % i, [128, D], BF16) for i in range(3)]),
                stats=Ring([sb(scope, "nst%d" % i, [128, 4], F32) for i in range(4)]),
                tps=Ring([psb(scope, "tps%d" % i, [128, 1024], BF16) for i in range(2)]),
            )
            return np_

        def norm_a(np_, src_rows, src_bufs, Aap, Bap, mods):
            xt = np_["xts"].next(); hn = np_["hns"].next(); hb = np_["hbs"].next(); st = np_["stats"].next()
            junk = np_["junk"]
            S.dma("sp", xt[:], src_rows, reads=src_bufs, writes=[xt])
            S.op("act", lambda e: e.activation(out=junk[:], in_=xt[:], func=AF.Square, accum_out=st[:, 0:1]),
                 reads=[xt], writes=[junk, st])
            S.op("act", lambda e: e.activation(out=st[:, 1:2], in_=st[:, 0:1], func=AF.Sqrt, scale=1.0 / D, bias=EPS),
                 reads=[st], writes=[st])
            S.op("dve", lambda e: e.reciprocal(out=st[:, 2:3], in_=st[:, 1:2]), reads=[st], writes=[st])
            S.op("dve", lambda e: e.scalar_tensor_tensor(out=hn[:], in0=xt[:], scalar=st[:, 2:3], in1=Aap,
                                                          op0=ALU.mult, op1=ALU.mult), reads=[xt, st] + mods, writes=[hn])
            S.op("dve", lambda e: e.tensor_tensor(out=hb[:], in0=hn[:], in1=Bap, op=ALU.add),
                 reads=[hn] + mods, writes=[hb])
            return hb

        def norm_b(np_, hb, hT_dst, hT_tt):
            tp = np_["tps"].next()
            for kc in range(8):
                S.op("pe", lambda e, kc=kc: e.transpose(tp[:, kc * 128:(kc + 1) * 128], hb[:, kc * 128:(kc + 1) * 128],
                                                        ident_bf[:]),
                     reads=[hb, ident_bf], writes=[tp], inc=(kc == 7))
            S.op("act", lambda e: e.activation(out=hT_dst, in_=tp[:].rearrange("p (a b) -> p a b", a=8), func=AF.Copy),
                 reads=[tp], writes=[hT_tt])

        def norm_seq(np_, items):
            prev = None
            for it in items:
                hb = norm_a(np_, *it[:5])
                if prev is not None:
                    norm_b(np_, prev[0], prev[1], prev[2])
                prev = (hb, it[5], it[6])
            norm_b(np_, prev[0], prev[1], prev[2])

        with ExitStack() as sm:
            hT_own = sb(sm, "hT_own", [128, 8, NT], BF16)
            aT = sb(sm, "aT", [128, 4, NT], BF16)
            sw = ExitStack()
            wfA = sb(sw, "wfA", [128, 8, 512], BF16)
            wfB = sb(sw, "wfB", [128, 8, 512], BF16)
            wi = sb(sw, "wi", [128, 8, 512], BF16)
            Sst = [sb(sw, "SstA", [128, 4, 128], F32), sb(sw, "SstB", [128, 4, 128], F32)]
            S.dma("pool", wfA[:], w_in_v[:, :, 512:1024], writes=[wfA])
            S.dma("pool", wfB[:], w_in_v[:, :, 1024:1536], writes=[wfB])
            S.dma("pool", wi[:], w_in_v[:, :, 1536:2048], writes=[wi])

            with ExitStack() as s12:
                modc = sb(s12, "modc", [128, 2 * D], F32)
                with ExitStack() as p1:
                    cv = sb(p1, "cv", [128, 16], F32)
                    scv = sb(p1, "scv", [128, 16], F32)
                    gns = sb(p1, "gns", [128, D], F32)
                    wada = [sb(p1, "wada%d" % i, [128, 8, 512], BF16) for i in range(2)]
                    bada = [sb(p1, "bada%d" % i, [128, 512], F32) for i in range(2)]
                    pp = Ring([psb(p1, "p1ps%d" % i, [128, 512], F32) for i in range(4)])
                    S.dma("sp", cv[:], cvec[:, :], writes=[cv])
                    S.dma("sp", gns[:], gains[0:1, 0:D].partition_broadcast(128), writes=[gns])
                    S.op("act", lambda e: e.activation(out=scv[:], in_=cv[:], func=AF.Silu), reads=[cv], writes=[scv])
                    S.op("dve", lambda e: e.tensor_copy(out=screp[:], in_=scv[:].unsqueeze(2).to_broadcast([128, 16, 128])),
                         reads=[scv], writes=[screp])
                    for j in range(4):
                        wb_, bb_ = wada[j % 2], bada[j % 2]
                        S.dma("pool", wb_[:], w_ada_v[:, :, j * 512:(j + 1) * 512], writes=[wb_])
                        S.dma("sp", bb_[:], b_ada[0:1, j * 512:(j + 1) * 512].partition_broadcast(128), writes=[bb_])
                        ps = pp.next()
                        mm(ps, ps[:], [(screp[:, kc, :], wb_[:, kc, :]) for kc in range(8)], [screp, wb_])
                        S.op("dve", lambda e, ps=ps, bb_=bb_, j=j: e.tensor_tensor(out=mod[:, j * 512:(j + 1) * 512], in0=ps[:],
                                                                                    in1=bb_[:], op=ALU.add),
                             reads=[ps, bb_], writes=[mod])
                        if j < 4:
                            ps = pp.next()
                            mm(ps, ps[:], [(screp[:, 8 + kc, :], wb_[:, kc, :]) for kc in range(8)], [screp, wb_])
                            S.op("dve", lambda e, ps=ps, bb_=bb_, j=j: e.tensor_tensor(out=modc[:, j * 512:(j + 1) * 512],
                                                                                        in0=ps[:], in1=bb_[:], op=ALU.add),
                                 reads=[ps, bb_], writes=[modc])

                    def scale1p(dst, lo, g0):
                        S.op("dve", lambda e: e.scalar_tensor_tensor(out=dst[:, lo:lo + D], in0=dst[:, lo:lo + D], scalar=1.0,
                                                                      in1=gns[:, g0 * D:(g0 + 1) * D], op0=ALU.add, op1=ALU.mult),
                             reads=[dst, gns], writes=[dst])

                    def scaleg(dst, lo, g0):
                        S.op("dve", lambda e: e.tensor_tensor(out=dst[:, lo:lo + D], in0=dst[:, lo:lo + D],
                                                              in1=gns[:, g0 * D:(g0 + 1) * D], op=ALU.mult),
                             reads=[dst, gns], writes=[dst])
                    scale1p(mod, 1 * D, 0); scale1p(modc, 1 * D, 0)
                    if stage == 1:
                        dump(mod, mod[:, 0:2 * D], 2 * D); dump(modc, modc[:, :], 2 * D)
                    S.barrier()
                if stage == 1:
                    S.barrier()
                    return nc, S

                with ExitStack() as p2:
                    npl = norm_pool(p2)
                    tps = npl["tps"]
                    hTg = sb(p2, "hTg", [128, 8, 512], BF16)
                    vg = sb(p2, "vg", [128, 4, 512], BF16)
                    ktT = sb(p2, "ktT", [128, 4, 512], BF16)
                    ktoks = Ring([sb(p2, "ktok%d" % i, [128, 512], BF16) for i in range(2)])
                    carry = sb(p2, "carry", [128, 4], F32)
                    tots = sb(p2, "tots", [128, 4], F32)
                    tf = {n: Ring([sb(p2, "p2%s%d" % (n, i), [128, 512], F32) for i in range(4 if n == "sig" else 2)])
                          for n in ("sig", "lf", "k", "pin", "pex")}
                    S0ps = [psb(p2, "S0psA", [128, 512], F32), psb(p2, "S0psB", [128, 512], F32)]
                    pr = Ring([psb(p2, "p2ps%d" % i, [128, 512], F32) for i in range(4)])
                    S.op("dve", lambda e: e.memset(carry[:], 0.0), writes=[carry])
                    nB = [0]
                    totB = 18

                    def gate_chain_state(z, ncol, d, h, mode):
                        lf = tf["lf"].next(); kk = tf["k"].next(); pin = tf["pin"].next()
                        pex = tf["pex"].next()
                        c = d * 4 + h
                        sg = z
                        S.op("act", lambda e: e.activation(out=lf[:, :ncol], in_=sg[:, :ncol], func=AF.Ln, scale=oml[:, c:c + 1],
                                                           bias=lbv[:, c:c + 1]), reads=[sg, oml, lbv], writes=[lf])
                        S.op("dve", lambda e: e.tensor_scalar(out=kk[:, :ncol], in0=sg[:, :ncol], scalar1=noml[:, c:c + 1],
                                                               scalar2=oml[:, c:c + 1], op0=ALU.mult, op1=ALU.add),
                             reads=[sg, noml, oml], writes=[kk])
                        if mode == "B":
                            S.op("dve", lambda e: e.tensor_tensor_scan(out=pin[:, :ncol], data0=ones_f[:, :ncol], data1=lf[:, :ncol],
                                                                        initial=carry[:, h:h + 1], op0=ALU.mult, op1=ALU.add),
                                 reads=[ones_f, lf, carry], writes=[pin])
                            S.op("act", lambda e: e.activation(out=carry[:, h:h + 1], in_=pin[:, ncol - 1:ncol], func=AF.Copy),
                                 reads=[pin], writes=[carry])
                            S.op("dve", lambda e: e.tensor_tensor(out=pex[:, :ncol], in0=pin[:, :ncol], in1=lf[:, :ncol],
                                                                  op=ALU.subtract), reads=[pin, lf], writes=[pex])
                            S.op("act", lambda e: e.activation(out=pex[:, :ncol], in_=pex[:, :ncol], func=AF.Exp),
                                 reads=[pex], writes=[pex])
                        else:
                            S.op("dve", lambda e: e.tensor_tensor_scan(out=pin[:, :ncol], data0=ones_f[:, :ncol], data1=lf[:, :ncol],
                                                                        initial=0.0, op0=ALU.mult, op1=ALU.add),
                                 reads=[ones_f, lf], writes=[pin])
                            S.op("act", lambda e: e.activation(out=tots[:, h:h + 1], in_=pin[:, ncol - 1:ncol], func=AF.Copy),
                                 reads=[pin], writes=[tots])
                            S.op("act", lambda e: e.activation(out=pex[:, :ncol], in_=pin[:, :ncol], func=AF.Exp, scale=-1.0,
                                                               bias=tots[:, h:h + 1]), reads=[pin, tots], writes=[pex])
                        S.op("dve", lambda e: e.tensor_tensor(out=ktT[:, h, :ncol], in0=kk[:, :ncol], in1=pex[:, :ncol], op=ALU.mult),
                             reads=[kk, pex], writes=[ktT])

                    def state_accum(ntile, d):
                        for t in range(ntile):
                            tp = tps.next(); kt = ktoks.next()
                            for h in range(4):
                                S.op("pe", lambda e, h=h, t=t, tp=tp: e.transpose(tp[:, h * 128:(h + 1) * 128],
                                                                                   ktT[:, h, t * 128:(t + 1) * 128], ident_bf[:]),
                                     reads=[ktT, ident_bf], writes=[tp], inc=(h == 3))
                            S.op("act", lambda e, tp=tp, kt=kt: e.activation(out=kt[:], in_=tp[:, 0:512], func=AF.Copy),
                                 reads=[tp], writes=[kt])
                            for h in range(4):
                                if d == 1:
                                    st_ = (nB[0] == 0); nB[0] += 1; sp_ = (nB[0] > totB * 4 - 4)
                                else:
                                    st_ = (t == 0 and h == 0); sp_ = (t == ntile - 1)
                                S.op("pe", lambda e, h=h, t=t, kt=kt, st_=st_, sp_=sp_: e.matmul(
                                    S0ps[d][:, h * 128:(h + 1) * 128], lhsT=kt[:, h * 128:(h + 1) * 128],
                                    rhs=vg[:, t, h * 128:(h + 1) * 128], start=st_, stop=sp_, skip_group_check=True),
                                    reads=[kt, vg], writes=[S0ps[d]], inc=True)

                    for g in range(4):
                        norm_seq(npl, [(x_oth[(g * 4 + t) * 128:(g * 4 + t + 1) * 128, :], [], mod[:, D:2 * D], mod[:, 0:D], [mod],
                                        hTg[:, :, t * 128:(t + 1) * 128], hTg) for t in range(4)])
                        for t in range(4):
                            ps = pr.next()
                            mm(ps, ps[:], [(hTg[:, kc, t * 128:(t + 1) * 128], wi[:, kc, :]) for kc in range(8)], [hTg, wi])
                            S.op("act", lambda e, ps=ps, t=t: e.activation(out=vg[:, t, :], in_=ps[:], func=AF.Copy), reads=[ps], writes=[vg])
                        sgs_ = []
                        for h in range(4):
                            ps = pr.next()
                            mm(ps, ps[:], [(wfB[:, kc, h * 128:(h + 1) * 128], hTg[:, kc, :]) for kc in range(8)], [hTg, wfB])
                            sg = tf["sig"].next()
                            S.op("act", lambda e, ps=ps, sg=sg: e.activation(out=sg[:], in_=ps[:], func=AF.Sigmoid), reads=[ps], writes=[sg])
                            sgs_.append(sg)
                        for h in range(4):
                            gate_chain_state(sgs_[h], 512, 1, h, "B")
                        state_accum(4, 1)
                    norm_seq(npl, [(ctx[t * 128:(t + 1) * 128, :], [], modc[:, D:2 * D], modc[:, 0:D], [modc],
                                    hTg[:, :, t * 128:(t + 1) * 128], hTg) for t in range(2)])
                    for t in range(2):
                        ps = pr.next()
                        mm(ps, ps[:], [(hTg[:, kc, t * 128:(t + 1) * 128], wi[:, kc, :]) for kc in range(8)], [hTg, wi])
                        S.op("act", lambda e, ps=ps, t=t: e.activation(out=vg[:, t, :], in_=ps[:], func=AF.Copy), reads=[ps], writes=[vg])
                    for (wf_, d_, mode_) in ((wfB, 1, "B"), (wfA, 0, "A")):
                        sgs_ = []
                        for h in range(4):
                            ps = pr.next()
                            mm(ps, ps[:, 0:256], [(wf_[:, kc, h * 128:(h + 1) * 128], hTg[:, kc, 0:256]) for kc in range(8)], [hTg, wf_])
                            sg = tf["sig"].next()
                            S.op("act", lambda e, ps=ps, sg=sg: e.activation(out=sg[:, 0:256], in_=ps[:, 0:256], func=AF.Sigmoid), reads=[ps], writes=[sg])
                            sgs_.append(sg)
                        for h in range(4):
                            gate_chain_state(sgs_[h], 256, d_, h, mode_)
                        state_accum(2, d_)
                    assert nB[0] == totB * 4
                    for d in range(2):
                        S.op("act", lambda e, d=d: e.activation(out=Sst[d][:].rearrange("p a b -> p (a b)"), in_=S0ps[d][:], func=AF.Copy),
                             reads=[S0ps[d]], writes=[Sst[d]])
                    if stage == 2:
                        dump(Sst[0], Sst[0][:].rearrange("p a b -> p (a b)"), 512)
                        dump(Sst[1], Sst[1][:].rearrange("p a b -> p (a b)"), 512)
                    S.barrier()
                if stage == 2:
                    S.barrier()
                    return nc, S

            with ExitStack() as s3:
                npl = norm_pool(s3)
                norm_seq(npl, [(x_own[t * 128:(t + 1) * 128, :], [], mod[:, D:2 * D], mod[:, 0:D], [mod],
                                hT_own[:, :, t * 128:(t + 1) * 128], hT_own) for t in range(16)])
                S.barrier()

            with ExitStack() as sh:
                v_own = sb(sh, "v_own", [128, 16, 512], BF16)
                hps = Ring([psb(sh, "hps%d" % i, [128, 512], F32) for i in range(6)])
                tps = Ring([psb(sh, "tpsh%d" % i, [128, 1024], BF16) for i in range(2)])
                for t in range(16):
                    ps = hps.next()
                    mm(ps, ps[:], [(hT_own[:, kc, t * 128:(t + 1) * 128], wi[:, kc, :]) for kc in range(8)], [hT_own, wi])
                    S.op("act", lambda e, ps=ps, t=t: e.activation(out=v_own[:, t, :], in_=ps[:], func=AF.Copy), reads=[ps], writes=[v_own])
                qh = [sb(sh, "qh%d" % d, [128, NT], BF16) for d in range(2)]
                kh = [sb(sh, "kh%d" % d, [128, NT], BF16) for d in range(2)]
                ktok = [sb(sh, "ktokh%d" % d, [128, 16, 128], BF16) for d in range(2)]
                oacc = sb(sh, "oacc", [128, NT], F32)
                gT = sb(sh, "gT", [128, NT], BF16)
                expT = [sb(sh, "expT%d" % d, [128, 33], F32) for d in range(2)]
                for d in range(2):
                    S.op("dve", lambda e, d=d: e.memset(expT[d][:, 32:33], 1.0), writes=[expT[d]])
                wqs = Ring([sb(sh, "wq%d" % i, [128, 8, 128], BF16) for i in range(2)])
                wgs = Ring([sb(sh, "wg%d" % i, [128, 8, 128], BF16) for i in range(2)])
                tf = {n: Ring([sb(sh, "h%s%d" % (n, i), [128, 512], F32) for i in range(2)])
                      for n in ("qf", "sig", "lf", "k", "pin", "e1", "e2")}
                Sfb = [[sb(sh, "Sf%d_%d" % (d, i), [128, 128], F32) for i in range(2)] for d in range(2)]
                sfi = [0, 0]
                Sbf = [Ring([sb(sh, "Sbf%d_%d" % (d, i), [128, 128], BF16) for i in range(3)]) for d in range(2)]
                ATs = Ring([sb(sh, "ATs%d" % i, [128, 128], BF16) for i in range(4)])
                og_sq = sb(sh, "og_sq", [128, 512], BF16)
                og_sd = sb(sh, "og_sd", [128, 512], F32)
                og_t1 = sb(sh, "og_t1", [128, 512], F32)

                for h in range(4):
                    hc = slice(h * 128, (h + 1) * 128)
                    wq_h = wqs.next(); wg_h = wgs.next()
                    S.dma("pool", wq_h[:], w_in_v[:, :, h * 128:(h + 1) * 128], writes=[wq_h])
                    S.dma("pool", wg_h[:], w_in_v[:, :, 2048 + h * 128:2048 + (h + 1) * 128], writes=[wg_h])
                    for tg in range(4):
                        cs = slice(tg * 512, (tg + 1) * 512)
                        ps = hps.next()
                        mm(ps, ps[:], [(wq_h[:, kc, :], hT_own[:, kc, cs]) for kc in range(8)], [wq_h, hT_own])
                        qf = tf["qf"].next()
                        S.op("act", lambda e, ps=ps, qf=qf: e.activation(out=qf[:], in_=ps[:], func=AF.Silu), reads=[ps], writes=[qf])
                        ps = hps.next()
                        mm(ps, ps[:], [(wg_h[:, kc, :], hT_own[:, kc, cs]) for kc in range(8)], [wg_h, hT_own])
                        S.op("act", lambda e, ps=ps, cs=cs: e.activation(out=gT[:, cs], in_=ps[:], func=AF.Silu), reads=[ps], writes=[gT])
                        sgd = []
                        for d in range(2):
                            wf = wfA if d == 0 else wfB
                            ps = hps.next()
                            mm(ps, ps[:], [(wf[:, kc, hc], hT_own[:, kc, cs]) for kc in range(8)], [wf, hT_own])
                            sg = tf["sig"].next()
                            S.op("act", lambda e, ps=ps, sg=sg: e.activation(out=sg[:], in_=ps[:], func=AF.Sigmoid), reads=[ps], writes=[sg])
                            sgd.append(sg)
                        for d in range(2):
                            c = d * 4 + h
                            sg = sgd[d]
                            lf = tf["lf"].next(); kk = tf["k"].next(); pin = tf["pin"].next()
                            e1 = tf["e1"].next(); e2 = tf["e2"].next()
                            S.op("act", lambda e, sg=sg, lf=lf, c=c: e.activation(out=lf[:], in_=sg[:], func=AF.Ln, scale=oml[:, c:c + 1],
                                                                                  bias=lbv[:, c:c + 1]), reads=[sg, oml, lbv], writes=[lf])
                            S.op("dve", lambda e, sg=sg, kk=kk, c=c: e.tensor_scalar(out=kk[:], in0=sg[:], scalar1=noml[:, c:c + 1],
                                                                                      scalar2=oml[:, c:c + 1], op0=ALU.mult, op1=ALU.add),
                                 reads=[sg, noml, oml], writes=[kk])
                            S.op("dve", lambda e, pin=pin, lf=lf: e.tensor_tensor_scan(out=pin[:], data0=resetm[:], data1=lf[:], initial=0.0,
                                                                                        op0=ALU.mult, op1=ALU.add),
                                 reads=[resetm, lf], writes=[pin])
                            S.op("act", lambda e, pin=pin, d=d, tg=tg: e.activation(out=expT[d][:, tg * 8:(tg + 1) * 8], in_=pin[:, 63::64],
                                                                                    func=AF.Exp), reads=[pin], writes=[expT[d]])
                            if d == 0:
                                S.op("act", lambda e, pin=pin, e1=e1: e.activation(out=e1[:], in_=pin[:], func=AF.Exp), reads=[pin], writes=[e1])
                                S.op("act", lambda e, pin=pin, e2=e2: e.activation(out=e2[:], in_=pin[:], func=AF.Exp, scale=-1.0),
                                     reads=[pin], writes=[e2])
                            else:
                                S.op("dve", lambda e, pin=pin, lf=lf: e.tensor_tensor(out=pin[:], in0=pin[:], in1=lf[:], op=ALU.subtract),
                                     reads=[pin, lf], writes=[pin])
                                S.op("act", lambda e, pin=pin, e1=e1: e.activation(out=e1[:], in_=pin[:], func=AF.Exp, scale=-1.0),
                                     reads=[pin], writes=[e1])
                                S.op("act", lambda e, pin=pin, e2=e2: e.activation(out=e2[:], in_=pin[:], func=AF.Exp), reads=[pin], writes=[e2])
                            S.op("dve", lambda e, qf=qf, e1=e1, d=d, cs=cs: e.tensor_tensor(out=qh[d][:, cs], in0=qf[:], in1=e1[:], op=ALU.mult),
                                 reads=[qf, e1], writes=[qh[d]])
                            S.op("dve", lambda e, kk=kk, e2=e2, d=d, cs=cs: e.tensor_tensor(out=kh[d][:, cs], in0=kk[:], in1=e2[:], op=ALU.mult),
                                 reads=[kk, e2], writes=[kh[d]])
                    for d in range(2):
                        for t4 in range(4):
                            tp = tps.next()
                            for j in range(4):
                                t = t4 * 4 + j
                                S.op("pe", lambda e, tp=tp, j=j, t=t, d=d: e.transpose(tp[:, j * 128:(j + 1) * 128],
                                                                                        kh[d][:, t * 128:(t + 1) * 128], ident_bf[:]),
                                     reads=[kh[d], ident_bf], writes=[tp], inc=(j == 3))
                            S.op("act", lambda e, tp=tp, d=d, t4=t4: e.activation(out=ktok[d][:, t4 * 4:(t4 + 1) * 4, :],
                                                                                  in_=tp[:, 0:512].rearrange("p (a b) -> p a b", a=4),
                                                                                  func=AF.Copy), reads=[tp], writes=[ktok[d]])
                    qi = [0, 0]
                    prev_ci = [32, None]
                    for d in range(2):
                        S.op("dve", lambda e, d=d: e.tensor_copy(out=Sfb[d][0][:], in_=Sst[d][:, h, :]), reads=[Sst[d]], writes=[Sfb[d][0]])

                    def o_store(t, oTp, first):
                        tok = slice(t * 128, (t + 1) * 128)
                        if first:
                            S.op("act", lambda e: e.activation(out=oacc[:, tok], in_=oTp[:, 0:128], func=AF.Copy), reads=[oTp], writes=[oacc])
                        else:
                            S.op("dve", lambda e: e.tensor_tensor(out=oacc[:, tok], in0=oTp[:, 0:128], in1=oacc[:, tok], op=ALU.add),
                                 reads=[oTp, oacc], writes=[oacc])

                    def step_pre(d, t):
                        tok = slice(t * 128, (t + 1) * 128)
                        order = (0, 1) if d == 0 else (1, 0)
                        mask = maskA if d == 0 else maskB
                        ATp = hps.next()
                        mm(ATp, ATp[:, 0:128], [(kh[d][:, tok], qh[d][:, tok])], [kh[d], qh[d]])
                        Pbs = {}
                        for c in order:
                            ck = slice(c * 64, (c + 1) * 64)
                            Pb = hps.next()
                            Pbs[c] = Pb
                            S.op("pe", lambda e, c=c, ck=ck, Pb=Pb: e.matmul(Pb[:, 0:128], lhsT=ktok[d][ck, t, :], rhs=v_own[ck, t, hc],
                                                                             start=True, stop=True),
                                 reads=[ktok[d], v_own], writes=[Pb])
                        at = ATs.next()
                        S.op("dve", lambda e: e.tensor_tensor(out=at[:], in0=ATp[:, 0:128], in1=mask[:], op=ALU.mult),
                             reads=[ATp, mask], writes=[at])
                        oTp = ATp
                        S.op("pe", lambda e: e.matmul(oTp[:, 0:128], lhsT=v_own[:, t, hc], rhs=at[:], start=True, stop=False),
                             reads=[v_own, at], writes=[oTp], inc=False)
                        return (d, t, order, Pbs, oTp)

                    def step_chunk(ctx_, k):
                        d, t, order, Pbs, oTp = ctx_
                        c = order[k]
                        ck = slice(c * 64, (c + 1) * 64)
                        toks = slice(t * 128 + c * 64, t * 128 + (c + 1) * 64)
                        ci = t * 2 + c
                        ei = prev_ci[0] if d == 0 else ci
                        qo, qn = Sfb[d][qi[d]], Sfb[d][1 - qi[d]]
                        qi[d] = 1 - qi[d]
                        nb = Sbf[d].next()
                        S.op("act", lambda e: e.activation(out=nb[:], in_=qo[:], func=AF.Identity, scale=expT[d][:, ei:ei + 1]),
                             reads=[qo, expT[d]], writes=[nb])
                        S.op("pe", lambda e: e.matmul(oTp[:, ck], lhsT=nb[:], rhs=qh[d][:, toks], start=False, stop=(k == 1)),
                             reads=[nb, qh[d]], writes=[oTp], inc=(k == 1))
                        S.op("dve", lambda e: e.scalar_tensor_tensor(out=qn[:], in0=qo[:], scalar=expT[d][:, ei:ei + 1], in1=Pbs[c][:, 0:128],
                                                                      op0=ALU.mult, op1=ALU.add), reads=[qo, expT[d], Pbs[c]], writes=[qn])
                        if d == 0:
                            prev_ci[0] = ci

                    for i in range(16):
                        ca = step_pre(0, i)
                        cb = step_pre(1, 15 - i)
                        for k in range(2):
                            step_chunk(ca, k)
                            step_chunk(cb, k)
                        o_store(i, ca[4], first=(i <= 7))
                        o_store(15 - i, cb[4], first=(i <= 7))
                    for g in range(4):
                        cs = slice(g * 512, (g + 1) * 512)
                        S.op("act", lambda e, cs=cs: e.activation(out=og_sq[:], in_=oacc[:, cs], func=AF.Square), reads=[oacc], writes=[og_sq])
                        ssp = hps.next()
                        mm(ssp, ssp[:], [(ones_bf[:], og_sq[:])], [ones_bf, og_sq])
                        S.op("act", lambda e, ssp=ssp: e.activation(out=og_sd[:], in_=ssp[:], func=AF.Sqrt, scale=1.0 / 128, bias=EPS),
                             reads=[ssp], writes=[og_sd])
                        S.op("dve", lambda e: e.reciprocal(out=og_sd[:], in_=og_sd[:]), reads=[og_sd], writes=[og_sd])
                        S.op("dve", lambda e, cs=cs: e.scalar_tensor_tensor(out=og_t1[:], in0=oacc[:, cs], scalar=gout_t[:, 0:1], in1=og_sd[:],
                                                                             op0=ALU.mult, op1=ALU.mult),
                             reads=[oacc, gout_t, og_sd], writes=[og_t1])
                        S.op("dve", lambda e, cs=cs: e.tensor_tensor(out=aT[:, h, cs], in0=og_t1[:], in1=gT[:, cs], op=ALU.mult),
                             reads=[og_t1, gT], writes=[aT])
                if stage == 3:
                    for h in range(4):
                        dump(aT, aT[:, h, 0:1024], 1024)
                S.barrier()
            sw.close()
            if stage == 3:
                S.barrier()
                return nc, S

            with ExitStack() as scm:
                bmT = sb(scm, "bmT", [128, 4, NT], BF16)
                modB = sb(scm, "modB", [128, 4 * D], F32)
                with ExitStack() as sc:
                    wu = sb(sc, "wu", [128, 8, 512], BF16)
                    wcv = sb(sc, "wcv", [128, 8, 512], BF16)
                    lng_bc = sb(sc, "lng_bc", [128, 512], F32)
                    lnb_bc = sb(sc, "lnb_bc", [128, 512], F32)
                    bs_bc = sb(sc, "bs_bc", [128, 512], F32)
                    wsT_bf = sb(sc, "wsT_bf", [128, 512], BF16)
                    S.dma("pool", wu[:], w_in_v[:, :, 2560:3072], writes=[wu])
                    S.dma("pool", wcv[:], w_in_v[:, :, 3072:3584], writes=[wcv])
                    S.dma("pool", wsT_bf[:], wsT[:, :], writes=[wsT_bf])
                    S.dma("sp", lng_bc[:], lng[0:1, :].partition_broadcast(128), writes=[lng_bc])
                    S.dma("sp", lnb_bc[:], lnb[0:1, :].partition_broadcast(128), writes=[lnb_bc])
                    S.dma("sp", bs_bc[:], bsp[0:1, :].partition_broadcast(128), writes=[bs_bc])
                    uTs = Ring([sb(sc, "uT%d" % i, [128, 4, 512], BF16) for i in range(2)])
                    ges = Ring([sb(sc, "ge%d" % i, [128, 512], F32) for i in range(2)])
                    xns = Ring([sb(sc, "xn%d" % i, [128, 512], F32) for i in range(2)])
                    vlns = Ring([sb(sc, "vln%d" % i, [128, 512], BF16) for i in range(2)])
                    t1s = Ring([sb(sc, "ct1%d" % i, [128, 512], F32) for i in range(2)])
                    csts = Ring([sb(sc, "cst%d" % i, [128, 12], F32) for i in range(4)])
                    cps = Ring([psb(sc, "cps%d" % i, [128, 512], F32) for i in range(6)])
                    wada2 = [sb(sc, "wadb%d" % i, [128, 8, 512], BF16) for i in range(2)]
                    bada2 = [sb(sc, "badb%d" % i, [128, 512], F32) for i in range(2)]
                    gn2 = sb(sc, "gn2", [128, D], F32)

                    def p1_late(j):
                        wb_, bb_ = wada2[j % 2], bada2[j % 2]
                        S.dma("pool", wb_[:], w_ada_v[:, :, j * 512:(j + 1) * 512], writes=[wb_])
                        S.dma("sp", bb_[:], b_ada[0:1, j * 512:(j + 1) * 512].partition_broadcast(128), writes=[bb_])
                        ps = cps.next()
                        mm(ps, ps[:], [(screp[:, kc, :], wb_[:, kc, :]) for kc in range(8)], [screp, wb_])
                        S.op("dve", lambda e: e.tensor_tensor(out=modB[:, (j - 4) * 512:(j - 3) * 512], in0=ps[:], in1=bb_[:], op=ALU.add),
                             reads=[ps, bb_], writes=[modB])

                    def late_scale(lo, g0, plus1):
                        S.dma("sp", gn2[:], gains[0:1, g0 * D:(g0 + 1) * D].partition_broadcast(128), writes=[gn2])
                        if plus1:
                            S.op("dve", lambda e: e.scalar_tensor_tensor(out=modB[:, lo:lo + D], in0=modB[:, lo:lo + D], scalar=1.0, in1=gn2[:],
                                                                          op0=ALU.add, op1=ALU.mult), reads=[modB, gn2], writes=[modB])
                        else:
                            S.op("dve", lambda e: e.tensor_tensor(out=modB[:, lo:lo + D], in0=modB[:, lo:lo + D], in1=gn2[:], op=ALU.mult),
                                 reads=[modB, gn2], writes=[modB])

                    for tg in range(4):
                        p1_late(4 + 2 * tg); p1_late(5 + 2 * tg)
                        cs = slice(tg * 512, (tg + 1) * 512)
                        uTg = uTs.next()
                        for g in range(4):
                            ps = cps.next()
                            mm(ps, ps[:], [(wu[:, kc, g * 128:(g + 1) * 128], hT_own[:, kc, cs]) for kc in range(8)], [wu, hT_own])
                            S.op("act", lambda e, ps=ps, g=g, uTg=uTg: e.activation(out=uTg[:, g, :], in_=ps[:], func=AF.Gelu_apprx_tanh),
                                 reads=[ps], writes=[uTg])
                        def cm_a(t):
                            tile_ = tg * 4 + t
                            tok = slice(tile_ * 128, (tile_ + 1) * 128)
                            ps = cps.next()
                            mm(ps, ps[:], [(hT_own[:, kc, tok], wcv[:, kc, :]) for kc in range(8)], [hT_own, wcv])
                            ge = ges.next(); xn = xns.next(); vln = vlns.next(); st = csts.next()
                            S.op("act", lambda e: e.activation(out=ge[:], in_=ps[:], func=AF.Gelu_apprx_tanh), reads=[ps], writes=[ge])
                            S.op("dve", lambda e: e.bn_stats(out=st[:, 0:6], in_=ge[:]), reads=[ge], writes=[st])
                            S.op("dve", lambda e: e.bn_aggr(out=st[:, 6:8], in_=st[:, 0:6]), reads=[st], writes=[st])
                            S.op("act", lambda e: e.activation(out=st[:, 8:9], in_=st[:, 7:8], func=AF.Sqrt, bias=EPS), reads=[st], writes=[st])
                            S.op("dve", lambda e: e.reciprocal(out=st[:, 9:10], in_=st[:, 8:9]), reads=[st], writes=[st])
                            S.op("dve", lambda e: e.tensor_scalar(out=xn[:], in0=ge[:], scalar1=st[:, 6:7], scalar2=st[:, 9:10],
                                                                   op0=ALU.subtract, op1=ALU.mult), reads=[ge, st], writes=[xn])
                            S.op("dve", lambda e: e.tensor_tensor(out=xn[:], in0=xn[:], in1=lng_bc[:], op=ALU.mult), reads=[xn, lng_bc], writes=[xn])
                            S.op("dve", lambda e: e.tensor_tensor(out=vln[:], in0=xn[:], in1=lnb_bc[:], op=ALU.add), reads=[xn, lnb_bc], writes=[vln])
                            return (t, tok, vln)

                        def cm_b(t, tok, vln):
                            zps = cps.next()
                            for g in range(4):
                                S.op("pe", lambda e, g=g: e.matmul(zps[:, g * 128:(g + 1) * 128], lhsT=vln[:, g * 128:(g + 1) * 128],
                                                                   rhs=wsT_bf[:, g * 128:(g + 1) * 128], start=True, stop=True, skip_group_check=True),
                                     reads=[vln, wsT_bf], writes=[zps], inc=(g == 3))
                            t1 = t1s.next()
                            S.op("dve", lambda e: e.tensor_tensor(out=t1[:], in0=zps[:], in1=bs_bc[:], op=ALU.add), reads=[zps, bs_bc], writes=[t1])
                            S.op("dve", lambda e: e.tensor_tensor(out=bmT[:, :, tok], in0=t1[:].rearrange("p (a b) -> p a b", a=4),
                                                                  in1=uTg[:, :, t * 128:(t + 1) * 128], op=ALU.mult), reads=[t1, uTg], writes=[bmT])

                        prev = None
                        for t in range(4):
                            cur_ = cm_a(t)
                            if prev is not None:
                                cm_b(*prev)
                            prev = cur_
                        cm_b(*prev)
                    late_scale(0, 1, False); late_scale(2 * D, 2, True); late_scale(3 * D, 3, False)
                    S.dma("sp", g2s[:, :], modB[:, 3 * D:4 * D], reads=[modB], writes=[g2s_b])
                    if stage == 4:
                        for g in range(4):
                            dump(bmT, bmT[:, g, 0:1024], 1024)
                    S.barrier()
                if stage == 4:
                    S.barrier()
                    return nc, S

                with ExitStack() as sg:
                    yT = sb(sg, "yT", [128, 8, NT], BF16)
                    wo = sb(sg, "wo", [128, 8, D], BF16)
                    S.dma("pool", wo[:], w_o.rearrange("(kc p) n -> p kc n", p=128), writes=[wo])
                    sgc = ExitStack()
                    wgas = Ring([sb(sgc, "wga%d" % i, [128, 8, 128], BF16) for i in range(2)])
                    wgbs = Ring([sb(sgc, "wgb%d" % i, [128, 8, 128], BF16) for i in range(2)])
                    was = Ring([sb(sgc, "wa%d" % i, [128, 4, 128], BF16) for i in range(2)])
                    wbs = Ring([sb(sgc, "wb%d" % i, [128, 4, 128], BF16) for i in range(2)])
                    sgs = Ring([sb(sgc, "sgm%d" % i, [128, 512], F32) for i in range(2)])
                    y1s = Ring([sb(sgc, "y1%d" % i, [128, 512], F32) for i in range(2)])
                    y2s = Ring([sb(sgc, "y2%d" % i, [128, 512], F32) for i in range(2)])
                    gps = Ring([psb(sgc, "gps%d" % i, [128, 512], F32) for i in range(8)])
                    w_a_v = w_a.rearrange("(kc p) n -> p kc n", p=128)
                    w_b_v = w_b.rearrange("(kc p) n -> p kc n", p=128)
                    for c in range(8):
                        wga = wgas.next(); wgb = wgbs.next(); wa_ = was.next(); wb_ = wbs.next()
                        S.dma("pool", wga[:], w_in_v[:, :, 3584 + c * 128:3584 + (c + 1) * 128], writes=[wga])
                        S.dma("pool", wgb[:], w_in_v[:, :, 4608 + c * 128:4608 + (c + 1) * 128], writes=[wgb])
                        S.dma("pool", wa_[:], w_a_v[:, :, c * 128:(c + 1) * 128], writes=[wa_])
                        S.dma("pool", wb_[:], w_b_v[:, :, c * 128:(c + 1) * 128], writes=[wb_])
                        for tg in range(4):
                            cs = slice(tg * 512, (tg + 1) * 512)
                            pa = gps.next()
                            mm(pa, pa[:], [(wa_[:, kc, :], aT[:, kc, cs]) for kc in range(4)], [wa_, aT])
                            pg = gps.next()
                            mm(pg, pg[:], [(wga[:, kc, :], hT_own[:, kc, cs]) for kc in range(8)], [wga, hT_own])
                            sga = sgs.next(); y1 = y1s.next()
                            S.op("act", lambda e, pg=pg, sga=sga: e.activation(out=sga[:], in_=pg[:], func=AF.Sigmoid), reads=[pg], writes=[sga])
                            S.op("dve", lambda e, sga=sga, pa=pa, y1=y1: e.tensor_tensor(out=y1[:], in0=pa[:], in1=sga[:], op=ALU.mult),
                                 reads=[pa, sga], writes=[y1])
                            pb = gps.next()
                            mm(pb, pb[:], [(wb_[:, kc, :], bmT[:, kc, cs]) for kc in range(4)], [wb_, bmT])
                            pg2 = gps.next()
                            mm(pg2, pg2[:], [(wgb[:, kc, :], hT_own[:, kc, cs]) for kc in range(8)], [wgb, hT_own])
                            sgb = sgs.next(); y2 = y2s.next()
                            S.op("act", lambda e, pg2=pg2, sgb=sgb: e.activation(out=sgb[:], in_=pg2[:], func=AF.Sigmoid), reads=[pg2], writes=[sgb])
                            S.op("dve", lambda e, sgb=sgb, pb=pb, y2=y2: e.tensor_tensor(out=y2[:], in0=pb[:], in1=sgb[:], op=ALU.mult),
                                 reads=[pb, sgb], writes=[y2])
                            S.op("dve", lambda e, y1=y1, y2=y2, c=c, cs=cs: e.tensor_tensor(out=yT[:, c, cs], in0=y1[:], in1=y2[:], op=ALU.add),
                                 reads=[y1, y2], writes=[yT])
                    if stage == 5:
                        for c in range(4):
                            dump(yT, yT[:, c, 0:1024], 1024)
                    S.barrier()
                    sgc.close()
                    xts = Ring([sb(sg, "mxt%d" % i, [128, D], F32) for i in range(2)])
                    x1t = Ring([sb(sg, "x1t%d" % i, [128, D], F32) for i in range(2)])
                    hns = Ring([sb(sg, "fhn%d" % i, [128, D], F32) for i in range(1)])
                    h2s = Ring([sb(sg, "fh2%d" % i, [128, D], F32) for i in range(2)])
                    h2Tf = Ring([sb(sg, "h2Tf%d" % i, [128, 8, 128], F32) for i in range(2)])
                    h2Tt = Ring([sb(sg, "h2Tt%d" % i, [128, 8, 128], BF16) for i in range(2)])
                    mjunk = sb(sg, "mjunk", [128, D], BF16)
                    msts = Ring([sb(sg, "mst%d" % i, [128, 8], F32) for i in range(4)])
                    wrt = sb(sg, "wrt", [128, 8, NE], F32)
                    brt_bc = sb(sg, "brt_bc", [128, NE], F32)
                    S.dma("sp", wrt[:], w_rt.rearrange("(kc p) n -> p kc n", p=128), writes=[wrt])
                    S.dma("sp", brt_bc[:], b_rt[0:1, :].partition_broadcast(128), writes=[brt_bc])
                    rt = {n: Ring([sb(sg, "rt_%s%d" % (n, i), [128, NE], F32) for i in range(3)]) for n in ("sc", "sel", "selm", "em", "w")}
                    m8g = Ring([sb(sg, "m8g%d" % i, [128, 8, 8], F32) for i in range(2)])
                    rsm = Ring([sb(sg, "rsm%d" % i, [128, 48], F32) for i in range(2)])
                    gps2 = Ring([psb(sg, "gps2_%d" % i, [128, 2 * 512], F32) for i in range(3)])
                    lpsr = Ring([psb(sg, "lps%d" % i, [128, 512], F32) for i in range(2)])

                    def ffn_a(t):
                        tok = slice(t * 128, (t + 1) * 128)
                        xt = xts.next(); x1 = x1t.next(); st = msts.next(); hn = hns.next(); h2 = h2s.next(); hf = h2Tf.next()
                        S.dma("sp", xt[:], x_own[tok, :], writes=[xt])
                        pz = gps2.next()
                        for n in range(2):
                            mm(pz, pz[:, n * 512:(n + 1) * 512], [(yT[:, kc, tok], wo[:, kc, n * 512:(n + 1) * 512]) for kc in range(8)], [yT, wo])
                        S.op("act", lambda e: e.activation(out=mjunk[:], in_=pz[:], func=AF.Square, accum_out=st[:, 0:1]), reads=[pz], writes=[mjunk, st])
                        S.op("act", lambda e: e.activation(out=st[:, 1:2], in_=st[:, 0:1], func=AF.Sqrt, scale=1.0 / D, bias=EPS), reads=[st], writes=[st])
                        S.op("dve", lambda e: e.reciprocal(out=st[:, 2:3], in_=st[:, 1:2]), reads=[st], writes=[st])
                        S.op("dve", lambda e: e.scalar_tensor_tensor(out=x1[:], in0=pz[:], scalar=st[:, 2:3], in1=modB[:, 0:D],
                                                                      op0=ALU.mult, op1=ALU.mult), reads=[pz, st, modB], writes=[x1])
                        S.op("dve", lambda e: e.tensor_tensor(out=x1[:], in0=x1[:], in1=xt[:], op=ALU.add), reads=[x1, xt], writes=[x1])
                        S.dma("sp", x1s[tok, :], x1[:], reads=[x1], writes=[x1s_bs[t % 4]])
                        if stage == 6 and t < 4:
                            dump(x1, x1[:, :], 1024)
                        S.op("act", lambda e: e.activation(out=mjunk[:], in_=x1[:], func=AF.Square, accum_out=st[:, 3:4]), reads=[x1], writes=[mjunk, st])
                        S.op("act", lambda e: e.activation(out=st[:, 4:5], in_=st[:, 3:4], func=AF.Sqrt, scale=1.0 / D, bias=EPS), reads=[st], writes=[st])
                        S.op("dve", lambda e: e.reciprocal(out=st[:, 5:6], in_=st[:, 4:5]), reads=[st], writes=[st])
                        S.op("dve", lambda e: e.scalar_tensor_tensor(out=hn[:], in0=x1[:], scalar=st[:, 5:6], in1=modB[:, 2 * D:3 * D],
                                                                      op0=ALU.mult, op1=ALU.mult), reads=[x1, st, modB], writes=[hn])
                        S.op("dve", lambda e: e.tensor_tensor(out=h2[:], in0=hn[:], in1=modB[:, D:2 * D], op=ALU.add), reads=[hn, modB], writes=[h2])
                        return (t, tok, h2, hf)

                    def ffn_a2(t, tok, h2, hf):
                        tp = gps2.next()
                        for kc in range(8):
                            S.op("pe", lambda e, kc=kc: e.transpose(tp[:, kc * 128:(kc + 1) * 128], h2[:, kc * 128:(kc + 1) * 128], ident_f[:]),
                                 reads=[h2, ident_f], writes=[tp], inc=(kc == 7))
                        hb16 = h2Tt.next()
                        S.op("act", lambda e: e.activation(out=hb16[:], in_=tp[:].rearrange("p (a b) -> p a b", a=8), func=AF.Copy),
                             reads=[tp], writes=[hb16])
                        S.dma("sp", h2Td[:, :, tok], hb16[:], reads=[hb16], writes=[h2Td_b])
                        S.op("act", lambda e: e.activation(out=hf[:], in_=tp[:].rearrange("p (a b) -> p a b", a=8), func=AF.Copy),
                             reads=[tp], writes=[hf])
                        lps = lpsr.next()
                        mm(lps, lps[:, 0:NE], [(hf[:, kc, :], wrt[:, kc, :]) for kc in range(8)], [hf, wrt])
                        sc_ = rt["sc"].next()
                        S.op("act", lambda e: e.activation(out=sc_[:], in_=lps[:, 0:NE], func=AF.Sigmoid), reads=[lps], writes=[sc_])
                        return sc_

                    def ffn_b(t, sc_):
                        sel = rt["sel"].next(); selm = rt["selm"].next(); em = rt["em"].next(); w_ = rt["w"].next()
                        mg = m8g.next(); sm_ = rsm.next()
                        S.op("dve", lambda e: e.tensor_tensor(out=sel[:], in0=sc_[:], in1=brt_bc[:], op=ALU.add), reads=[sc_, brt_bc], writes=[sel])
                        for g in range(8):
                            S.op("dve", lambda e, g=g: e.max(out=mg[:, g, :], in_=sel[:, g * 8:(g + 1) * 8]), reads=[sel], writes=[mg])
                        S.op("dve", lambda e: e.tensor_tensor(out=sm_[:, 0:8], in0=mg[:, :, 0], in1=mg[:, :, 1], op=ALU.add), reads=[mg], writes=[sm_])
                        S.op("dve", lambda e: e.max(out=sm_[:, 8:16], in_=sm_[:, 0:8]), reads=[sm_], writes=[sm_])
                        S.op("dve", lambda e: e.tensor_scalar(out=sm_[:, 16:24], in0=sm_[:, 0:8], scalar1=sm_[:, 11:12], scalar2=None, op0=ALU.is_ge),
                             reads=[sm_], writes=[sm_])
                        S.op("dve", lambda e: e.tensor_scalar(out=sm_[:, 24:32], in0=sm_[:, 16:24], scalar1=4.0, scalar2=-4.0, op0=ALU.mult, op1=ALU.add),
                             reads=[sm_], writes=[sm_])
                        S.op("dve", lambda e: e.tensor_tensor(out=selm[:].rearrange("p (a b) -> p a b", a=8), in0=sel[:].rearrange("p (a b) -> p a b", a=8),
                                                              in1=sm_[:, 16:24].unsqueeze(2).to_broadcast([128, 8, 8]), op=ALU.mult),
                             reads=[sel, sm_], writes=[selm])
                        S.op("dve", lambda e: e.tensor_tensor(out=selm[:].rearrange("p (a b) -> p a b", a=8), in0=selm[:].rearrange("p (a b) -> p a b", a=8),
                                                              in1=sm_[:, 24:32].unsqueeze(2).to_broadcast([128, 8, 8]), op=ALU.add),
                             reads=[selm, sm_], writes=[selm])
                        S.op("dve", lambda e: e.max(out=sm_[:, 32:40], in_=selm[:]), reads=[selm], writes=[sm_])
                        S.op("dve", lambda e: e.tensor_scalar(out=em[:], in0=selm[:], scalar1=sm_[:, 39:40], scalar2=None, op0=ALU.is_ge),
                             reads=[selm, sm_], writes=[em])
                        S.op("dve", lambda e: e.tensor_tensor(out=w_[:], in0=sc_[:], in1=em[:], op=ALU.mult), reads=[sc_, em], writes=[w_])
                        S.op("dve", lambda e: e.tensor_tensor_scan(out=em[:], data0=ones_f[:, 0:NE], data1=w_[:], initial=0.0, op0=ALU.mult, op1=ALU.add),
                             reads=[ones_f, w_], writes=[em])
                        S.op("dve", lambda e: e.tensor_scalar(out=sm_[:, 40:41], in0=em[:, NE - 1:NE], scalar1=0.4, scalar2=None, op0=ALU.mult),
                             reads=[em], writes=[sm_])
                        S.op("dve", lambda e: e.reciprocal(out=sm_[:, 41:42], in_=sm_[:, 40:41]), reads=[sm_], writes=[sm_])
                        S.op("dve", lambda e: e.tensor_scalar(out=wt_all[:, t, 0:NE], in0=w_[:], scalar1=sm_[:, 41:42], scalar2=None, op0=ALU.mult),
                             reads=[w_, sm_], writes=[wt_all])

                    pa = ffn_a(0)
                    prev_b = None
                    for t in range(16):
                        nxt = ffn_a(t + 1) if t + 1 < 16 else None
                        sc_t = ffn_a2(*pa)
                        if prev_b is not None:
                            ffn_b(*prev_b)
                        prev_b = (t, sc_t)
                        pa = nxt
                    ffn_b(*prev_b)
                    if stage == 7:
                        dump(wt_all, wt_all[:].rearrange("p a b -> p (a b)"), 16 * (NE + 1))
                    S.barrier()
        if stage in (5, 6):
            S.barrier()
            return nc, S

        with ExitStack() as se:
            h2T = sb(se, "h2T", [128, 8, NT], BF16)
            acc = sb(se, "acc", [128, 16, D], F32)
            acc_b = [Buf("acc%d" % t) for t in range(16)]
            h2T_b = [Buf("h2T_tg%d" % i) for i in range(4)]
            for i in range(4):
                S.dma("sp", h2T[:, :, i * 512:(i + 1) * 512], h2Td[:, :, i * 512:(i + 1) * 512], reads=[h2Td_b], writes=[h2T_b[i]])
            eps_ = Ring([psb(se, "eps%d" % i, [128, 512], F32) for i in range(8)])
            if stage == 7:
                for kc in range(2):
                    dump(h2T_b[0], h2T[:, kc, 0:512], 512); dump(h2T_b[1], h2T[:, kc, 512:1024], 512)
                S.barrier()
                return nc, S

            with ExitStack() as sx:
                wgus = Ring([sb(sx, "wgu%d" % i, [128, 8, 512], BF16) for i in range(3)])
                wdns = Ring([sb(sx, "wdn%d" % i, [128, 2, D], BF16) for i in range(3)])
                actTs = Ring([sb(sx, "actT%d" % i, [128, 2, 512], BF16) for i in range(3)])
                sgts = Ring([sb(sx, "sgt%d" % i, [128, 512], F32) for i in range(2)])
                xts_f = Ring([sb(sx, "oxt%d" % i, [128, D], F32) for i in range(2)])
                ots_f = Ring([sb(sx, "ot%d" % i, [128, D], F32) for i in range(2)])
                ojunk = sb(sx, "ojunk", [128, D], BF16)
                ost = Ring([sb(sx, "ost%d" % i, [128, 4], F32) for i in range(4)])
                g2t = sb(sx, "g2t", [128, D], F32)
                S.dma("sp", g2t[:], g2s[:, :], reads=[g2s_b], writes=[g2t])

                def final_tile(t):
                    tok = slice(t * 128, (t + 1) * 128)
                    xt = xts_f.next(); ot = ots_f.next(); st = ost.next()
                    S.dma("sp", xt[:], x1s[tok, :], reads=[x1s_bs[t % 4]], writes=[xt])
                    S.op("act", lambda e: e.activation(out=ojunk[:], in_=acc[:, t, :], func=AF.Square, accum_out=st[:, 0:1]),
                         reads=[acc_b[t]], writes=[ojunk, st])
                    S.op("act", lambda e: e.activation(out=st[:, 1:2], in_=st[:, 0:1], func=AF.Sqrt, scale=1.0 / D, bias=EPS), reads=[st], writes=[st])
                    S.op("dve", lambda e: e.reciprocal(out=st[:, 2:3], in_=st[:, 1:2]), reads=[st], writes=[st])
                    S.op("dve", lambda e: e.scalar_tensor_tensor(out=ot[:], in0=acc[:, t, :], scalar=st[:, 2:3], in1=g2t[:],
                                                                  op0=ALU.mult, op1=ALU.mult), reads=[acc_b[t], st, g2t], writes=[ot])
                    S.op("dve", lambda e: e.tensor_tensor(out=ot[:], in0=ot[:], in1=xt[:], op=ALU.add), reads=[ot, xt], writes=[ot])
                    S.dma("sp", out[tok, :], ot[:], reads=[ot], writes=[out_bs[t % 4]])

                n_exp = NE + 1 if stage >= 9 else (2 if stage == 8 else NE + 1)
                wbuf = {}

                def moe_load(ex):
                    wg = wgus.next(); wd = wdns.next()
                    S.dma("pool", wg[:], w_gu[ex].rearrange("(kc p) n -> p kc n", p=128), writes=[wg])
                    S.dma("pool", wd[:], w_dn[ex].rearrange("(kc p) n -> p kc n", p=128), writes=[wd])
                    wbuf[ex] = (wg, wd)

                def moe_s1(ex, tg):
                    wg = wbuf[ex][0]
                    cs = slice(tg * 512, (tg + 1) * 512)
                    G = []
                    for c in range(4):
                        ps = eps_.next()
                        mm(ps, ps[:], [(wg[:, kc, c * 128:(c + 1) * 128], h2T[:, kc, cs]) for kc in range(8)], [wg, h2T_b[tg]])
                        G.append(ps)
                    aT_ = actTs.next()
                    for j in range(2):
                        sgt = sgts.next()
                        S.op("act", lambda e, sgt=sgt, g_=G[j]: e.activation(out=sgt[:], in_=g_[:], func=AF.Silu), reads=[G[j]], writes=[sgt])
                        S.op("dve", lambda e, sgt=sgt, j=j, g_=G[2 + j]: e.tensor_tensor(out=aT_[:, j, :], in0=g_[:], in1=sgt[:], op=ALU.mult),
                             reads=[G[2 + j], sgt], writes=[aT_])
                    return aT_

                def moe_s2(ex, tg, aT_):
                    wd = wbuf[ex][1]
                    for t in range(4):
                        tile_ = tg * 4 + t
                        for n in range(2):
                            dps = eps_.next()
                            mm(dps, dps[:], [(aT_[:, j, t * 128:(t + 1) * 128], wd[:, j, n * 512:(n + 1) * 512]) for j in range(2)], [aT_, wd])
                            S.op("dve", lambda e, dps=dps, tile_=tile_, n=n: e.scalar_tensor_tensor(
                                out=acc[:, tile_, n * 512:(n + 1) * 512], in0=dps[:], scalar=wt_all[:, tile_, ex:ex + 1],
                                in1=acc[:, tile_, n * 512:(n + 1) * 512], op0=ALU.mult, op1=ALU.add),
                                reads=[dps, wt_all, acc_b[tile_]], writes=[acc_b[tile_]])

                its = [(ex, tg) for ex in range(n_exp) for tg in range(4)]
                moe_load(0)
                if n_exp > 1:
                    moe_load(1)
                for t in range(16):
                    S.op("pool", lambda e, t=t: e.memset(acc[:, t, :], 0.0), writes=[acc_b[t]])
                pend = None
                for i, (ex, tg) in enumerate(its):
                    if tg == 0 and ex >= 1 and ex + 1 < n_exp:
                        moe_load(ex + 1)
                    aT_ = moe_s1(ex, tg)
                    if pend is not None:
                        moe_s2(*pend)
                        if pend[0] == n_exp - 1:
                            for t_ in range(4):
                                final_tile(pend[1] * 4 + t_)
                    pend = (ex, tg, aT_)
                moe_s2(*pend)
                for t_ in range(4):
                    final_tile(pend[1] * 4 + t_)
                S.barrier()

        S.barrier()
    return nc, S


def _consts():
    s = np.arange(128)[:, None]; t = np.arange(128)[None, :]
    same = (s // 64) == (t // 64)
    maskA = (same & (s <= t)).astype(np.float32)
    maskB = (same & (s >= t)).astype(np.float32)
    resetm = np.ones((128, 512), np.float32); resetm[:, ::64] = 0.0
    return np.eye(128, dtype=np.float32), maskA, maskB, resetm


def make_in_maps(inp):
    f = lambda a: np.ascontiguousarray(np.asarray(a, dtype=np.float32))
    x = f(inp["x"]); ctx = f(inp["ctx"]); c = f(inp["c"]); cc = f(inp["c_ctx"])
    w_in = f(inp["w_in"][0])
    w_in_sw = w_in.copy(); w_in_sw[:, 512:1024] = w_in[:, 1024:1536]; w_in_sw[:, 1024:1536] = w_in[:, 512:1024]
    lbl = f(inp["lb_logits"])
    ws = f(inp["w_spatial"][0]); bs = f(inp["b_spatial"][0])
    gains = np.concatenate([f(inp[k][0]) for k in ("g_pre_mix", "g_post_mix", "g_pre_ffn", "g_post_ffn")])[None, :]
    w_gu = np.concatenate([f(inp["w_expert_gu"][0]), f(inp["w_shared_gu"])], axis=0)
    w_dn = np.concatenate([f(inp["w_expert_down"][0]), f(inp["w_shared_down"])], axis=0)
    ident, maskA, maskB, resetm = _consts()
    shared = dict(w_ada=f(inp["w_ada"][0]), b_ada=f(inp["b_ada"]), gains=f(gains), gout=f(inp["g_hgrn_out"][0][:, None]),
                  lng=f(inp["cm_ln_g"]), lnb=f(inp["cm_ln_b"]), w_a=f(inp["w_branch_a"][0]), w_b=f(inp["w_branch_b"][0]),
                  w_o=f(inp["w_out"][0]), w_rt=f(inp["w_router"][0]), b_rt=f(inp["b_router"]), w_gu=w_gu, w_dn=w_dn,
                  ident=ident, maskA=maskA, maskB=maskB, resetm=resetm)
    maps = []
    for i in range(8):
        b, half = i // 2, i % 2
        m = dict(shared)
        if half == 0:
            xs = x[b]; m["ctx"] = f(ctx[b]); m["w_in"] = w_in; dirs = [0, 1]; ws_l, bs_l = ws, bs
        else:
            xs = x[b, ::-1]; m["ctx"] = f(ctx[b, ::-1]); m["w_in"] = w_in_sw; dirs = [1, 0]
            ws_l, bs_l = ws[:, ::-1, ::-1], bs[::-1, :]
        m["x_own"] = f(xs[0:NT]); m["x_oth"] = f(xs[NT:2 * NT])
        m["cvec"] = f(np.concatenate([c[b].reshape(8, 128).T, cc.reshape(8, 128).T], axis=1))
        l4 = lbl[dirs].reshape(2, 2, 4, 128)
        m["lbl"] = f(l4.transpose(3, 0, 1, 2).reshape(128, 16))
        m["wsT"] = f(ws_l.transpose(2, 0, 1).reshape(128, 512))
        m["bsp"] = f(bs_l.T.reshape(1, 512))
        maps.append(m)
    return maps


_CACHE = {}


def kernel(**inputs):
    if "nc" not in _CACHE:
        _CACHE["nc"] = build_program()[0]
    nc = _CACHE["nc"]
    maps = make_in_maps(inputs)
    res = run_bass_kernel_spmd(nc, maps, core_ids=list(range(8)))
    B, T = inputs["x"].shape[0], inputs["x"].shape[1]
    out = np.empty((B, T, D), np.float32)
    for i in range(8):
        b, half = i // 2, i % 2
        r = np.asarray(res.results[i]["out"], dtype=np.float32)
        if half == 0:
            out[b, 0:NT] = r
        else:
            out[b, NT:2 * NT] = r[::-1]
    return out
```

```python
import os
import numpy as np
from contextlib import ExitStack
import concourse.bass as bass
import concourse.mybir as mybir
from concourse.bass_utils import run_bass_kernel_spmd

F32 = mybir.dt.float32
BF16 = mybir.dt.bfloat16
AF = mybir.ActivationFunctionType
ALU = mybir.AluOpType

NT = 2048
D = 1024
EPS = 1e-6
NE = 64


class Tok:
    __slots__ = ("sem", "val", "eng")

    def __init__(self, sem, val, eng):
        self.sem, self.val, self.eng = sem, val, eng


class Buf:
    def __init__(self, name):
        self.name = name
        self.w = None
        self.r = []
        self.dsem = None
        self.dcnt = 0
        self.excl = False


class TT:
    def __init__(self, t, name):
        self.t = t
        self.b = Buf(name)

    def __getitem__(self, k):
        return self.t[k]


class Sched:
    def __init__(self, nc, es):
        self.nc, self.es = nc, es
        self.eng = {"pe": nc.tensor, "act": nc.scalar, "dve": nc.vector, "pool": nc.gpsimd, "sp": nc.sync}
        self.esem, self.ecnt = {}, {}
        for e in ("pe", "act", "dve", "pool"):
            self.esem[e] = es.enter_context(nc.semaphore("sem_" + e))
            self.ecnt[e] = 0
        self.seen = {e: {} for e in self.eng}
        self.dsems = []
        self.nwaits = 0
        self.ninstr = {e: 0 for e in self.eng}

    def _wait(self, e, tok):
        key = id(tok.sem)
        if self.seen[e].get(key, 0) >= tok.val:
            return
        if tok.eng != "dma":
            assert tok.val <= self.ecnt[tok.eng], "wait on future inc (%s on %s)" % (e, tok.eng)
        self.eng[e].wait_ge(tok.sem, tok.val)
        self.seen[e][key] = tok.val
        self.nwaits += 1

    def _deps(self, e, reads, writes):
        same_ok = (e == "pe") or os.environ.get("KD_SAMEENG", "1") == "0"
        for b in reads:
            if b.w is not None:
                self._wait(e, b.w)
            if b.excl:
                for t in b.r:
                    if t.eng != e:
                        self._wait(e, t)
        for b in writes:
            if b.w is not None and not (same_ok and b.w.eng == e):
                self._wait(e, b.w)
            for t in b.r:
                if not (same_ok and t.eng == e):
                    self._wait(e, t)

    @staticmethod
    def _compact(toks):
        best = {}
        for t in toks:
            k = id(t.sem)
            if k not in best or best[k].val < t.val:
                best[k] = t
        return list(best.values())

    def _mark(self, tok, reads, writes):
        for b in reads:
            b.r.append(tok)
            if len(b.r) > 32:
                b.r = self._compact(b.r)
        for b in writes:
            b.w = tok
            b.r = []

    def op(self, e, fn, reads=(), writes=(), inc=True):
        reads = [x.b if isinstance(x, TT) else x for x in reads]
        writes = [x.b if isinstance(x, TT) else x for x in writes]
        self._deps(e, reads, writes)
        ins = fn(self.eng[e])
        self.ninstr[e] += 1
        if inc:
            self.ecnt[e] += 1
            ins.then_inc(self.esem[e], 1)
            tok = Tok(self.esem[e], self.ecnt[e], e)
        else:
            tok = Tok(self.esem[e], self.ecnt[e] + 1, e)
        self._mark(tok, reads, writes)
        return ins

    def dma(self, e, out, in_, reads=(), writes=()):
        reads = [x.b if isinstance(x, TT) else x for x in reads]
        writes = [x.b if isinstance(x, TT) else x for x in writes]
        self._deps(e, reads, writes)
        owner = writes[0] if writes else reads[0]
        if owner.dsem is None:
            owner.dsem = self.es.enter_context(self.nc.semaphore("ds%d_%s" % (len(self.dsems), owner.name)))
            self.dsems.append(owner)
        ins = self.eng[e].dma_start(out=out, in_=in_)
        owner.dcnt += 16
        ins.then_inc(owner.dsem, 16)
        self.ninstr[e] += 1
        self._mark(Tok(owner.dsem, owner.dcnt, "dma"), reads, writes)
        return ins

    def barrier(self):
        toks = [Tok(self.esem[e], self.ecnt[e], e) for e in self.esem if self.ecnt[e] > 0]
        toks += [Tok(b.dsem, b.dcnt, "dma") for b in self.dsems]
        for e in self.eng:
            for t in toks:
                if t.eng != e:
                    self._wait(e, t)


class Ring:
    def __init__(self, items):
        self.items, self.i = items, 0

    def next(self):
        it = self.items[self.i % len(self.items)]
        self.i += 1
        return it


def build_program(stage=99, dbg_w=0):
    nc = bass.Bass("TRN2", target_bir_lowering=False)

    def din(name, shape):
        return nc.dram_tensor(name, list(shape), F32, kind="ExternalInput").ap()

    x_own = din("x_own", [NT, D]); x_oth = din("x_oth", [NT, D]); ctx = din("ctx", [256, D])
    cvec = din("cvec", [128, 16]); w_ada = din("w_ada", [D, 6 * D]); b_ada = din("b_ada", [1, 6 * D])
    gains = din("gains", [1, 4 * D]); w_in = din("w_in", [D, 5632]); lbl = din("lbl", [128, 16])
    gout = din("gout", [128, 1]); lng = din("lng", [1, 512]); lnb = din("lnb", [1, 512])
    wsT = din("wsT", [128, 512]); bsp = din("bsp", [1, 512])
    w_a = din("w_a", [512, D]); w_b = din("w_b", [512, D]); w_o = din("w_o", [D, D])
    w_rt = din("w_rt", [D, NE]); b_rt = din("b_rt", [1, NE])
    w_gu = din("w_gu", [NE + 1, D, 512]); w_dn = din("w_dn", [NE + 1, 256, D])
    ident_d = din("ident", [128, 128]); maskA_d = din("maskA", [128, 128]); maskB_d = din("maskB", [128, 128])
    resetm_d = din("resetm", [128, 512])
    out = nc.dram_tensor("out", [NT, D], F32, kind="ExternalOutput").ap()
    x1s = nc.dram_tensor("x1s", [NT, D], F32).ap()
    h2Td = nc.dram_tensor("h2Td", [128, 8, NT], BF16).ap()
    h2Td_b = Buf("h2Td")
    g2s = nc.dram_tensor("g2s", [128, D], F32).ap()
    g2s_b = Buf("g2s")
    dbg = nc.dram_tensor("dbg", [128, dbg_w], F32, kind="ExternalOutput").ap() if dbg_w else None
    x1s_bs = [Buf("x1s%d" % i) for i in range(4)]; out_bs = [Buf("out%d" % i) for i in range(4)]; dbg_b = Buf("dbg")

    w_in_v = w_in.rearrange("(kc p) n -> p kc n", p=128)
    w_ada_v = w_ada.rearrange("(kc p) n -> p kc n", p=128)

    with ExitStack() as es:
        S = Sched(nc, es)

        uid = [0]

        def sb(scope, name, shape, dt):
            uid[0] += 1
            return TT(scope.enter_context(nc.sbuf_tensor("s%d_%s" % (uid[0], name), list(shape), dt)), name)

        def psb(scope, name, shape, dt):
            uid[0] += 1
            t = TT(scope.enter_context(nc.psum_tensor("p%d_%s" % (uid[0], name), list(shape), dt)), name)
            t.b.excl = True
            return t

        def mm(ps, out_ap, pairs, reads, first=True, last=True, inc=None):
            n = len(pairs)
            for i, (l, r) in enumerate(pairs):
                fin = (i == n - 1)
                S.op("pe", lambda e, l=l, r=r, i=i, fin=fin: e.matmul(out_ap, lhsT=l, rhs=r, start=(first and i == 0),
                                                                     stop=(last and fin)),
                     reads=reads, writes=[ps], inc=(fin if inc is None else (inc and fin)))

        dbg_col = [0]

        def dump(tt, ap, w):
            if dbg is None:
                return
            S.dma("pool", dbg[:, dbg_col[0]:dbg_col[0] + w], ap, reads=[tt], writes=[dbg_b])
            dbg_col[0] += w

        ident_bf = sb(es, "ident_bf", [128, 128], BF16)
        ident_f = sb(es, "ident_f", [128, 128], F32)
        maskA = sb(es, "maskA", [128, 128], BF16)
        maskB = sb(es, "maskB", [128, 128], BF16)
        resetm = sb(es, "resetm", [128, 512], F32)
        ones_bf = sb(es, "ones_bf", [128, 128], BF16)
        ones_f = sb(es, "ones_f", [128, 512], F32)
        mod = sb(es, "mod", [128, 2 * D], F32)
        wt_all = sb(es, "wt_all", [128, 16, NE + 1], F32)
        lbt = sb(es, "lbt", [128, 16], F32)
        lbv = sb(es, "lbv", [128, 8], F32)
        oml = sb(es, "oml", [128, 8], F32)
        noml = sb(es, "noml", [128, 8], F32)
        gout_t = sb(es, "gout_t", [128, 1], F32)
        screp = sb(es, "screp", [128, 16, 128], BF16)

        S.dma("pool", ident_bf[:], ident_d[:, :], writes=[ident_bf])
        S.dma("sp", ident_f[:], ident_d[:, :], writes=[ident_f])
        S.dma("pool", maskA[:], maskA_d[:, :], writes=[maskA])
        S.dma("pool", maskB[:], maskB_d[:, :], writes=[maskB])
        S.dma("sp", resetm[:], resetm_d[:, :], writes=[resetm])
        S.dma("sp", lbt[:], lbl[:, :], writes=[lbt])
        S.dma("sp", gout_t[:], gout[:, :], writes=[gout_t])
        S.op("dve", lambda e: e.memset(ones_bf[:], 1.0), writes=[ones_bf])
        S.op("dve", lambda e: e.memset(ones_f[:], 1.0), writes=[ones_f])
        S.op("dve", lambda e: e.memset(wt_all[:], 1.0), writes=[wt_all])
        lb3 = lbt[:].rearrange("p (d s h) -> p d s h", d=2, s=2)
        S.op("dve", lambda e: e.tensor_tensor(out=oml[:].rearrange("p (d h) -> p d h", d=2), in0=lb3[:, :, 0, :],
                                              in1=lb3[:, :, 1, :], op=ALU.subtract), reads=[lbt], writes=[oml])
        S.op("act", lambda e: e.activation(out=lbv[:], in_=oml[:], func=AF.Sigmoid), reads=[oml], writes=[lbv])
        S.op("dve", lambda e: e.tensor_scalar(out=oml[:], in0=lbv[:], scalar1=-1.0, scalar2=1.0, op0=ALU.mult, op1=ALU.add),
             reads=[lbv], writes=[oml])
        S.op("dve", lambda e: e.tensor_scalar(out=noml[:], in0=lbv[:], scalar1=1.0, scalar2=-1.0, op0=ALU.mult, op1=ALU.add),
             reads=[lbv], writes=[noml])

        def norm_pool(scope):
            np_ = dict(
                xts=Ring([sb(scope, "xt%d" % i, [128, D], F32) for i in range(3)]),
                junk=sb(scope, "junk", [128, D], BF16),
                hns=Ring([sb(scope, "hn%d" % i, [128, D], F32) for i in range(2)]),
                hbs=Ring([sb(scope, "hb%d" % i, [128, D], BF16) for i in range(3)]),
                stats=Ring([sb(scope, "nst%d" % i, [128, 4], F32) for i in range(4)]),
                tps=Ring([psb(scope, "tps%d" % i, [128, 1024], BF16) for i in range(2)]),
            )
            return np_

        def norm_a(np_, src_rows, src_bufs, Aap, Bap, mods):
            xt = np_["xts"].next(); hn = np_["hns"].next(); hb = np_["hbs"].next(); st = np_["stats"].next()
            junk = np_["junk"]
            S.dma("sp", xt[:], src_rows, reads=src_bufs, writes=[xt])
            S.op("act", lambda e: e.activation(out=junk[:], in_=xt[:], func=AF.Square, accum_out=st[:, 0:1]),
                 reads=[xt], writes=[junk, st])
            S.op("act", lambda e: e.activation(out=st[:, 1:2], in_=st[:, 0:1], func=AF.Sqrt, scale=1.0 / D, bias=EPS),
                 reads=[st], writes=[st])
            S.op("dve", lambda e: e.reciprocal(out=st[:, 2:3], in_=st[:, 1:2]), reads=[st], writes=[st])
            S.op("dve", lambda e: e.scalar_tensor_tensor(out=hn[:], in0=xt[:], scalar=st[:, 2:3], in1=Aap,
                                                          op0=ALU.mult, op1=ALU.mult), reads=[xt, st] + mods, writes=[hn])
            S.op("dve", lambda e: e.tensor_tensor(out=hb[:], in0=hn[:], in1=Bap, op=ALU.add),
                 reads=[hn] + mods, writes=[hb])
            return hb

        def norm_b(np_, hb, hT_dst, hT_tt):
            tp = np_["tps"].next()
            for kc in range(8):
                S.op("pe", lambda e, kc=kc: e.transpose(tp[:, kc * 128:(kc + 1) * 128], hb[:, kc * 128:(kc + 1) * 128],
                                                        ident_bf[:]),
                     reads=[hb, ident_bf], writes=[tp], inc=(kc == 7))
            S.op("act", lambda e: e.activation(out=hT_dst, in_=tp[:].rearrange("p (a b) -> p a b", a=8), func=AF.Copy),
                 reads=[tp], writes=[hT_tt])

        def norm_seq(np_, items):
            prev = None
            for it in items:
                hb = norm_a(np_, *it[:5])
                if prev is not None:
                    norm_b(np_, prev[0], prev[1], prev[2])
                prev = (hb, it[5], it[6])
            norm_b(np_, prev[0], prev[1], prev[2])

        with ExitStack() as sm:
            hT_own = sb(sm, "hT_own", [128, 8, NT], BF16)
            aT = sb(sm, "aT", [128, 4, NT], BF16)
            sw = ExitStack()
            wfA = sb(sw, "wfA", [128, 8, 512], BF16)
            wfB = sb(sw, "wfB", [128, 8, 512], BF16)
            wi = sb(sw, "wi", [128, 8, 512], BF16)
            Sst = [sb(sw, "SstA", [128, 4, 128], F32), sb(sw, "SstB", [128, 4, 128], F32)]

            with ExitStack() as s12:
                modc = sb(s12, "modc", [128, 2 * D], F32)
                with ExitStack() as p1:
                    cv = sb(p1, "cv", [128, 16], F32)
                    scv = sb(p1, "scv", [128, 16], F32)
                    gns = sb(p1, "gns", [128, D], F32)
                    wada = [sb(p1, "wada%d" % i, [128, 8, 512], BF16) for i in range(2)]
                    bada = [sb(p1, "bada%d" % i, [128, 512], F32) for i in range(2)]
                    pp = Ring([psb(p1, "p1ps%d" % i, [128, 512], F32) for i in range(4)])
                    S.dma("sp", cv[:], cvec[:, :], writes=[cv])
                    S.dma("sp", gns[:], gains[0:1, 0:D].partition_broadcast(128), writes=[gns])
                    S.op("act", lambda e: e.activation(out=scv[:], in_=cv[:], func=AF.Silu), reads=[cv], writes=[scv])
                    S.op("dve", lambda e: e.tensor_copy(out=screp[:], in_=scv[:].unsqueeze(2).to_broadcast([128, 16, 128])),
                         reads=[scv], writes=[screp])
                    for j in range(4):
                        wb_, bb_ = wada[j % 2], bada[j % 2]
                        S.dma("pool", wb_[:], w_ada_v[:, :, j * 512:(j + 1) * 512], writes=[wb_])
                        S.dma("sp", bb_[:], b_ada[0:1, j * 512:(j + 1) * 512].partition_broadcast(128), writes=[bb_])
                        ps = pp.next()
                        mm(ps, ps[:], [(screp[:, kc, :], wb_[:, kc, :]) for kc in range(8)], [screp, wb_])
                        S.op("dve", lambda e, ps=ps, bb_=bb_, j=j: e.tensor_tensor(out=mod[:, j * 512:(j + 1) * 512], in0=ps[:],
                                                                                    in1=bb_[:], op=ALU.add),
                             reads=[ps, bb_], writes=[mod])
                        if j < 4:
                            ps = pp.next()
                            mm(ps, ps[:], [(screp[:, 8 + kc, :], wb_[:, kc, :]) for kc in range(8)], [screp, wb_])
                            S.op("dve", lambda e, ps=ps, bb_=bb_, j=j: e.tensor_tensor(out=modc[:, j * 512:(j + 1) * 512],
                                                                                        in0=ps[:], in1=bb_[:], op=ALU.add),
                                 reads=[ps, bb_], writes=[modc])

                    def scale1p(dst, lo, g0):
                        S.op("dve", lambda e: e.scalar_tensor_tensor(out=dst[:, lo:lo + D], in0=dst[:, lo:lo + D], scalar=1.0,
                                                                      in1=gns[:, g0 * D:(g0 + 1) * D], op0=ALU.add, op1=ALU.mult),
                             reads=[dst, gns], writes=[dst])

                    def scaleg(dst, lo, g0):
                        S.op("dve", lambda e: e.tensor_tensor(out=dst[:, lo:lo + D], in0=dst[:, lo:lo + D],
                                                              in1=gns[:, g0 * D:(g0 + 1) * D], op=ALU.mult),
                             reads=[dst, gns], writes=[dst])
                    S.dma("pool", wfA[:], w_in_v[:, :, 512:1024], writes=[wfA])
                    S.dma("pool", wfB[:], w_in_v[:, :, 1024:1536], writes=[wfB])
                    S.dma("pool", wi[:], w_in_v[:, :, 1536:2048], writes=[wi])
                    scale1p(mod, 1 * D, 0); scale1p(modc, 1 * D, 0)
                    if stage == 1:
                        dump(mod, mod[:, 0:2 * D], 2 * D); dump(modc, modc[:, :], 2 * D)
                    S.barrier()
                if stage == 1:
                    S.barrier()
                    return nc, S

                with ExitStack() as p2:
                    npl = norm_pool(p2)
                    tps = npl["tps"]
                    hTg = sb(p2, "hTg", [128, 8, 512], BF16)
                    vg = sb(p2, "vg", [128, 4, 512], BF16)
                    ktT = sb(p2, "ktT", [128, 4, 512], BF16)
                    ktoks = Ring([sb(p2, "ktok%d" % i, [128, 512], BF16) for i in range(2)])
                    carry = sb(p2, "carry", [128, 4], F32)
                    tots = sb(p2, "tots", [128, 4], F32)
                    tf = {n: Ring([sb(p2, "p2%s%d" % (n, i), [128, 512], F32) for i in range(4 if n == "sig" else 2)])
                          for n in ("sig", "lf", "k", "pin", "pex")}
                    S0ps = [psb(p2, "S0psA", [128, 512], F32), psb(p2, "S0psB", [128, 512], F32)]
                    pr = Ring([psb(p2, "p2ps%d" % i, [128, 512], F32) for i in range(4)])
                    S.op("dve", lambda e: e.memset(carry[:], 0.0), writes=[carry])
                    nB = [0]
                    totB = 18

                    def gate_chain_state(z, ncol, d, h, mode):
                        lf = tf["lf"].next(); kk = tf["k"].next(); pin = tf["pin"].next()
                        pex = tf["pex"].next()
                        c = d * 4 + h
                        sg = z
                        S.op("act", lambda e: e.activation(out=lf[:, :ncol], in_=sg[:, :ncol], func=AF.Ln, scale=oml[:, c:c + 1],
                                                           bias=lbv[:, c:c + 1]), reads=[sg, oml, lbv], writes=[lf])
                        S.op("dve", lambda e: e.tensor_scalar(out=kk[:, :ncol], in0=sg[:, :ncol], scalar1=noml[:, c:c + 1],
                                                               scalar2=oml[:, c:c + 1], op0=ALU.mult, op1=ALU.add),
                             reads=[sg, noml, oml], writes=[kk])
                        if mode == "B":
                            S.op("dve", lambda e: e.tensor_tensor_scan(out=pin[:, :ncol], data0=ones_f[:, :ncol], data1=lf[:, :ncol],
                                                                        initial=carry[:, h:h + 1], op0=ALU.mult, op1=ALU.add),
                                 reads=[ones_f, lf, carry], writes=[pin])
                            S.op("act", lambda e: e.activation(out=carry[:, h:h + 1], in_=pin[:, ncol - 1:ncol], func=AF.Copy),
                                 reads=[pin], writes=[carry])
                            S.op("dve", lambda e: e.tensor_tensor(out=pex[:, :ncol], in0=pin[:, :ncol], in1=lf[:, :ncol],
                                                                  op=ALU.subtract), reads=[pin, lf], writes=[pex])
                            S.op("act", lambda e: e.activation(out=pex[:, :ncol], in_=pex[:, :ncol], func=AF.Exp),
                                 reads=[pex], writes=[pex])
                        else:
                            S.op("dve", lambda e: e.tensor_tensor_scan(out=pin[:, :ncol], data0=ones_f[:, :ncol], data1=lf[:, :ncol],
                                                                        initial=0.0, op0=ALU.mult, op1=ALU.add),
                                 reads=[ones_f, lf], writes=[pin])
                            S.op("act", lambda e: e.activation(out=tots[:, h:h + 1], in_=pin[:, ncol - 1:ncol], func=AF.Copy),
                                 reads=[pin], writes=[tots])
                            S.op("act", lambda e: e.activation(out=pex[:, :ncol], in_=pin[:, :ncol], func=AF.Exp, scale=-1.0,
                                                               bias=tots[:, h:h + 1]), reads=[pin, tots], writes=[pex])
                        S.op("dve", lambda e: e.tensor_tensor(out=ktT[:, h, :ncol], in0=kk[:, :ncol], in1=pex[:, :ncol], op=ALU.mult),
                             reads=[kk, pex], writes=[ktT])

                    def state_accum(ntile, d):
                        for t in range(ntile):
                            tp = tps.next(); kt = ktoks.next()
                            for h in range(4):
                                S.op("pe", lambda e, h=h, t=t, tp=tp: e.transpose(tp[:, h * 128:(h + 1) * 128],
                                                                                   ktT[:, h, t * 128:(t + 1) * 128], ident_bf[:]),
                                     reads=[ktT, ident_bf], writes=[tp], inc=(h == 3))
                            S.op("act", lambda e, tp=tp, kt=kt: e.activation(out=kt[:], in_=tp[:, 0:512], func=AF.Copy),
                                 reads=[tp], writes=[kt])
                            for h in range(4):
                                if d == 1:
                                    st_ = (nB[0] == 0); nB[0] += 1; sp_ = (nB[0] > totB * 4 - 4)
                                else:
                                    st_ = (t == 0 and h == 0); sp_ = (t == ntile - 1)
                                S.op("pe", lambda e, h=h, t=t, kt=kt, st_=st_, sp_=sp_: e.matmul(
                                    S0ps[d][:, h * 128:(h + 1) * 128], lhsT=kt[:, h * 128:(h + 1) * 128],
                                    rhs=vg[:, t, h * 128:(h + 1) * 128], start=st_, stop=sp_, skip_group_check=True),
                                    reads=[kt, vg], writes=[S0ps[d]], inc=True)

                    for g in range(4):
                        norm_seq(npl, [(x_oth[(g * 4 + t) * 128:(g * 4 + t + 1) * 128, :], [], mod[:, D:2 * D], mod[:, 0:D], [mod],
                                        hTg[:, :, t * 128:(t + 1) * 128], hTg) for t in range(4)])
                        for t in range(4):
                            ps = pr.next()
                            mm(ps, ps[:], [(hTg[:, kc, t * 128:(t + 1) * 128], wi[:, kc, :]) for kc in range(8)], [hTg, wi])
                            S.op("act", lambda e, ps=ps, t=t: e.activation(out=vg[:, t, :], in_=ps[:], func=AF.Copy), reads=[ps], writes=[vg])
                        sgs_ = []
                        for h in range(4):
                            ps = pr.next()
                            mm(ps, ps[:], [(wfB[:, kc, h * 128:(h + 1) * 128], hTg[:, kc, :]) for kc in range(8)], [hTg, wfB])
                            sg = tf["sig"].next()
                            S.op("act", lambda e, ps=ps, sg=sg: e.activation(out=sg[:], in_=ps[:], func=AF.Sigmoid), reads=[ps], writes=[sg])
                            sgs_.append(sg)
                        for h in range(4):
                            gate_chain_state(sgs_[h], 512, 1, h, "B")
                        state_accum(4, 1)
                    norm_seq(npl, [(ctx[t * 128:(t + 1) * 128, :], [], modc[:, D:2 * D], modc[:, 0:D], [modc],
                                    hTg[:, :, t * 128:(t + 1) * 128], hTg) for t in range(2)])
                    for t in range(2):
                        ps = pr.next()
                        mm(ps, ps[:], [(hTg[:, kc, t * 128:(t + 1) * 128], wi[:, kc, :]) for kc in range(8)], [hTg, wi])
                        S.op("act", lambda e, ps=ps, t=t: e.activation(out=vg[:, t, :], in_=ps[:], func=AF.Copy), reads=[ps], writes=[vg])
                    for (wf_, d_, mode_) in ((wfB, 1, "B"), (wfA, 0, "A")):
                        sgs_ = []
                        for h in range(4):
                            ps = pr.next()
                            mm(ps, ps[:, 0:256], [(wf_[:, kc, h * 128:(h + 1) * 128], hTg[:, kc, 0:256]) for kc in range(8)], [hTg, wf_])
                            sg = tf["sig"].next()
                            S.op("act", lambda e, ps=ps, sg=sg: e.activation(out=sg[:, 0:256], in_=ps[:, 0:256], func=AF.Sigmoid), reads=[ps], writes=[sg])
                            sgs_.append(sg)
                        for h in range(4):
                            gate_chain_state(sgs_[h], 256, d_, h, mode_)
                        state_accum(2, d_)
                    assert nB[0] == totB * 4
                    for d in range(2):
                        S.op("act", lambda e, d=d: e.activation(out=Sst[d][:].rearrange("p a b -> p (a b)"), in_=S0ps[d][:], func=AF.Copy),
                             reads=[S0ps[d]], writes=[Sst[d]])
                    if stage == 2:
                        dump(Sst[0], Sst[0][:].rearrange("p a b -> p (a b)"), 512)
                        dump(Sst[1], Sst[1][:].rearrange("p a b -> p (a b)"), 512)
                    S.barrier()
                if stage == 2:
                    S.barrier()
                    return nc, S

            with ExitStack() as s3:
                npl = norm_pool(s3)
                norm_seq(npl, [(x_own[t * 128:(t + 1) * 128, :], [], mod[:, D:2 * D], mod[:, 0:D], [mod],
                                hT_own[:, :, t * 128:(t + 1) * 128], hT_own) for t in range(16)])
                S.barrier()

            with ExitStack() as sh:
                v_own = sb(sh, "v_own", [128, 16, 512], BF16)
                hps = Ring([psb(sh, "hps%d" % i, [128, 512], F32) for i in range(6)])
                tps = Ring([psb(sh, "tpsh%d" % i, [128, 1024], BF16) for i in range(2)])
                for t in range(16):
                    ps = hps.next()
                    mm(ps, ps[:], [(hT_own[:, kc, t * 128:(t + 1) * 128], wi[:, kc, :]) for kc in range(8)], [hT_own, wi])
                    S.op("act", lambda e, ps=ps, t=t: e.activation(out=v_own[:, t, :], in_=ps[:], func=AF.Copy), reads=[ps], writes=[v_own])
                qh = [sb(sh, "qh%d" % d, [128, NT], BF16) for d in range(2)]
                kh = [sb(sh, "kh%d" % d, [128, NT], BF16) for d in range(2)]
                ktok = [sb(sh, "ktokh%d" % d, [128, 16, 128], BF16) for d in range(2)]
                oacc = sb(sh, "oacc", [128, NT], F32)
                gT = sb(sh, "gT", [128, NT], BF16)
                expT = [sb(sh, "expT%d" % d, [128, 33], F32) for d in range(2)]
                for d in range(2):
                    S.op("dve", lambda e, d=d: e.memset(expT[d][:, 32:33], 1.0), writes=[expT[d]])
                wqs = Ring([sb(sh, "wq%d" % i, [128, 8, 128], BF16) for i in range(2)])
                wgs = Ring([sb(sh, "wg%d" % i, [128, 8, 128], BF16) for i in range(2)])
                tf = {n: Ring([sb(sh, "h%s%d" % (n, i), [128, 512], F32) for i in range(2)])
                      for n in ("qf", "sig", "lf", "k", "pin", "e1", "e2")}
                Sfb = [[sb(sh, "Sf%d_%d" % (d, i), [128, 128], F32) for i in range(2)] for d in range(2)]
                sfi = [0, 0]
                Sbf = [Ring([sb(sh, "Sbf%d_%d" % (d, i), [128, 128], BF16) for i in range(3)]) for d in range(2)]
                ATs = Ring([sb(sh, "ATs%d" % i, [128, 128], BF16) for i in range(4)])
                og_sq = sb(sh, "og_sq", [128, 512], BF16)
                og_sd = sb(sh, "og_sd", [128, 512], F32)
                og_t1 = sb(sh, "og_t1", [128, 512], F32)

                for h in range(4):
                    hc = slice(h * 128, (h + 1) * 128)
                    wq_h = wqs.next(); wg_h = wgs.next()
                    S.dma("pool", wq_h[:], w_in_v[:, :, h * 128:(h + 1) * 128], writes=[wq_h])
                    S.dma("pool", wg_h[:], w_in_v[:, :, 2048 + h * 128:2048 + (h + 1) * 128], writes=[wg_h])
                    for tg in range(4):
                        cs = slice(tg * 512, (tg + 1) * 512)
                        ps = hps.next()
                        mm(ps, ps[:], [(wq_h[:, kc, :], hT_own[:, kc, cs]) for kc in range(8)], [wq_h, hT_own])
                        qf = tf["qf"].next()
                        S.op("act", lambda e, ps=ps, qf=qf: e.activation(out=qf[:], in_=ps[:], func=AF.Silu), reads=[ps], writes=[qf])
                        ps = hps.next()
                        mm(ps, ps[:], [(wg_h[:, kc, :], hT_own[:, kc, cs]) for kc in range(8)], [wg_h, hT_own])
                        S.op("act", lambda e, ps=ps, cs=cs: e.activation(out=gT[:, cs], in_=ps[:], func=AF.Silu), reads=[ps], writes=[gT])
                        sgd = []
                        for d in range(2):
                            wf = wfA if d == 0 else wfB
                            ps = hps.next()
                            mm(ps, ps[:], [(wf[:, kc, hc], hT_own[:, kc, cs]) for kc in range(8)], [wf, hT_own])
                            sg = tf["sig"].next()
                            S.op("act", lambda e, ps=ps, sg=sg: e.activation(out=sg[:], in_=ps[:], func=AF.Sigmoid), reads=[ps], writes=[sg])
                            sgd.append(sg)
                        for d in range(2):
                            c = d * 4 + h
                            sg = sgd[d]
                            lf = tf["lf"].next(); kk = tf["k"].next(); pin = tf["pin"].next()
                            e1 = tf["e1"].next(); e2 = tf["e2"].next()
                            S.op("act", lambda e, sg=sg, lf=lf, c=c: e.activation(out=lf[:], in_=sg[:], func=AF.Ln, scale=oml[:, c:c + 1],
                                                                                  bias=lbv[:, c:c + 1]), reads=[sg, oml, lbv], writes=[lf])
                            S.op("dve", lambda e, sg=sg, kk=kk, c=c: e.tensor_scalar(out=kk[:], in0=sg[:], scalar1=noml[:, c:c + 1],
                                                                                      scalar2=oml[:, c:c + 1], op0=ALU.mult, op1=ALU.add),
                                 reads=[sg, noml, oml], writes=[kk])
                            S.op("dve", lambda e, pin=pin, lf=lf: e.tensor_tensor_scan(out=pin[:], data0=resetm[:], data1=lf[:], initial=0.0,
                                                                                        op0=ALU.mult, op1=ALU.add),
                                 reads=[resetm, lf], writes=[pin])
                            S.op("act", lambda e, pin=pin, d=d, tg=tg: e.activation(out=expT[d][:, tg * 8:(tg + 1) * 8], in_=pin[:, 63::64],
                                                                                    func=AF.Exp), reads=[pin], writes=[expT[d]])
                            if d == 0:
                                S.op("act", lambda e, pin=pin, e1=e1: e.activation(out=e1[:], in_=pin[:], func=AF.Exp), reads=[pin], writes=[e1])
                                S.op("act", lambda e, pin=pin, e2=e2: e.activation(out=e2[:], in_=pin[:], func=AF.Exp, scale=-1.0),
                                     reads=[pin], writes=[e2])
                            else:
                                S.op("dve", lambda e, pin=pin, lf=lf: e.tensor_tensor(out=pin[:], in0=pin[:], in1=lf[:], op=ALU.subtract),
                                     reads=[pin, lf], writes=[pin])
                                S.op("act", lambda e, pin=pin, e1=e1: e.activation(out=e1[:], in_=pin[:], func=AF.Exp, scale=-1.0),
                                     reads=[pin], writes=[e1])
                                S.op("act", lambda e, pin=pin, e2=e2: e.activation(out=e2[:], in_=pin[:], func=AF.Exp), reads=[pin], writes=[e2])
                            S.op("dve", lambda e, qf=qf, e1=e1, d=d, cs=cs: e.tensor_tensor(out=qh[d][:, cs], in0=qf[:], in1=e1[:], op=ALU.mult),
                                 reads=[qf, e1], writes=[qh[d]])
                            S.op("dve", lambda e, kk=kk, e2=e2, d=d, cs=cs: e.tensor_tensor(out=kh[d][:, cs], in0=kk[:], in1=e2[:], op=ALU.mult),
                                 reads=[kk, e2], writes=[kh[d]])
                    for d in range(2):
                        for t4 in range(4):
                            tp = tps.next()
                            for j in range(4):
                                t = t4 * 4 + j
                                S.op("pe", lambda e, tp=tp, j=j, t=t, d=d: e.transpose(tp[:, j * 128:(j + 1) * 128],
                                                                                        kh[d][:, t * 128:(t + 1) * 128], ident_bf[:]),
                                     reads=[kh[d], ident_bf], writes=[tp], inc=(j == 3))
                            S.op("act", lambda e, tp=tp, d=d, t4=t4: e.activation(out=ktok[d][:, t4 * 4:(t4 + 1) * 4, :],
                                                                                  in_=tp[:, 0:512].rearrange("p (a b) -> p a b", a=4),
                                                                                  func=AF.Copy), reads=[tp], writes=[ktok[d]])
                    qi = [0, 0]
                    prev_ci = [32, None]
                    for d in range(2):
                        S.op("dve", lambda e, d=d: e.tensor_copy(out=Sfb[d][0][:], in_=Sst[d][:, h, :]), reads=[Sst[d]], writes=[Sfb[d][0]])

                    def o_store(t, oTp, first):
                        tok = slice(t * 128, (t + 1) * 128)
                        if first:
                            S.op("act", lambda e: e.activation(out=oacc[:, tok], in_=oTp[:, 0:128], func=AF.Copy), reads=[oTp], writes=[oacc])
                        else:
                            S.op("dve", lambda e: e.tensor_tensor(out=oacc[:, tok], in0=oTp[:, 0:128], in1=oacc[:, tok], op=ALU.add),
                                 reads=[oTp, oacc], writes=[oacc])

                    def step_pre(d, t):
                        tok = slice(t * 128, (t + 1) * 128)
                        order = (0, 1) if d == 0 else (1, 0)
                        mask = maskA if d == 0 else maskB
                        ATp = hps.next()
                        mm(ATp, ATp[:, 0:128], [(kh[d][:, tok], qh[d][:, tok])], [kh[d], qh[d]])
                        Pbs = {}
                        for c in order:
                            ck = slice(c * 64, (c + 1) * 64)
                            Pb = hps.next()
                            Pbs[c] = Pb
                            S.op("pe", lambda e, c=c, ck=ck, Pb=Pb: e.matmul(Pb[:, 0:128], lhsT=ktok[d][ck, t, :], rhs=v_own[ck, t, hc],
                                                                             start=True, stop=True),
                                 reads=[ktok[d], v_own], writes=[Pb])
                        at = ATs.next()
                        S.op("dve", lambda e: e.tensor_tensor(out=at[:], in0=ATp[:, 0:128], in1=mask[:], op=ALU.mult),
                             reads=[ATp, mask], writes=[at])
                        oTp = ATp
                        S.op("pe", lambda e: e.matmul(oTp[:, 0:128], lhsT=v_own[:, t, hc], rhs=at[:], start=True, stop=False),
                             reads=[v_own, at], writes=[oTp], inc=False)
                        return (d, t, order, Pbs, oTp)

                    def step_chunk(ctx_, k):
                        d, t, order, Pbs, oTp = ctx_
                        c = order[k]
                        ck = slice(c * 64, (c + 1) * 64)
                        toks = slice(t * 128 + c * 64, t * 128 + (c + 1) * 64)
                        ci = t * 2 + c
                        ei = prev_ci[0] if d == 0 else ci
                        qo, qn = Sfb[d][qi[d]], Sfb[d][1 - qi[d]]
                        qi[d] = 1 - qi[d]
                        nb = Sbf[d].next()
                        S.op("act", lambda e: e.activation(out=nb[:], in_=qo[:], func=AF.Identity, scale=expT[d][:, ei:ei + 1]),
                             reads=[qo, expT[d]], writes=[nb])
                        S.op("pe", lambda e: e.matmul(oTp[:, ck], lhsT=nb[:], rhs=qh[d][:, toks], start=False, stop=(k == 1)),
                             reads=[nb, qh[d]], writes=[oTp], inc=(k == 1))
                        S.op("dve", lambda e: e.scalar_tensor_tensor(out=qn[:], in0=qo[:], scalar=expT[d][:, ei:ei + 1], in1=Pbs[c][:, 0:128],
                                                                      op0=ALU.mult, op1=ALU.add), reads=[qo, expT[d], Pbs[c]], writes=[qn])
                        if d == 0:
                            prev_ci[0] = ci

                    for i in range(16):
                        ca = step_pre(0, i)
                        cb = step_pre(1, 15 - i)
                        for k in range(2):
                            step_chunk(ca, k)
                            step_chunk(cb, k)
                        o_store(i, ca[4], first=(i <= 7))
                        o_store(15 - i, cb[4], first=(i <= 7))
                    for g in range(4):
                        cs = slice(g * 512, (g + 1) * 512)
                        S.op("act", lambda e, cs=cs: e.activation(out=og_sq[:], in_=oacc[:, cs], func=AF.Square), reads=[oacc], writes=[og_sq])
                        ssp = hps.next()
                        mm(ssp, ssp[:], [(ones_bf[:], og_sq[:])], [ones_bf, og_sq])
                        S.op("act", lambda e, ssp=ssp: e.activation(out=og_sd[:], in_=ssp[:], func=AF.Sqrt, scale=1.0 / 128, bias=EPS),
                             reads=[ssp], writes=[og_sd])
                        S.op("dve", lambda e: e.reciprocal(out=og_sd[:], in_=og_sd[:]), reads=[og_sd], writes=[og_sd])
                        S.op("dve", lambda e, cs=cs: e.scalar_tensor_tensor(out=og_t1[:], in0=oacc[:, cs], scalar=gout_t[:, 0:1], in1=og_sd[:],
                                                                             op0=ALU.mult, op1=ALU.mult),
                             reads=[oacc, gout_t, og_sd], writes=[og_t1])
                        S.op("dve", lambda e, cs=cs: e.tensor_tensor(out=aT[:, h, cs], in0=og_t1[:], in1=gT[:, cs], op=ALU.mult),
                             reads=[og_t1, gT], writes=[aT])
                if stage == 3:
                    for h in range(4):
                        dump(aT, aT[:, h, 0:1024], 1024)
                S.barrier()
            sw.close()
            if stage == 3:
                S.barrier()
                return nc, S

            with ExitStack() as scm:
                bmT = sb(scm, "bmT", [128, 4, NT], BF16)
                modB = sb(scm, "modB", [128, 4 * D], F32)
                with ExitStack() as sc:
                    wu = sb(sc, "wu", [128, 8, 512], BF16)
                    wcv = sb(sc, "wcv", [128, 8, 512], BF16)
                    lng_bc = sb(sc, "lng_bc", [128, 512], F32)
                    lnb_bc = sb(sc, "lnb_bc", [128, 512], F32)
                    bs_bc = sb(sc, "bs_bc", [128, 512], F32)
                    wsT_bf = sb(sc, "wsT_bf", [128, 512], BF16)
                    S.dma("pool", wu[:], w_in_v[:, :, 2560:3072], writes=[wu])
                    S.dma("pool", wcv[:], w_in_v[:, :, 3072:3584], writes=[wcv])
                    S.dma("pool", wsT_bf[:], wsT[:, :], writes=[wsT_bf])
                    S.dma("sp", lng_bc[:], lng[0:1, :].partition_broadcast(128), writes=[lng_bc])
                    S.dma("sp", lnb_bc[:], lnb[0:1, :].partition_broadcast(128), writes=[lnb_bc])
                    S.dma("sp", bs_bc[:], bsp[0:1, :].partition_broadcast(128), writes=[bs_bc])
                    uTs = Ring([sb(sc, "uT%d" % i, [128, 4, 512], BF16) for i in range(2)])
                    ges = Ring([sb(sc, "ge%d" % i, [128, 512], F32) for i in range(2)])
                    xns = Ring([sb(sc, "xn%d" % i, [128, 512], F32) for i in range(2)])
                    vlns = Ring([sb(sc, "vln%d" % i, [128, 512], BF16) for i in range(2)])
                    t1s = Ring([sb(sc, "ct1%d" % i, [128, 512], F32) for i in range(2)])
                    csts = Ring([sb(sc, "cst%d" % i, [128, 12], F32) for i in range(4)])
                    cps = Ring([psb(sc, "cps%d" % i, [128, 512], F32) for i in range(6)])
                    wada2 = [sb(sc, "wadb%d" % i, [128, 8, 512], BF16) for i in range(2)]
                    bada2 = [sb(sc, "badb%d" % i, [128, 512], F32) for i in range(2)]
                    gn2 = sb(sc, "gn2", [128, D], F32)

                    def p1_late(j):
                        wb_, bb_ = wada2[j % 2], bada2[j % 2]
                        S.dma("pool", wb_[:], w_ada_v[:, :, j * 512:(j + 1) * 512], writes=[wb_])
                        S.dma("sp", bb_[:], b_ada[0:1, j * 512:(j + 1) * 512].partition_broadcast(128), writes=[bb_])
                        ps = cps.next()
                        mm(ps, ps[:], [(screp[:, kc, :], wb_[:, kc, :]) for kc in range(8)], [screp, wb_])
                        S.op("dve", lambda e: e.tensor_tensor(out=modB[:, (j - 4) * 512:(j - 3) * 512], in0=ps[:], in1=bb_[:], op=ALU.add),
                             reads=[ps, bb_], writes=[modB])

                    def late_scale(lo, g0, plus1):
                        S.dma("sp", gn2[:], gains[0:1, g0 * D:(g0 + 1) * D].partition_broadcast(128), writes=[gn2])
                        if plus1:
                            S.op("dve", lambda e: e.scalar_tensor_tensor(out=modB[:, lo:lo + D], in0=modB[:, lo:lo + D], scalar=1.0, in1=gn2[:],
                                                                          op0=ALU.add, op1=ALU.mult), reads=[modB, gn2], writes=[modB])
                        else:
                            S.op("dve", lambda e: e.tensor_tensor(out=modB[:, lo:lo + D], in0=modB[:, lo:lo + D], in1=gn2[:], op=ALU.mult),
                                 reads=[modB, gn2], writes=[modB])

                    for tg in range(4):
                        p1_late(4 + 2 * tg); p1_late(5 + 2 * tg)
                        cs = slice(tg * 512, (tg + 1) * 512)
                        uTg = uTs.next()
                        for g in range(4):
                            ps = cps.next()
                            mm(ps, ps[:], [(wu[:, kc, g * 128:(g + 1) * 128], hT_own[:, kc, cs]) for kc in range(8)], [wu, hT_own])
                            S.op("act", lambda e, ps=ps, g=g, uTg=uTg: e.activation(out=uTg[:, g, :], in_=ps[:], func=AF.Gelu_apprx_tanh),
                                 reads=[ps], writes=[uTg])
                        def cm_a(t):
                            tile_ = tg * 4 + t
                            tok = slice(tile_ * 128, (tile_ + 1) * 128)
                            ps = cps.next()
                            mm(ps, ps[:], [(hT_own[:, kc, tok], wcv[:, kc, :]) for kc in range(8)], [hT_own, wcv])
                            ge = ges.next(); xn = xns.next(); vln = vlns.next(); st = csts.next()
                            S.op("act", lambda e: e.activation(out=ge[:], in_=ps[:], func=AF.Gelu_apprx_tanh), reads=[ps], writes=[ge])
                            S.op("dve", lambda e: e.bn_stats(out=st[:, 0:6], in_=ge[:]), reads=[ge], writes=[st])
                            S.op("dve", lambda e: e.bn_aggr(out=st[:, 6:8], in_=st[:, 0:6]), reads=[st], writes=[st])
                            S.op("act", lambda e: e.activation(out=st[:, 8:9], in_=st[:, 7:8], func=AF.Sqrt, bias=EPS), reads=[st], writes=[st])
                            S.op("dve", lambda e: e.reciprocal(out=st[:, 9:10], in_=st[:, 8:9]), reads=[st], writes=[st])
                            S.op("dve", lambda e: e.tensor_scalar(out=xn[:], in0=ge[:], scalar1=st[:, 6:7], scalar2=st[:, 9:10],
                                                                   op0=ALU.subtract, op1=ALU.mult), reads=[ge, st], writes=[xn])
                            S.op("dve", lambda e: e.tensor_tensor(out=xn[:], in0=xn[:], in1=lng_bc[:], op=ALU.mult), reads=[xn, lng_bc], writes=[xn])
                            S.op("dve", lambda e: e.tensor_tensor(out=vln[:], in0=xn[:], in1=lnb_bc[:], op=ALU.add), reads=[xn, lnb_bc], writes=[vln])
                            return (t, tok, vln)

                        def cm_b(t, tok, vln):
                            zps = cps.next()
                            for g in range(4):
                                S.op("pe", lambda e, g=g: e.matmul(zps[:, g * 128:(g + 1) * 128], lhsT=vln[:, g * 128:(g + 1) * 128],
                                                                   rhs=wsT_bf[:, g * 128:(g + 1) * 128], start=True, stop=True, skip_group_check=True),
                                     reads=[vln, wsT_bf], writes=[zps], inc=(g == 3))
                            t1 = t1s.next()
                            S.op("dve", lambda e: e.tensor_tensor(out=t1[:], in0=zps[:], in1=bs_bc[:], op=ALU.add), reads=[zps, bs_bc], writes=[t1])
                            S.op("dve", lambda e: e.tensor_tensor(out=bmT[:, :, tok], in0=t1[:].rearrange("p (a b) -> p a b", a=4),
                                                                  in1=uTg[:, :, t * 128:(t + 1) * 128], op=ALU.mult), reads=[t1, uTg], writes=[bmT])

                        prev = None
                        for t in range(4):
                            cur_ = cm_a(t)
                            if prev is not None:
                                cm_b(*prev)
                            prev = cur_
                        cm_b(*prev)
                    late_scale(0, 1, False); late_scale(2 * D, 2, True); late_scale(3 * D, 3, False)
                    S.dma("sp", g2s[:, :], modB[:, 3 * D:4 * D], reads=[modB], writes=[g2s_b])
                    if stage == 4:
                        for g in range(4):
                            dump(bmT, bmT[:, g, 0:1024], 1024)
                    S.barrier()
                if stage == 4:
                    S.barrier()
                    return nc, S

                with ExitStack() as sg:
                    yT = sb(sg, "yT", [128, 8, NT], BF16)
                    wo = sb(sg, "wo", [128, 8, D], BF16)
                    S.dma("pool", wo[:], w_o.rearrange("(kc p) n -> p kc n", p=128), writes=[wo])
                    sgc = ExitStack()
                    wgas = Ring([sb(sgc, "wga%d" % i, [128, 8, 128], BF16) for i in range(2)])
                    wgbs = Ring([sb(sgc, "wgb%d" % i, [128, 8, 128], BF16) for i in range(2)])
                    was = Ring([sb(sgc, "wa%d" % i, [128, 4, 128], BF16) for i in range(2)])
                    wbs = Ring([sb(sgc, "wb%d" % i, [128, 4, 128], BF16) for i in range(2)])
                    sgs = Ring([sb(sgc, "sgm%d" % i, [128, 512], F32) for i in range(2)])
                    y1s = Ring([sb(sgc, "y1%d" % i, [128, 512], F32) for i in range(2)])
                    y2s = Ring([sb(sgc, "y2%d" % i, [128, 512], F32) for i in range(2)])
                    gps = Ring([psb(sgc, "gps%d" % i, [128, 512], F32) for i in range(8)])
                    w_a_v = w_a.rearrange("(kc p) n -> p kc n", p=128)
                    w_b_v = w_b.rearrange("(kc p) n -> p kc n", p=128)
                    for c in range(8):
                        wga = wgas.next(); wgb = wgbs.next(); wa_ = was.next(); wb_ = wbs.next()
                        S.dma("pool", wga[:], w_in_v[:, :, 3584 + c * 128:3584 + (c + 1) * 128], writes=[wga])
                        S.dma("pool", wgb[:], w_in_v[:, :, 4608 + c * 128:4608 + (c + 1) * 128], writes=[wgb])
                        S.dma("pool", wa_[:], w_a_v[:, :, c * 128:(c + 1) * 128], writes=[wa_])
                        S.dma("pool", wb_[:], w_b_v[:, :, c * 128:(c + 1) * 128], writes=[wb_])
                        for tg in range(4):
                            cs = slice(tg * 512, (tg + 1) * 512)
                            pa = gps.next()
                            mm(pa, pa[:], [(wa_[:, kc, :], aT[:, kc, cs]) for kc in range(4)], [wa_, aT])
                            pg = gps.next()
                            mm(pg, pg[:], [(wga[:, kc, :], hT_own[:, kc, cs]) for kc in range(8)], [wga, hT_own])
                            sga = sgs.next(); y1 = y1s.next()
                            S.op("act", lambda e, pg=pg, sga=sga: e.activation(out=sga[:], in_=pg[:], func=AF.Sigmoid), reads=[pg], writes=[sga])
                            S.op("dve", lambda e, sga=sga, pa=pa, y1=y1: e.tensor_tensor(out=y1[:], in0=pa[:], in1=sga[:], op=ALU.mult),
                                 reads=[pa, sga], writes=[y1])
                            pb = gps.next()
                            mm(pb, pb[:], [(wb_[:, kc, :], bmT[:, kc, cs]) for kc in range(4)], [wb_, bmT])
                            pg2 = gps.next()
                            mm(pg2, pg2[:], [(wgb[:, kc, :], hT_own[:, kc, cs]) for kc in range(8)], [wgb, hT_own])
                            sgb = sgs.next(); y2 = y2s.next()
                            S.op("act", lambda e, pg2=pg2, sgb=sgb: e.activation(out=sgb[:], in_=pg2[:], func=AF.Sigmoid), reads=[pg2], writes=[sgb])
                            S.op("dve", lambda e, sgb=sgb, pb=pb, y2=y2: e.tensor_tensor(out=y2[:], in0=pb[:], in1=sgb[:], op=ALU.mult),
                                 reads=[pb, sgb], writes=[y2])
                            S.op("dve", lambda e, y1=y1, y2=y2, c=c, cs=cs: e.tensor_tensor(out=yT[:, c, cs], in0=y1[:], in1=y2[:], op=ALU.add),
                                 reads=[y1, y2], writes=[yT])
                    if stage == 5:
                        for c in range(4):
                            dump(yT, yT[:, c, 0:1024], 1024)
                    S.barrier()
                    sgc.close()
                    xts = Ring([sb(sg, "mxt%d" % i, [128, D], F32) for i in range(2)])
                    x1t = Ring([sb(sg, "x1t%d" % i, [128, D], F32) for i in range(2)])
                    hns = Ring([sb(sg, "fhn%d" % i, [128, D], F32) for i in range(1)])
                    h2s = Ring([sb(sg, "fh2%d" % i, [128, D], F32) for i in range(2)])
                    h2Tf = Ring([sb(sg, "h2Tf%d" % i, [128, 8, 128], F32) for i in range(2)])
                    h2Tt = Ring([sb(sg, "h2Tt%d" % i, [128, 8, 128], BF16) for i in range(2)])
                    mjunk = sb(sg, "mjunk", [128, D], BF16)
                    msts = Ring([sb(sg, "mst%d" % i, [128, 8], F32) for i in range(4)])
                    wrt = sb(sg, "wrt", [128, 8, NE], F32)
                    brt_bc = sb(sg, "brt_bc", [128, NE], F32)
                    S.dma("sp", wrt[:], w_rt.rearrange("(kc p) n -> p kc n", p=128), writes=[wrt])
                    S.dma("sp", brt_bc[:], b_rt[0:1, :].partition_broadcast(128), writes=[brt_bc])
                    rt = {n: Ring([sb(sg, "rt_%s%d" % (n, i), [128, NE], F32) for i in range(3)]) for n in ("sc", "sel", "selm", "em", "w")}
                    m8g = Ring([sb(sg, "m8g%d" % i, [128, 8, 8], F32) for i in range(2)])
                    rsm = Ring([sb(sg, "rsm%d" % i, [128, 48], F32) for i in range(2)])
                    gps2 = Ring([psb(sg, "gps2_%d" % i, [128, 2 * 512], F32) for i in range(3)])
                    lpsr = Ring([psb(sg, "lps%d" % i, [128, 512], F32) for i in range(2)])

                    def ffn_a(t):
                        tok = slice(t * 128, (t + 1) * 128)
                        xt = xts.next(); x1 = x1t.next(); st = msts.next(); hn = hns.next(); h2 = h2s.next(); hf = h2Tf.next()
                        S.dma("sp", xt[:], x_own[tok, :], writes=[xt])
                        pz = gps2.next()
                        for n in range(2):
                            mm(pz, pz[:, n * 512:(n + 1) * 512], [(yT[:, kc, tok], wo[:, kc, n * 512:(n + 1) * 512]) for kc in range(8)], [yT, wo])
                        S.op("act", lambda e: e.activation(out=mjunk[:], in_=pz[:], func=AF.Square, accum_out=st[:, 0:1]), reads=[pz], writes=[mjunk, st])
                        S.op("act", lambda e: e.activation(out=st[:, 1:2], in_=st[:, 0:1], func=AF.Sqrt, scale=1.0 / D, bias=EPS), reads=[st], writes=[st])
                        S.op("dve", lambda e: e.reciprocal(out=st[:, 2:3], in_=st[:, 1:2]), reads=[st], writes=[st])
                        S.op("dve", lambda e: e.scalar_tensor_tensor(out=x1[:], in0=pz[:], scalar=st[:, 2:3], in1=modB[:, 0:D],
                                                                      op0=ALU.mult, op1=ALU.mult), reads=[pz, st, modB], writes=[x1])
                        S.op("dve", lambda e: e.tensor_tensor(out=x1[:], in0=x1[:], in1=xt[:], op=ALU.add), reads=[x1, xt], writes=[x1])
                        S.dma("sp", x1s[tok, :], x1[:], reads=[x1], writes=[x1s_bs[t % 4]])
                        if stage == 6 and t < 4:
                            dump(x1, x1[:, :], 1024)
                        S.op("act", lambda e: e.activation(out=mjunk[:], in_=x1[:], func=AF.Square, accum_out=st[:, 3:4]), reads=[x1], writes=[mjunk, st])
                        S.op("act", lambda e: e.activation(out=st[:, 4:5], in_=st[:, 3:4], func=AF.Sqrt, scale=1.0 / D, bias=EPS), reads=[st], writes=[st])
                        S.op("dve", lambda e: e.reciprocal(out=st[:, 5:6], in_=st[:, 4:5]), reads=[st], writes=[st])
                        S.op("dve", lambda e: e.scalar_tensor_tensor(out=hn[:], in0=x1[:], scalar=st[:, 5:6], in1=modB[:, 2 * D:3 * D],
                                                                      op0=ALU.mult, op1=ALU.mult), reads=[x1, st, modB], writes=[hn])
                        S.op("dve", lambda e: e.tensor_tensor(out=h2[:], in0=hn[:], in1=modB[:, D:2 * D], op=ALU.add), reads=[hn, modB], writes=[h2])
                        return (t, tok, h2, hf)

                    def ffn_a2(t, tok, h2, hf):
                        tp = gps2.next()
                        for kc in range(8):
                            S.op("pe", lambda e, kc=kc: e.transpose(tp[:, kc * 128:(kc + 1) * 128], h2[:, kc * 128:(kc + 1) * 128], ident_f[:]),
                                 reads=[h2, ident_f], writes=[tp], inc=(kc == 7))
                        hb16 = h2Tt.next()
                        S.op("act", lambda e: e.activation(out=hb16[:], in_=tp[:].rearrange("p (a b) -> p a b", a=8), func=AF.Copy),
                             reads=[tp], writes=[hb16])
                        S.dma("sp", h2Td[:, :, tok], hb16[:], reads=[hb16], writes=[h2Td_b])
                        S.op("act", lambda e: e.activation(out=hf[:], in_=tp[:].rearrange("p (a b) -> p a b", a=8), func=AF.Copy),
                             reads=[tp], writes=[hf])
                        lps = lpsr.next()
                        mm(lps, lps[:, 0:NE], [(hf[:, kc, :], wrt[:, kc, :]) for kc in range(8)], [hf, wrt])
                        sc_ = rt["sc"].next()
                        S.op("act", lambda e: e.activation(out=sc_[:], in_=lps[:, 0:NE], func=AF.Sigmoid), reads=[lps], writes=[sc_])
                        return sc_

                    def ffn_b(t, sc_):
                        sel = rt["sel"].next(); selm = rt["selm"].next(); em = rt["em"].next(); w_ = rt["w"].next()
                        mg = m8g.next(); sm_ = rsm.next()
                        S.op("dve", lambda e: e.tensor_tensor(out=sel[:], in0=sc_[:], in1=brt_bc[:], op=ALU.add), reads=[sc_, brt_bc], writes=[sel])
                        for g in range(8):
                            S.op("dve", lambda e, g=g: e.max(out=mg[:, g, :], in_=sel[:, g * 8:(g + 1) * 8]), reads=[sel], writes=[mg])
                        S.op("dve", lambda e: e.tensor_tensor(out=sm_[:, 0:8], in0=mg[:, :, 0], in1=mg[:, :, 1], op=ALU.add), reads=[mg], writes=[sm_])
                        S.op("dve", lambda e: e.max(out=sm_[:, 8:16], in_=sm_[:, 0:8]), reads=[sm_], writes=[sm_])
                        S.op("dve", lambda e: e.tensor_scalar(out=sm_[:, 16:24], in0=sm_[:, 0:8], scalar1=sm_[:, 11:12], scalar2=None, op0=ALU.is_ge),
                             reads=[sm_], writes=[sm_])
                        S.op("dve", lambda e: e.tensor_scalar(out=sm_[:, 24:32], in0=sm_[:, 16:24], scalar1=4.0, scalar2=-4.0, op0=ALU.mult, op1=ALU.add),
                             reads=[sm_], writes=[sm_])
                        S.op("dve", lambda e: e.tensor_tensor(out=selm[:].rearrange("p (a b) -> p a b", a=8), in0=sel[:].rearrange("p (a b) -> p a b", a=8),
                                                              in1=sm_[:, 16:24].unsqueeze(2).to_broadcast([128, 8, 8]), op=ALU.mult),
                             reads=[sel, sm_], writes=[selm])
                        S.op("dve", lambda e: e.tensor_tensor(out=selm[:].rearrange("p (a b) -> p a b", a=8), in0=selm[:].rearrange("p (a b) -> p a b", a=8),
                                                              in1=sm_[:, 24:32].unsqueeze(2).to_broadcast([128, 8, 8]), op=ALU.add),
                             reads=[selm, sm_], writes=[selm])
                        S.op("dve", lambda e: e.max(out=sm_[:, 32:40], in_=selm[:]), reads=[selm], writes=[sm_])
                        S.op("dve", lambda e: e.tensor_scalar(out=em[:], in0=selm[:], scalar1=sm_[:, 39:40], scalar2=None, op0=ALU.is_ge),
                             reads=[selm, sm_], writes=[em])
                        S.op("dve", lambda e: e.tensor_tensor(out=w_[:], in0=sc_[:], in1=em[:], op=ALU.mult), reads=[sc_, em], writes=[w_])
                        S.op("dve", lambda e: e.tensor_tensor_scan(out=em[:], data0=ones_f[:, 0:NE], data1=w_[:], initial=0.0, op0=ALU.mult, op1=ALU.add),
                             reads=[ones_f, w_], writes=[em])
                        S.op("dve", lambda e: e.tensor_scalar(out=sm_[:, 40:41], in0=em[:, NE - 1:NE], scalar1=0.4, scalar2=None, op0=ALU.mult),
                             reads=[em], writes=[sm_])
                        S.op("dve", lambda e: e.reciprocal(out=sm_[:, 41:42], in_=sm_[:, 40:41]), reads=[sm_], writes=[sm_])
                        S.op("dve", lambda e: e.tensor_scalar(out=wt_all[:, t, 0:NE], in0=w_[:], scalar1=sm_[:, 41:42], scalar2=None, op0=ALU.mult),
                             reads=[w_, sm_], writes=[wt_all])

                    pa = ffn_a(0)
                    prev_b = None
                    for t in range(16):
                        nxt = ffn_a(t + 1) if t + 1 < 16 else None
                        sc_t = ffn_a2(*pa)
                        if prev_b is not None:
                            ffn_b(*prev_b)
                        prev_b = (t, sc_t)
                        pa = nxt
                    ffn_b(*prev_b)
                    if stage == 7:
                        dump(wt_all, wt_all[:].rearrange("p a b -> p (a b)"), 16 * (NE + 1))
                    S.barrier()
        if stage in (5, 6):
            S.barrier()
            return nc, S

        with ExitStack() as se:
            h2T = sb(se, "h2T", [128, 8, NT], BF16)
            acc = sb(se, "acc", [128, 16, D], F32)
            acc_b = [Buf("acc%d" % t) for t in range(16)]
            h2T_b = [Buf("h2T_tg%d" % i) for i in range(4)]
            for i in range(4):
                S.dma("sp", h2T[:, :, i * 512:(i + 1) * 512], h2Td[:, :, i * 512:(i + 1) * 512], reads=[h2Td_b], writes=[h2T_b[i]])
            eps_ = Ring([psb(se, "eps%d" % i, [128, 512], F32) for i in range(8)])
            if stage == 7:
                for kc in range(2):
                    dump(h2T_b[0], h2T[:, kc, 0:512], 512); dump(h2T_b[1], h2T[:, kc, 512:1024], 512)
                S.barrier()
                return nc, S

            with ExitStack() as sx:
                wgus = Ring([sb(sx, "wgu%d" % i, [128, 8, 512], BF16) for i in range(3)])
                wdns = Ring([sb(sx, "wdn%d" % i, [128, 2, D], BF16) for i in range(3)])
                actTs = Ring([sb(sx, "actT%d" % i, [128, 2, 512], BF16) for i in range(3)])
                sgts = Ring([sb(sx, "sgt%d" % i, [128, 512], F32) for i in range(2)])
                xts_f = Ring([sb(sx, "oxt%d" % i, [128, D], F32) for i in range(2)])
                ots_f = Ring([sb(sx, "ot%d" % i, [128, D], F32) for i in range(2)])
                ojunk = sb(sx, "ojunk", [128, D], BF16)
                ost = Ring([sb(sx, "ost%d" % i, [128, 4], F32) for i in range(4)])
                g2t = sb(sx, "g2t", [128, D], F32)
                S.dma("sp", g2t[:], g2s[:, :], reads=[g2s_b], writes=[g2t])

                def final_tile(t):
                    tok = slice(t * 128, (t + 1) * 128)
                    xt = xts_f.next(); ot = ots_f.next(); st = ost.next()
                    S.dma("sp", xt[:], x1s[tok, :], reads=[x1s_bs[t % 4]], writes=[xt])
                    S.op("act", lambda e: e.activation(out=ojunk[:], in_=acc[:, t, :], func=AF.Square, accum_out=st[:, 0:1]),
                         reads=[acc_b[t]], writes=[ojunk, st])
                    S.op("act", lambda e: e.activation(out=st[:, 1:2], in_=st[:, 0:1], func=AF.Sqrt, scale=1.0 / D, bias=EPS), reads=[st], writes=[st])
                    S.op("dve", lambda e: e.reciprocal(out=st[:, 2:3], in_=st[:, 1:2]), reads=[st], writes=[st])
                    S.op("dve", lambda e: e.scalar_tensor_tensor(out=ot[:], in0=acc[:, t, :], scalar=st[:, 2:3], in1=g2t[:],
                                                                  op0=ALU.mult, op1=ALU.mult), reads=[acc_b[t], st, g2t], writes=[ot])
                    S.op("dve", lambda e: e.tensor_tensor(out=ot[:], in0=ot[:], in1=xt[:], op=ALU.add), reads=[ot, xt], writes=[ot])
                    S.dma("sp", out[tok, :], ot[:], reads=[ot], writes=[out_bs[t % 4]])

                n_exp = NE + 1 if stage >= 9 else (2 if stage == 8 else NE + 1)
                wbuf = {}

                def moe_load(ex):
                    wg = wgus.next(); wd = wdns.next()
                    S.dma("pool", wg[:], w_gu[ex].rearrange("(kc p) n -> p kc n", p=128), writes=[wg])
                    S.dma("pool", wd[:], w_dn[ex].rearrange("(kc p) n -> p kc n", p=128), writes=[wd])
                    wbuf[ex] = (wg, wd)

                def moe_s1(ex, tg):
                    wg = wbuf[ex][0]
                    cs = slice(tg * 512, (tg + 1) * 512)
                    G = []
                    for c in range(4):
                        ps = eps_.next()
                        mm(ps, ps[:], [(wg[:, kc, c * 128:(c + 1) * 128], h2T[:, kc, cs]) for kc in range(8)], [wg, h2T_b[tg]])
                        G.append(ps)
                    aT_ = actTs.next()
                    for j in range(2):
                        sgt = sgts.next()
                        S.op("act", lambda e, sgt=sgt, g_=G[j]: e.activation(out=sgt[:], in_=g_[:], func=AF.Silu), reads=[G[j]], writes=[sgt])
                        S.op("dve", lambda e, sgt=sgt, j=j, g_=G[2 + j]: e.tensor_tensor(out=aT_[:, j, :], in0=g_[:], in1=sgt[:], op=ALU.mult),
                             reads=[G[2 + j], sgt], writes=[aT_])
                    return aT_

                def moe_s2(ex, tg, aT_):
                    wd = wbuf[ex][1]
                    for t in range(4):
                        tile_ = tg * 4 + t
                        for n in range(2):
                            dps = eps_.next()
                            mm(dps, dps[:], [(aT_[:, j, t * 128:(t + 1) * 128], wd[:, j, n * 512:(n + 1) * 512]) for j in range(2)], [aT_, wd])
                            S.op("dve", lambda e, dps=dps, tile_=tile_, n=n: e.scalar_tensor_tensor(
                                out=acc[:, tile_, n * 512:(n + 1) * 512], in0=dps[:], scalar=wt_all[:, tile_, ex:ex + 1],
                                in1=acc[:, tile_, n * 512:(n + 1) * 512], op0=ALU.mult, op1=ALU.add),
                                reads=[dps, wt_all, acc_b[tile_]], writes=[acc_b[tile_]])

                its = [(ex, tg) for ex in range(n_exp) for tg in range(4)]
                moe_load(0)
                if n_exp > 1:
                    moe_load(1)
                for t in range(16):
                    S.op("pool", lambda e, t=t: e.memset(acc[:, t, :], 0.0), writes=[acc_b[t]])
                pend = None
                for i, (ex, tg) in enumerate(its):
                    if tg == 0 and ex >= 1 and ex + 1 < n_exp:
                        moe_load(ex + 1)
                    aT_ = moe_s1(ex, tg)
                    if pend is not None:
                        moe_s2(*pend)
                        if pend[0] == n_exp - 1:
                            for t_ in range(4):
                                final_tile(pend[1] * 4 + t_)
                    pend = (ex, tg, aT_)
                moe_s2(*pend)
                for t_ in range(4):
                    final_tile(pend[1] * 4 + t_)
                S.barrier()

        S.barrier()
    return nc, S


def _consts():
    s = np.arange(128)[:, None]; t = np.arange(128)[None, :]
    same = (s // 64) == (t // 64)
    maskA = (same & (s <= t)).astype(np.float32)
    maskB = (same & (s >= t)).astype(np.float32)
    resetm = np.ones((128, 512), np.float32); resetm[:, ::64] = 0.0
    return np.eye(128, dtype=np.float32), maskA, maskB, resetm


def make_in_maps(inp):
    f = lambda a: np.ascontiguousarray(np.asarray(a, dtype=np.float32))
    x = f(inp["x"]); ctx = f(inp["ctx"]); c = f(inp["c"]); cc = f(inp["c_ctx"])
    w_in = f(inp["w_in"][0])
    w_in_sw = w_in.copy(); w_in_sw[:, 512:1024] = w_in[:, 1024:1536]; w_in_sw[:, 1024:1536] = w_in[:, 512:1024]
    lbl = f(inp["lb_logits"])
    ws = f(inp["w_spatial"][0]); bs = f(inp["b_spatial"][0])
    gains = np.concatenate([f(inp[k][0]) for k in ("g_pre_mix", "g_post_mix", "g_pre_ffn", "g_post_ffn")])[None, :]
    w_gu = np.concatenate([f(inp["w_expert_gu"][0]), f(inp["w_shared_gu"])], axis=0)
    w_dn = np.concatenate([f(inp["w_expert_down"][0]), f(inp["w_shared_down"])], axis=0)
    ident, maskA, maskB, resetm = _consts()
    shared = dict(w_ada=f(inp["w_ada"][0]), b_ada=f(inp["b_ada"]), gains=f(gains), gout=f(inp["g_hgrn_out"][0][:, None]),
                  lng=f(inp["cm_ln_g"]), lnb=f(inp["cm_ln_b"]), w_a=f(inp["w_branch_a"][0]), w_b=f(inp["w_branch_b"][0]),
                  w_o=f(inp["w_out"][0]), w_rt=f(inp["w_router"][0]), b_rt=f(inp["b_router"]), w_gu=w_gu, w_dn=w_dn,
                  ident=ident, maskA=maskA, maskB=maskB, resetm=resetm)
    maps = []
    for i in range(8):
        b, half = i // 2, i % 2
        m = dict(shared)
        if half == 0:
            xs = x[b]; m["ctx"] = f(ctx[b]); m["w_in"] = w_in; dirs = [0, 1]; ws_l, bs_l = ws, bs
        else:
            xs = x[b, ::-1]; m["ctx"] = f(ctx[b, ::-1]); m["w_in"] = w_in_sw; dirs = [1, 0]
            ws_l, bs_l = ws[:, ::-1, ::-1], bs[::-1, :]
        m["x_own"] = f(xs[0:NT]); m["x_oth"] = f(xs[NT:2 * NT])
        m["cvec"] = f(np.concatenate([c[b].reshape(8, 128).T, cc.reshape(8, 128).T], axis=1))
        l4 = lbl[dirs].reshape(2, 2, 4, 128)
        m["lbl"] = f(l4.transpose(3, 0, 1, 2).reshape(128, 16))
        m["wsT"] = f(ws_l.transpose(2, 0, 1).reshape(128, 512))
        m["bsp"] = f(bs_l.T.reshape(1, 512))
        maps.append(m)
    return maps


_CACHE = {}


def kernel(**inputs):
    if "nc" not in _CACHE:
        _CACHE["nc"] = build_program()[0]
    nc = _CACHE["nc"]
    maps = make_in_maps(inputs)
    res = run_bass_kernel_spmd(nc, maps, core_ids=list(range(8)))
    B, T = inputs["x"].shape[0], inputs["x"].shape[1]
    out = np.empty((B, T, D), np.float32)
    for i in range(8):
        b, half = i // 2, i % 2
        r = np.asarray(res.results[i]["out"], dtype=np.float32)
        if half == 0:
            out[b, 0:NT] = r
        else:
            out[b, NT:2 * NT] = r[::-1]
    return out
```
